# Optimizing a Trainium2 kernel written in Bass

```python
import jax
import jax.numpy as jnp
from jax import lax
import numpy as np

D_MODEL = 1024
BATCH = 2
SEQ = 8192
DEPTH = 2

HEAD_DIM = 64
BLOCK = 128
NORM_EPS = 1e-6
FOX_HEADS = 4
FORGET_BIAS_MEAN = 2.0
SWA_Q_HEADS = 4
SWA_KV_HEADS = 2
SWA_GROUP = SWA_Q_HEADS // SWA_KV_HEADS
SWA_WINDOW = 128
MLA_HEADS = 4
MLA_Q_LORA = 256
MLA_KV_LORA = 128
MLA_NOPE_DIM = 64
MLA_ROPE_DIM = 32
MLA_V_DIM = 64
ROPE_THETA = 10000.0
DIL_HEADS = 4
DIL_PATTERNS = ((128, 1), (512, 4), (2048, 16))
MIX_WIDTH = (FOX_HEADS + SWA_Q_HEADS + DIL_HEADS) * HEAD_DIM + MLA_HEADS * MLA_V_DIM
IN_SIZES = (
    FOX_HEADS * HEAD_DIM, FOX_HEADS * HEAD_DIM, FOX_HEADS * HEAD_DIM, FOX_HEADS,
    SWA_Q_HEADS * HEAD_DIM, SWA_KV_HEADS * HEAD_DIM, SWA_KV_HEADS * HEAD_DIM,
    MLA_Q_LORA, MLA_KV_LORA, MLA_ROPE_DIM,
    DIL_HEADS * HEAD_DIM, DIL_HEADS * HEAD_DIM, DIL_HEADS * HEAD_DIM,
)
N_IN = sum(IN_SIZES)
FFN_DIM = 3584
N_EXPERTS = 8
TOP_K = 2
N_DENSE = (DEPTH + 1) // 2
N_MOE = DEPTH // 2

kernel_name = "hybrid_parallel_heads_fox_swa_mla_dilated_moe"


def rms_norm(x, g):
    xf = x.astype(jnp.float32)
    y = xf * lax.rsqrt(jnp.mean(xf * xf, axis=-1, keepdims=True) + NORM_EPS)
    return (y * g.astype(jnp.float32)).astype(x.dtype)


def to_heads(t, n_heads):
    B, S, _ = t.shape
    return t.reshape(B, S, n_heads, -1).transpose(0, 2, 1, 3)


def from_heads(t):
    B, H, S, d = t.shape
    return t.transpose(0, 2, 1, 3).reshape(B, S, H * d)


def alibi_slopes():
    n = SWA_Q_HEADS + DIL_HEADS
    return 2.0 ** (-8.0 * jnp.arange(1, n + 1, dtype=jnp.float32) / n)


def rope(t, positions):
    half = t.shape[-1] // 2
    inv_freq = ROPE_THETA ** (-jnp.arange(half, dtype=jnp.float32) / half)
    ang = positions[:, None, :, None].astype(jnp.float32) * inv_freq
    cos, sin = jnp.cos(ang), jnp.sin(ang)
    t1 = t[..., :half].astype(jnp.float32)
    t2 = t[..., half:].astype(jnp.float32)
    return jnp.concatenate([t1 * cos - t2 * sin, t2 * cos + t1 * sin], axis=-1).astype(t.dtype)


def causal_dense_attention(q, k, v, scale, log_decay=None):
    B, H, S, dq = q.shape
    nb = S // BLOCK
    key_pos = jnp.arange(S)
    xs = {"i": jnp.arange(nb), "q": jnp.moveaxis(q.reshape(B, H, nb, BLOCK, dq), 2, 0)}
    if log_decay is not None:
        xs["d"] = jnp.moveaxis(log_decay.reshape(B, H, nb, BLOCK), 2, 0)

    def one_block(blk):
        s = jnp.einsum("bhqd,bhkd->bhqk", blk["q"], k).astype(jnp.float32) * scale
        if "d" in blk:
            s = s + blk["d"][..., :, None] - log_decay[..., None, :]
        q_pos = blk["i"] * BLOCK + jnp.arange(BLOCK)
        s = jnp.where(key_pos[None, :] <= q_pos[:, None], s, -jnp.inf)
        p = jax.nn.softmax(s, axis=-1)
        return jnp.einsum("bhqk,bhkd->bhqd", p.astype(v.dtype), v)

    out = lax.map(one_block, xs)
    return jnp.moveaxis(out, 0, 2).reshape(B, H, S, v.shape[-1])


def banded_attention(q, k, v, span, slopes, dist_unit, scale):
    B, K, G, L, dh = q.shape
    dv = v.shape[-1]
    nb = -(-L // BLOCK)
    pad = nb * BLOCK - L
    q = jnp.pad(q, ((0, 0), (0, 0), (0, 0), (0, pad), (0, 0))).reshape(B, K, G, nb, BLOCK, dh)
    kv_pad = ((0, 0), (0, 0), (BLOCK, pad), (0, 0))
    k = jnp.pad(k, kv_pad).reshape(B, K, nb + 1, BLOCK, dh)
    v = jnp.pad(v, kv_pad).reshape(B, K, nb + 1, BLOCK, dv)
    k_band = jnp.concatenate([k[:, :, :-1], k[:, :, 1:]], axis=3)
    v_band = jnp.concatenate([v[:, :, :-1], v[:, :, 1:]], axis=3)
    s = jnp.einsum("bkgnqd,bknjd->bkgnqj", q, k_band).astype(jnp.float32) * scale
    dist = BLOCK + jnp.arange(BLOCK)[:, None] - jnp.arange(2 * BLOCK)[None, :]
    key_idx = (jnp.arange(nb)[:, None] - 1) * BLOCK + jnp.arange(2 * BLOCK)[None, :]
    valid = ((dist >= 0) & (dist <= span))[None] & (key_idx >= 0)[:, None, :]
    s = s - slopes.astype(jnp.float32)[None, :, :, None, None, None] * (dist * dist_unit).astype(jnp.float32)
    s = jnp.where(valid, s, -jnp.inf)
    m = jnp.max(s, axis=-1, keepdims=True)
    p = jnp.exp(s - m)
    l = jnp.sum(p, axis=-1, keepdims=True)
    o = jnp.einsum("bkgnqj,bknjd->bkgnqd", (p / l).astype(v.dtype), v_band)
    lse = (m + jnp.log(l))[..., 0]
    o = o.reshape(B, K, G, nb * BLOCK, dv)[:, :, :, :L]
    lse = lse.reshape(B, K, G, nb * BLOCK)[:, :, :, :L]
    return o, lse


def fox_mixer(q, k, v, f_logit, b_forget):
    qh, kh, vh = to_heads(q, FOX_HEADS), to_heads(k, FOX_HEADS), to_heads(v, FOX_HEADS)
    log_f = jax.nn.log_sigmoid(f_logit.astype(jnp.float32) + b_forget.astype(jnp.float32))
    cum = jnp.cumsum(jnp.transpose(log_f, (0, 2, 1)), axis=-1)
    return from_heads(causal_dense_attention(qh, kh, vh, HEAD_DIM ** -0.5, cum))


def swa_sink_mixer(q, k, v, sink, slopes):
    B, S, _ = q.shape
    qh = q.reshape(B, S, SWA_KV_HEADS, SWA_GROUP, HEAD_DIM).transpose(0, 2, 3, 1, 4)
    kh, vh = to_heads(k, SWA_KV_HEADS), to_heads(v, SWA_KV_HEADS)
    o, lse = banded_attention(qh, kh, vh, SWA_WINDOW - 1,
                              slopes.reshape(SWA_KV_HEADS, SWA_GROUP), 1, HEAD_DIM ** -0.5)
    sink = sink.astype(jnp.float32).reshape(SWA_KV_HEADS, SWA_GROUP)[None, :, :, None]
    o = o * jax.nn.sigmoid(lse - sink)[..., None].astype(o.dtype)
    return o.transpose(0, 3, 1, 2, 4).reshape(B, S, SWA_Q_HEADS * HEAD_DIM)


def mla_mixer(c_q, c_kv, k_rope, positions, q_norm, w_q_up, kv_norm, w_kv_up):
    B, S, _ = c_q.shape
    q = to_heads(rms_norm(c_q, q_norm) @ w_q_up, MLA_HEADS)
    kv = to_heads(rms_norm(c_kv, kv_norm) @ w_kv_up, MLA_HEADS)
    q = jnp.concatenate([q[..., :MLA_NOPE_DIM], rope(q[..., MLA_NOPE_DIM:], positions)], axis=-1)
    k_r = rope(k_rope[:, None], positions)
    k = jnp.concatenate([kv[..., :MLA_NOPE_DIM],
                         jnp.broadcast_to(k_r, (B, MLA_HEADS, S, MLA_ROPE_DIM))], axis=-1)
    o = causal_dense_attention(q, k, kv[..., MLA_NOPE_DIM:], (MLA_NOPE_DIM + MLA_ROPE_DIM) ** -0.5)
    return from_heads(o)


def by_residue(t, dil):
    B, H, S, d = t.shape
    return t.reshape(B, H, S // dil, dil, d).transpose(0, 1, 3, 2, 4).reshape(B, H * dil, S // dil, d)


def dilated_mixer(q, k, v, slopes):
    B, S, _ = q.shape
    qh, kh, vh = to_heads(q, DIL_HEADS), to_heads(k, DIL_HEADS), to_heads(v, DIL_HEADS)
    outs, lses = [], []
    for window, dil in DIL_PATTERNS:
        L = S // dil
        o, lse = banded_attention(by_residue(qh, dil)[:, :, None], by_residue(kh, dil), by_residue(vh, dil),
                                  window // dil, jnp.repeat(slopes, dil)[:, None], dil, HEAD_DIM ** -0.5)
        outs.append(o[:, :, 0].reshape(B, DIL_HEADS, dil, L, HEAD_DIM).transpose(0, 1, 3, 2, 4)
                    .reshape(B, DIL_HEADS, S, HEAD_DIM))
        lses.append(lse[:, :, 0].reshape(B, DIL_HEADS, dil, L).transpose(0, 1, 3, 2).reshape(B, DIL_HEADS, S))
    w = jax.nn.softmax(jnp.stack(lses), axis=0)
    o = jnp.sum(w[..., None].astype(qh.dtype) * jnp.stack(outs), axis=0)
    return from_heads(o)


def swiglu(t, w_gate, w_up, w_down):
    return (jax.nn.silu(t @ w_gate) * (t @ w_up)) @ w_down


def moe_swiglu(h, router, w_gate, w_up, w_down):
    B, S, D = h.shape
    t = h.reshape(B * S, D)
    logits = (t @ router).astype(jnp.float32)
    top_val, top_idx = lax.top_k(logits, TOP_K)
    gates = jax.nn.softmax(top_val, axis=-1)
    combine = jnp.sum(jax.nn.one_hot(top_idx, N_EXPERTS, dtype=jnp.float32) * gates[..., None], axis=1)
    out = jnp.zeros_like(t)
    for e in range(N_EXPERTS):
        out = out + combine[:, e:e + 1].astype(t.dtype) * swiglu(t, w_gate[e], w_up[e], w_down[e])
    return out.reshape(B, S, D)


def setup_inputs(seed: int = 0) -> dict:
    key = jax.random.key(seed)
    ks = jax.random.split(key, 24)
    f32 = jnp.float32

    def nrm(k, shape, fan_in):
        return jax.random.normal(k, shape, f32) * fan_in ** -0.5

    def gain(k, shape):
        return 1.0 + 0.02 * jax.random.normal(k, shape, f32)

    x = jax.random.normal(ks[0], (BATCH, SEQ, D_MODEL), f32)
    start = jax.random.randint(ks[1], (BATCH, 1), 0, 1024, dtype=jnp.int32)
    positions = start + jnp.arange(SEQ, dtype=jnp.int32)[None, :]
    return {
        "x": x,
        "positions": positions,
        "attn_norm": gain(ks[2], (DEPTH, D_MODEL)),
        "w_in": nrm(ks[3], (DEPTH, D_MODEL, N_IN), D_MODEL),
        "b_forget": FORGET_BIAS_MEAN + 0.1 * jax.random.normal(ks[4], (DEPTH, FOX_HEADS), f32),
        "mla_q_norm": gain(ks[5], (DEPTH, MLA_Q_LORA)),
        "w_q_up": nrm(ks[6], (DEPTH, MLA_Q_LORA, MLA_HEADS * (MLA_NOPE_DIM + MLA_ROPE_DIM)), MLA_Q_LORA),
        "mla_kv_norm": gain(ks[7], (DEPTH, MLA_KV_LORA)),
        "w_kv_up": nrm(ks[8], (DEPTH, MLA_KV_LORA, MLA_HEADS * (MLA_NOPE_DIM + MLA_V_DIM)), MLA_KV_LORA),
        "sinks": 0.5 * jax.random.normal(ks[9], (DEPTH, SWA_Q_HEADS), f32),
        "w_out": nrm(ks[10], (DEPTH, MIX_WIDTH, D_MODEL), MIX_WIDTH),
        "ffn_norm": gain(ks[11], (DEPTH, D_MODEL)),
        "dense_w_gate": nrm(ks[12], (N_DENSE, D_MODEL, FFN_DIM), D_MODEL),
        "dense_w_up": nrm(ks[13], (N_DENSE, D_MODEL, FFN_DIM), D_MODEL),
        "dense_w_down": nrm(ks[14], (N_DENSE, FFN_DIM, D_MODEL), FFN_DIM),
        "router": nrm(ks[15], (N_MOE, D_MODEL, N_EXPERTS), D_MODEL),
        "moe_w_gate": nrm(ks[16], (N_MOE, N_EXPERTS, D_MODEL, FFN_DIM), D_MODEL),
        "moe_w_up": nrm(ks[17], (N_MOE, N_EXPERTS, D_MODEL, FFN_DIM), D_MODEL),
        "moe_w_down": nrm(ks[18], (N_MOE, N_EXPERTS, FFN_DIM, D_MODEL), FFN_DIM),
        "final_norm": gain(ks[19], (D_MODEL,)),
    }


def reference(x, positions, attn_norm, w_in, b_forget, mla_q_norm, w_q_up, mla_kv_norm, w_kv_up, sinks,
              w_out, ffn_norm, dense_w_gate, dense_w_up, dense_w_down, router, moe_w_gate, moe_w_up,
              moe_w_down, final_norm):
    slopes = alibi_slopes()
    splits = np.cumsum(IN_SIZES)[:-1].tolist()
    for layer in range(DEPTH):
        h = rms_norm(x, attn_norm[layer])
        (fq, fk, fv, ff, sq, sk, sv, cq, ckv, kr, dq, dk, dv) = jnp.split(h @ w_in[layer], splits, axis=-1)
        y_a = fox_mixer(fq, fk, fv, ff, b_forget[layer])
        y_b = swa_sink_mixer(sq, sk, sv, sinks[layer], slopes[:SWA_Q_HEADS])
        y_c = mla_mixer(cq, ckv, kr, positions, mla_q_norm[layer], w_q_up[layer],
                        mla_kv_norm[layer], w_kv_up[layer])
        y_d = dilated_mixer(dq, dk, dv, slopes[SWA_Q_HEADS:])
        mixed = jnp.concatenate([y_a, y_b, y_c, y_d], axis=-1)
        x = x + mixed @ w_out[layer]
        h = rms_norm(x, ffn_norm[layer])
        j = layer // 2
        if layer % 2 == 0:
            x = x + swiglu(h, dense_w_gate[j], dense_w_up[j], dense_w_down[j])
        else:
            x = x + moe_swiglu(h, router[j], moe_w_gate[j], moe_w_up[j], moe_w_down[j])
    return rms_norm(x, final_norm)
```

```python
import contextlib
import os
import numpy as np
import ml_dtypes
import concourse.bass as bass
import concourse.mybir as mybir
from concourse.bass_utils import run_bass_kernel_spmd

F32 = mybir.dt.float32
BF16 = mybir.dt.bfloat16
I32 = mybir.dt.int32
AF = mybir.ActivationFunctionType
ALU = mybir.AluOpType
AX = mybir.AxisListType

D = 1024
S = 8192
NB = 2
NCORE = 8
TSH = 2048
FF = 3584
NFC = FF // 128
NE = 8
EPS = 1e-6
NCOL = 993
C_FQ, C_FK, C_FV, C_FF = 0, 64, 128, 192
C_SQ, C_SK, C_SV = 193, 257, 321
C_CQ = 385
C_DQ, C_DK, C_DV = 801, 865, 929
DIL = ((128, 1), (512, 4), (2048, 16))
PI = float(np.pi)
DBG2 = int(os.environ.get('K_DBG2', '0'))


class Res:
    __slots__ = ("name", "writer", "readers")

    def __init__(self, name):
        self.name = name
        self.writer = None
        self.readers = {}


class Prog:
    ENGS = ("pe", "act", "dve", "pool", "sp")

    def __init__(self, nc):
        self.nc = nc
        self.es = contextlib.ExitStack()
        self.streams = {e: [] for e in self.ENGS}
        self.count = {}
        self.sem = {}
        self.waited = {e: {} for e in self.ENGS}
        for e in self.ENGS:
            self.newsem("E_" + e)
        self.n_ops = 0
        self.nds = 0

    def newsem(self, name):
        self.sem[name] = self.es.enter_context(self.nc.semaphore(name))
        self.count[name] = 0
        return name

    def dsem(self):
        if getattr(self, "free_d", None):
            return self.free_d.pop()
        self.nds += 1
        return self.newsem(f"D{self.nds}")

    def barrier(self):
        toks = set((k, v) for k, v in self.count.items() if v > 0)
        for e in self.ENGS:
            self._waits(e, toks)
        self.free_d = sorted([k for k in self.count if k.startswith("D") and not k.startswith("DCC")], reverse=True)

    def sbuf(self, name, shape, dtype):
        return self.es.enter_context(self.nc.sbuf_tensor(name, list(shape), dtype))

    def psum(self, name, shape, dtype):
        return self.es.enter_context(self.nc.psum_tensor(name, list(shape), dtype))

    def _waits(self, eng, toks):
        for (s, v) in sorted(toks):
            if s == "E_pe" and eng == "pe":
                continue
            if self.waited[eng].get(s, 0) >= v:
                continue
            self.waited[eng][s] = v
            self.streams[eng].append(("wait", s, v))

    def _deps(self, reads, writes, own=None):
        toks = set()
        for r in reads:
            if r.writer:
                toks.add(r.writer)
        for w in writes:
            if w.writer and w.writer[0] != own:
                toks.add(w.writer)
            for t in w.readers.values():
                toks.add(t)
        return toks

    def op(self, eng, fn, reads=(), writes=(), signal=True):
        self._waits(eng, self._deps(reads, writes))
        s = "E_" + eng
        self.count[s] += 1
        tok = (s, self.count[s])
        self.streams[eng].append(("op", fn, s, self.count[s]))
        for r in reads:
            r.readers[s] = tok
        for w in writes:
            w.writer = tok
            w.readers = {}
        self.n_ops += 1

    def dma(self, q, fn, reads=(), writes=(), sem=None, inc=16):
        self._waits(q, self._deps(reads, writes, own=sem))
        self.count[sem] += inc
        tok = (sem, self.count[sem])
        self.streams[q].append(("op", fn, sem, inc))
        for r in reads:
            r.readers[sem] = tok
        for w in writes:
            w.writer = tok
            w.readers = {}
        self.n_ops += 1

    def wait_all(self, eng, resources):
        toks = set()
        for r in resources:
            if r.writer:
                toks.add(r.writer)
        self._waits(eng, toks)
        self._waits(eng, set((k, v) for k, v in self.count.items() if k.startswith("D") and v > 0))

    def emit(self):
        nc = self.nc
        streams = self.streams
        sem = self.sem

        import bisect
        needed = {}
        for lst in streams.values():
            for it in lst:
                if it[0] == "wait" and it[1].startswith("E_"):
                    needed.setdefault(it[1], set()).add(it[2])
        order = {k: sorted(v) for k, v in needed.items()}

        def phys(sname, v):
            return bisect.bisect_right(order[sname], v)

        def replay(e, lst):
            for it in lst:
                if it[0] == "wait":
                    if it[1].startswith("E_"):
                        e.wait_ge(sem[it[1]], phys(it[1], it[2]))
                    else:
                        e.wait_ge(sem[it[1]], it[2])
                else:
                    ins = it[1](e)
                    if it[2].startswith("E_"):
                        if it[3] in needed.get(it[2], ()):
                            ins.then_inc(sem[it[2]], 1)
                    else:
                        ins.then_inc(sem[it[2]], it[3])

        with nc.Block() as block:
            @block.tensor
            def _(e):
                replay(e, streams["pe"])

            @block.scalar
            def _(e):
                replay(e, streams["act"])

            @block.vector
            def _(e):
                replay(e, streams["dve"])

            @block.gpsimd
            def _(e):
                replay(e, streams["pool"])

            @block.sync
            def _(e):
                replay(e, streams["sp"])
        self.es.close()


ARENA_BYTES = 211968
_ESZ = {F32: 4, BF16: 2, I32: 4}


class Ctx:
    def __init__(self, nc):
        self.nc = nc
        self.P = Prog(nc)
        P = self.P
        self.pb = [P.psum(f"pb{i}", [128, 512], F32) for i in range(8)]
        self.rpb = [Res(f"pb{i}") for i in range(8)]
        self.psb = self.pb[7][:].bitcast(BF16)
        self.psbv = [self.pb[i][:].bitcast(BF16) for i in range(8)]
        self.nt = 0
        self.arena = P.sbuf("arena", [128, ARENA_BYTES // 2], BF16)
        self.off = 0
        self.uid = 0
        idf, r_idf = self.tile([128, 128], F32)
        self.ident, self.r_ident = self.tile([128, 128], BF16)
        P.op("pool", lambda e: e.memset(idf[:], 1.0), writes=[r_idf])
        P.op("pool", lambda e: e.affine_select(out=idf[:], in_=idf[:], pattern=[[-1, 128]], compare_op=ALU.is_equal,
                                               fill=0.0, base=0, channel_multiplier=1), reads=[r_idf], writes=[r_idf])
        P.op("dve", lambda e: e.tensor_copy(self.ident[:], idf[:]), reads=[r_idf], writes=[self.r_ident])
        self.junk, self.r_junk = self.tile([128, 1024], BF16)
        self.base = self.off

    def reset(self):
        self.off = self.base

    def tile(self, shape, dt, name=None):
        self.nt += 1
        nm = f"{name or 't'}{self.nt}"
        n = 1
        for d_ in shape[1:]:
            n *= d_
        nbytes = n * _ESZ[dt]
        off = (self.off + 63) // 64 * 64
        self.off = off + nbytes
        assert self.off <= ARENA_BYTES, f"SBUF arena overflow allocating {nm} {shape}: {self.off}"
        ap = self.arena[0:shape[0], off // 2:(off + nbytes) // 2]
        if dt != BF16:
            ap = ap.bitcast(dt)
        if len(shape) == 3:
            ap = ap.rearrange("p (a b) -> p a b", a=shape[1])
        elif len(shape) == 4:
            ap = ap.rearrange("p (a b c) -> p a b c", a=shape[1], b=shape[2])
        return ap, Res(nm)

    def ring(self, key, n, shape, dt, with_sem=False):
        bufs = []
        for i in range(n):
            t, r = self.tile(shape, dt, name=key)
            bufs.append((t, r, self.P.dsem()) if with_sem else (t, r))
        state = {"i": 0}

        def nxt():
            b = bufs[state["i"] % n]
            state["i"] += 1
            return b
        return nxt


def dram_in(nc, name, shape, dt):
    return nc.dram_tensor(name, list(shape), dt, kind="ExternalInput").ap()


def dram_out(nc, name, shape, dt):
    return nc.dram_tensor(name, list(shape), dt, kind="ExternalOutput").ap()


def emit_rstd(C, x_ap, r_x, ss, r_ss, n, width_ap=None):
    P = C.P
    P.op("dve", lambda e: e.memset(ss, 0.0), writes=[r_ss])
    P.op("act", lambda e: e.activation(out=C.junk[:, 0:n], in_=x_ap, func=AF.Square, accum_out=ss),
         reads=[r_x, r_ss], writes=[C.r_junk, r_ss])
    P.op("dve", lambda e: e.tensor_scalar(out=ss, in0=ss, scalar1=1.0 / n, scalar2=EPS, op0=ALU.mult, op1=ALU.add),
         reads=[r_ss], writes=[r_ss])
    P.op("act", lambda e: e.activation(out=ss, in_=ss, func=AF.Sqrt), reads=[r_ss], writes=[r_ss])
    P.op("dve", lambda e: e.reciprocal(ss, ss), reads=[r_ss], writes=[r_ss])


def emit_transposes_to(C, hb, r_hb, dst_ap, r_dst, nchunk, eng="act", bank=7):
    P = C.P
    psb = C.psbv[bank]
    for k in range(nchunk):
        P.op("pe", lambda e, k=k: e.transpose(psb[:, k * 128:(k + 1) * 128], hb[:, k * 128:(k + 1) * 128], C.ident[:]),
             reads=[r_hb, C.r_ident], writes=[C.rpb[bank]], signal=(k == nchunk - 1))
    src = psb[:, 0:nchunk * 128].rearrange("p (k t) -> p k t", k=nchunk)
    if eng == "act":
        P.op("act", lambda e: e.copy(dst_ap, src), reads=[C.rpb[bank]], writes=[r_dst])
    else:
        P.op("dve", lambda e: e.tensor_copy(dst_ap, src), reads=[C.rpb[bank]], writes=[r_dst])


def emit_C(C, T, mode, last):
    nc = C.nc
    P = C.P
    x_in = T["x"]
    g_next = T["g_next"]
    full = mode != "N"
    ne = NE if mode == "moe" else 1
    if full:
        mixT = T.get("mixT")
        w_out = T["w_out"]
        g_ffn = T["g_ffn"]
        wg, wu, wd = T["wg"], T["wu"], T["wd"]
        if mode == "moe":
            router = T["router"]
    if last:
        y_out = T["y"]
    else:
        x_out = T.get("x_out")
        hT_out = T["hT"]
    d_out = Res("d_out")
    x_blk = x_in.rearrange("(b p) d -> p b d", p=128)
    NTB = TSH // 128

    gbn, r_gbn = C.tile([128, D], F32)
    s_c = P.dsem()
    P.dma("sp", lambda e: e.dma_start(out=gbn[:], in_=g_next.partition_broadcast(128)), writes=[r_gbn], sem=P.dsem())
    ssr = C.ring("ss", 4, [128, 1], F32)
    hbr = C.ring("hb", 3, [128, D], BF16)
    stg = None if last else C.ring("stg", 2, [128, 8, 128], BF16, with_sem=True)
    hT_dram = None if last else hT_out.rearrange("(kc p) t -> p kc t", p=128)

    def emit_next(xa, r_xa, tb):
        ss, r_ss = ssr()
        emit_rstd(C, xa, r_xa, ss[:], r_ss, D)
        if last:
            yo, r_yo, s_yo = outr()
            P.op("dve", lambda e: e.scalar_tensor_tensor(out=yo[:], in0=xa, scalar=ss[:], in1=gbn[:], op0=ALU.mult,
                                                          op1=ALU.mult), reads=[r_xa, r_ss, r_gbn], writes=[r_yo])
            P.dma("sp", lambda e: e.dma_start(out=y_out[tb * 128:(tb + 1) * 128, :], in_=yo[:]), reads=[r_yo],
                  writes=[d_out], sem=s_yo)
        else:
            hb, r_hb = hbr()
            P.op("dve", lambda e: e.scalar_tensor_tensor(out=hb[:], in0=xa, scalar=ss[:], in1=gbn[:], op0=ALU.mult,
                                                          op1=ALU.mult), reads=[r_xa, r_ss, r_gbn], writes=[r_hb])
            st, r_st, s_st = stg()
            emit_transposes_to(C, hb, r_hb, st[:], r_st, 8)
            P.dma("sp", lambda e: e.dma_start(out=hT_dram[:, :, tb * 128:(tb + 1) * 128], in_=st[:]), reads=[r_st],
                  writes=[d_out], sem=s_st)

    if last:
        outr = C.ring("yo", 2, [128, D], F32, with_sem=True)

    if not full:
        xr = C.ring("xs", 3, [128, D], F32, with_sem=True)
        for tb in range(NTB):
            xt, r_xt, s_xt = xr()
            P.dma("sp", lambda e, tb=tb, xt=xt: e.dma_start(out=xt[:], in_=x_blk[:, tb, :]), writes=[r_xt], sem=s_xt)
            emit_next(xt[:], r_xt, tb)
            if x_out is not None:
                P.dma("sp", lambda e, tb=tb, xt=xt: e.dma_start(out=x_out[tb * 128:(tb + 1) * 128, :], in_=xt[:]),
                      reads=[r_xt], writes=[d_out], sem=s_xt)
        return

    C.uid += 1
    x1s = nc.dram_tensor(f"x1s{C.uid}", [TSH, D], F32).ap()
    r_x1s = [Res(f"x1s{i}") for i in range(NTB)]
    gbf, r_gbf = C.tile([128, D], F32)
    P.dma("sp", lambda e: e.dma_start(out=gbf[:], in_=g_ffn.partition_broadcast(128)), writes=[r_gbf], sem=P.dsem())
    wo, r_wo = C.tile([128, 8, D], BF16)
    s_wo = P.dsem()
    P.dma("pool", lambda e: e.dma_start(out=wo[:], in_=w_out.rearrange("(kc p) n -> p kc n", p=128)), writes=[r_wo],
          sem=s_wo)
    hT, _ = C.tile([128, 8, TSH], BF16)
    r_hT = [Res(f"hT{i}") for i in range(NTB)]
    if mode == "moe":
        rt_f, r_rtf = C.tile([128, 8, NE], F32)
        rt_hi, r_rthi = C.tile([128, 8, NE], BF16)
        rt_lo, r_rtlo = C.tile([128, 8, NE], BF16)
        P.dma("sp", lambda e: e.dma_start(out=rt_f[:], in_=router.rearrange("(kc p) n -> p kc n", p=128)),
              writes=[r_rtf], sem=P.dsem())
        P.op("dve", lambda e: e.tensor_copy(rt_hi[:], rt_f[:]), reads=[r_rtf], writes=[r_rthi])
        P.op("dve", lambda e: e.tensor_tensor(out=rt_lo[:], in0=rt_f[:], in1=rt_hi[:], op=ALU.subtract),
             reads=[r_rtf, r_rthi], writes=[r_rtlo])
        comb, _ = C.tile([128, NTB, NE], F32)
        r_comb = [Res(f"comb{i}") for i in range(NTB)]
        hfr = C.ring("hf", 1, [128, D], F32)
        hlr = C.ring("hl", 2, [128, D], BF16)
        lor = C.ring("loT", 1, [128, 8, 128], BF16)
        m8r = C.ring("m8", 2, [128, 8], F32)
        smr = C.ring("sm", 2, [128, 8], F32)

    xr = C.ring("xs", 2, [128, D], F32, with_sem=True)
    mxr = C.ring("mx", 2, [128, 8, 128], BF16, with_sem=True)
    mix_v = None if mixT is None else mixT.rearrange("(kc p) t -> p kc t", p=128)

    st1 = {}

    def stage1_a(tb):
        xt, r_xt, s_xt = xr()
        P.dma("sp", lambda e, tb=tb, xt=xt: e.dma_start(out=xt[:], in_=x_blk[:, tb, :]), writes=[r_xt], sem=s_xt)
        mx, r_mx, s_mx = mxr()
        if mix_v is not None:
            P.dma("sp", lambda e, tb=tb, mx=mx: e.dma_start(out=mx[:], in_=mix_v[:, :, tb * 128:(tb + 1) * 128]),
                  writes=[r_mx], sem=s_mx)
        else:
            def ld_mix(e, tb=tb, mx=mx):
                e.reg_add(T["reg_tmp"], T["reg_tok"], tb * 128)
                src = bass.AP(T["mix_all"], T["reg_tmp"], [[S, 128], [128 * S, 8], [1, 128]])
                return e.dma_start(out=mx[:], in_=src)
            P.dma("pool", ld_mix, reads=T["mix_reads"], writes=[r_mx], sem=s_mx)
        for hf_ in range(2):
            pbk = tb % 2 * 2 + hf_
            for kc in range(8):
                P.op("pe", lambda e, kc=kc, hf_=hf_, pbk=pbk, mx=mx: e.matmul(
                    C.pb[pbk][:, :], lhsT=mx[:, kc, :], rhs=wo[:, kc, hf_ * 512:(hf_ + 1) * 512],
                    start=(kc == 0), stop=(kc == 7)), reads=[r_mx, r_wo], writes=[C.rpb[pbk]], signal=(kc == 7))
            P.op("dve", lambda e, hf_=hf_, pbk=pbk, xt=xt: e.tensor_tensor(
                out=xt[:, hf_ * 512:(hf_ + 1) * 512], in0=xt[:, hf_ * 512:(hf_ + 1) * 512], in1=C.pb[pbk][:, :],
                op=ALU.add), reads=[r_xt, C.rpb[pbk]], writes=[r_xt])
        P.dma("sp", lambda e, tb=tb, xt=xt: e.dma_start(out=x1s[tb * 128:(tb + 1) * 128, :], in_=xt[:]),
              reads=[r_xt], writes=[r_x1s[tb]], sem=s_xt)
        ss, r_ss = ssr()
        emit_rstd(C, xt[:], r_xt, ss[:], r_ss, D)
        hb, r_hb = hbr()
        if mode != "moe":
            P.op("dve", lambda e, xt=xt, ss=ss, hb=hb: e.scalar_tensor_tensor(
                out=hb[:], in0=xt[:], scalar=ss[:], in1=gbf[:], op0=ALU.mult, op1=ALU.mult),
                reads=[r_xt, r_ss, r_gbf], writes=[r_hb])
            st1[tb] = (hb, r_hb, None, None)
        else:
            hf, r_hf = hfr()
            hl, r_hl = hlr()
            P.op("dve", lambda e, xt=xt, ss=ss, hf=hf: e.scalar_tensor_tensor(
                out=hf[:], in0=xt[:], scalar=ss[:], in1=gbf[:], op0=ALU.mult, op1=ALU.mult),
                reads=[r_xt, r_ss, r_gbf], writes=[r_hf])
            P.op("dve", lambda e, hf=hf, hb=hb: e.tensor_copy(hb[:], hf[:]), reads=[r_hf], writes=[r_hb])
            P.op("dve", lambda e, hf=hf, hb=hb, hl=hl: e.tensor_tensor(out=hl[:], in0=hf[:], in1=hb[:], op=ALU.subtract),
                 reads=[r_hf, r_hb], writes=[r_hl])
            st1[tb] = (hb, r_hb, hl, r_hl)

    def stage1_b(tb):
        hb, r_hb, hl, r_hl = st1.pop(tb)
        emit_transposes_to(C, hb, r_hb, hT[:, :, tb * 128:(tb + 1) * 128], r_hT[tb], 8)
        if mode == "moe":
            lo, r_lo = lor()
            emit_transposes_to(C, hl, r_hl, lo[:], r_lo, 8, eng="dve")
            n = 0
            for kc in range(8):
                for (a, ra, b_, rb) in ((hT[:, kc, tb * 128:(tb + 1) * 128], r_hT[tb], rt_hi, r_rthi),
                                        (lo[:, kc, :], r_lo, rt_hi, r_rthi),
                                        (hT[:, kc, tb * 128:(tb + 1) * 128], r_hT[tb], rt_lo, r_rtlo)):
                    P.op("pe", lambda e, a=a, b_=b_, kc=kc, n=n: e.matmul(
                        C.pb[4][:, 0:NE], lhsT=a, rhs=b_[:, kc, :], start=(n == 0), stop=(n == 23)),
                        reads=[ra, rb], writes=[C.rpb[4]], signal=(n == 23))
                    n += 1
            lg, r_lg = smr()
            m8, r_m8 = m8r()
            cb = comb[:, tb, :]
            P.op("act", lambda e, lg=lg: e.copy(lg[:], C.pb[4][:, 0:NE]), reads=[C.rpb[4]], writes=[r_lg])
            P.op("dve", lambda e, lg=lg, m8=m8: e.max(out=m8[:], in_=lg[:]), reads=[r_lg], writes=[r_m8])
            P.op("dve", lambda e, lg=lg, m8=m8, cb=cb: e.tensor_scalar(
                out=cb, in0=lg[:], scalar1=m8[:, 1:2], scalar2=None, op0=ALU.is_ge),
                reads=[r_lg, r_m8], writes=[r_comb[tb]])
            P.op("dve", lambda e, lg=lg, m8=m8: e.tensor_scalar(
                out=lg[:], in0=lg[:], scalar1=m8[:, 0:1], scalar2=None, op0=ALU.subtract),
                reads=[r_lg, r_m8], writes=[r_lg])
            P.op("act", lambda e, lg=lg: e.activation(out=lg[:], in_=lg[:], func=AF.Exp), reads=[r_lg], writes=[r_lg])
            P.op("dve", lambda e, lg=lg, cb=cb: e.tensor_tensor(out=cb, in0=cb, in1=lg[:], op=ALU.mult),
                 reads=[r_lg, r_comb[tb]], writes=[r_comb[tb]])
            P.op("dve", lambda e, m8=m8, cb=cb: e.tensor_reduce(out=m8[:, 2:3], in_=cb, axis=AX.X, op=ALU.add),
                 reads=[r_comb[tb], r_m8], writes=[r_m8])
            P.op("dve", lambda e, m8=m8: e.reciprocal(m8[:, 2:3], m8[:, 2:3]), reads=[r_m8], writes=[r_m8])
            P.op("dve", lambda e, m8=m8, cb=cb: e.tensor_scalar(
                out=cb, in0=cb, scalar1=m8[:, 2:3], scalar2=None, op0=ALU.mult),
                reads=[r_comb[tb], r_m8], writes=[r_comb[tb]])


    for i in range(NTB + 1):
        if i < NTB:
            stage1_a(i)
        if i >= 1:
            stage1_b(i - 1)

    HT = 1024
    NHB = HT // 128
    actT, _ = C.tile([128, NFC, HT], BF16)
    r_act = [[Res(f"act{f}_{s}") for s in range(HT // 512)] for f in range(NFC)]
    xacc, _ = C.tile([128, NHB, D], F32)
    r_xacc = [Res(f"xacc{i}") for i in range(NHB)]
    s_xa = [P.dsem() for _ in range(NHB)]
    wgr = C.ring("wg", 3, [128, 8, 128], BF16, with_sem=True)
    wur = C.ring("wu", 3, [128, 8, 128], BF16, with_sem=True)
    wdr = C.ring("wd", 3, [128, D], BF16, with_sem=True)
    sgr = C.ring("sg", 2, [128, 512], F32)
    for half in range(TSH // HT):
        for j in range(NHB):
            tb = half * NHB + j
            P.dma("sp", lambda e, tb=tb, j=j: e.dma_start(out=xacc[:, j, :], in_=x1s[tb * 128:(tb + 1) * 128, :]),
                  reads=[r_x1s[tb]], writes=[r_xacc[j]], sem=s_xa[j])
        for ex in range(0 if os.environ.get('K_SKIP2') else ne):
            wg_v = wg[ex].rearrange("(kc p) f -> p kc f", p=128)
            wu_v = wu[ex].rearrange("(kc p) f -> p kc f", p=128)
            for fc in range(NFC):
                g_t, r_g, s_g = wgr()
                u_t, r_u, s_u = wur()
                P.dma("pool", lambda e, fc=fc, g_t=g_t, wg_v=wg_v: e.dma_start(
                    out=g_t[:], in_=wg_v[:, :, fc * 128:(fc + 1) * 128]), writes=[r_g], sem=s_g)
                P.dma("pool", lambda e, fc=fc, u_t=u_t, wu_v=wu_v: e.dma_start(
                    out=u_t[:], in_=wu_v[:, :, fc * 128:(fc + 1) * 128]), writes=[r_u], sem=s_u)
                if DBG2 == 1:
                    continue
                for st_ in range(HT // 512):
                    t0 = half * HT + st_ * 512
                    rh = [r_hT[(t0 // 128) + i] for i in range(4)]
                    pg, pu = (0, 1) if (fc * 2 + st_) % 2 == 0 else (2, 3)
                    for kc in range(8):
                        P.op("pe", lambda e, kc=kc, g_t=g_t, t0=t0, pg=pg: e.matmul(
                            C.pb[pg][:, :], lhsT=g_t[:, kc, :], rhs=hT[:, kc, t0:t0 + 512], start=(kc == 0),
                            stop=(kc == 7)), reads=[r_g] + rh, writes=[C.rpb[pg]], signal=(kc == 7))
                    for kc in range(8):
                        P.op("pe", lambda e, kc=kc, u_t=u_t, t0=t0, pu=pu: e.matmul(
                            C.pb[pu][:, :], lhsT=u_t[:, kc, :], rhs=hT[:, kc, t0:t0 + 512], start=(kc == 0),
                            stop=(kc == 7)), reads=[r_u] + rh, writes=[C.rpb[pu]], signal=(kc == 7))
                    sg, r_sg = sgr()
                    P.op("act", lambda e, sg=sg, pg=pg: e.activation(out=sg[:], in_=C.pb[pg][:, :], func=AF.Silu),
                         reads=[C.rpb[pg]], writes=[r_sg])
                    P.op("dve", lambda e, sg=sg, pu=pu, fc=fc, st_=st_: e.tensor_tensor(
                        out=actT[:, fc, st_ * 512:(st_ + 1) * 512], in0=sg[:], in1=C.pb[pu][:, :], op=ALU.mult),
                        reads=[r_sg, C.rpb[pu]], writes=[r_act[fc][st_]])
            for jg in range(0 if DBG2 in (1, 2) else HT // 512):
                for fc in range(NFC):
                    d_t, r_d, s_d = wdr()
                    P.dma("pool", lambda e, fc=fc, d_t=d_t, ex=ex: e.dma_start(
                        out=d_t[:], in_=wd[ex, fc * 128:(fc + 1) * 128, :]), writes=[r_d], sem=s_d)
                    for jj in range(4):
                        j = jg * 4 + jj
                        for hf_ in range(2):
                            bk = jj * 2 + hf_
                            P.op("pe", lambda e, fc=fc, j=j, hf_=hf_, bk=bk, d_t=d_t: e.matmul(
                                C.pb[bk][:, :], lhsT=actT[:, fc, j * 128:(j + 1) * 128],
                                rhs=d_t[:, hf_ * 512:(hf_ + 1) * 512], start=(fc == 0), stop=(fc == NFC - 1)),
                                reads=[r_d, r_act[fc][jg]], writes=[C.rpb[bk]], signal=(fc == NFC - 1 or (jj == 3 and hf_ == 1)))
                for jj in range(4):
                    j = jg * 4 + jj
                    tb = half * NHB + j
                    for hf_ in range(2):
                        bk = jj * 2 + hf_
                        if mode == "moe":
                            P.op("dve", lambda e, j=j, hf_=hf_, bk=bk, tb=tb, ex=ex: e.scalar_tensor_tensor(
                                out=xacc[:, j, hf_ * 512:(hf_ + 1) * 512], in0=C.pb[bk][:, :],
                                scalar=comb[:, tb, ex:ex + 1], in1=xacc[:, j, hf_ * 512:(hf_ + 1) * 512],
                                op0=ALU.mult, op1=ALU.add), reads=[C.rpb[bk], r_comb[tb], r_xacc[j]],
                                writes=[r_xacc[j]])
                        else:
                            P.op("dve", lambda e, j=j, hf_=hf_, bk=bk: e.tensor_tensor(
                                out=xacc[:, j, hf_ * 512:(hf_ + 1) * 512], in0=xacc[:, j, hf_ * 512:(hf_ + 1) * 512],
                                in1=C.pb[bk][:, :], op=ALU.add), reads=[C.rpb[bk], r_xacc[j]], writes=[r_xacc[j]])
        for j in range(NHB):
            tb = half * NHB + j
            if not last and x_out is not None:
                P.dma("sp", lambda e, tb=tb, j=j: e.dma_start(out=x_out[tb * 128:(tb + 1) * 128, :], in_=xacc[:, j, :]),
                      reads=[r_xacc[j]], writes=[d_out], sem=s_xa[j])
            emit_next(xacc[:, j, :], r_xacc[j], tb)
    return


def build_C(mode, last):
    nc = bass.Bass("TRN2", target_bir_lowering=False)
    C = Ctx(nc)
    ne = NE if mode == "moe" else 1
    T = {"x": dram_in(nc, "x", [TSH, D], F32), "g_next": dram_in(nc, "g_next", [D], F32)}
    if mode != "N":
        T.update(mixT=dram_in(nc, "mixT", [D, TSH], BF16), w_out=dram_in(nc, "w_out", [D, D], F32),
                 g_ffn=dram_in(nc, "g_ffn", [D], F32), wg=dram_in(nc, "wg", [ne, D, FF], F32),
                 wu=dram_in(nc, "wu", [ne, D, FF], F32), wd=dram_in(nc, "wd", [ne, FF, D], F32))
        if mode == "moe":
            T["router"] = dram_in(nc, "router", [D, NE], F32)
    if last:
        T["y"] = dram_out(nc, "y", [TSH, D], F32)
    else:
        T["x_out"] = dram_out(nc, "x_out", [TSH, D], F32)
        T["hT"] = dram_out(nc, "hT", [D, TSH], BF16)
    emit_C(C, T, mode, last)
    C.P.wait_all("sp", [])
    C.P.emit()
    return nc


def emit_AB(C, T):
    nc = C.nc
    P = C.P
    hT_in = T["hT"]
    w_in, wq_up, wkv_up, qn, kvn = T["w_in"], T["wq_up"], T["wkv_up"], T["qn"], T["kvn"]
    scal, pos, cst, yT = T["scal"], T["pos"], T["cst"], T["yT"]
    C.uid += 1
    vscr = nc.dram_tensor(f"vscr{C.uid}", [S, 64], BF16).ap()
    d_out = Res("d_out")
    r_vscr = Res("vscr")
    NT = S // 512
    s_c = P.dsem()

    w_sb, r_w = C.tile([128, 8, NCOL], BF16)
    s_w = P.dsem()
    w_v = w_in.rearrange("(kc p) n -> p kc n", p=128)
    for kc in range(8):
        P.dma("pool", lambda e, kc=kc: e.dma_start(out=w_sb[:, kc, :], in_=w_v[:, kc, :]), writes=[r_w], sem=s_w)
    wq_f, r_wqf = C.tile([128, 2, 96], F32)
    wq_s, r_wq = C.tile([128, 2, 96], BF16)
    qn_t, r_qn = C.tile([128, 2], F32)
    wkv_f, r_wkvf = C.tile([128, 128], F32)
    wkv_s, r_wkv = C.tile([128, 128], BF16)
    kvn_t, r_kvn = C.tile([128, 1], F32)
    P.dma("sp", lambda e: e.dma_start(out=wq_f[:], in_=wq_up.rearrange("(c p) n -> p c n", p=128)), writes=[r_wqf], sem=P.dsem())
    for c in range(2):
        P.dma("sp", lambda e, c=c: e.dma_start(out=qn_t[:, c:c + 1], in_=qn[c * 128:(c + 1) * 128].rearrange("(p o) -> p o", o=1)),
              writes=[r_qn], sem=P.dsem())
    P.dma("sp", lambda e: e.dma_start(out=wkv_f[:], in_=wkv_up), writes=[r_wkvf], sem=P.dsem())
    P.dma("sp", lambda e: e.dma_start(out=kvn_t[:], in_=kvn.rearrange("(p o) -> p o", o=1)), writes=[r_kvn], sem=P.dsem())
    for c in range(2):
        P.op("dve", lambda e, c=c: e.tensor_scalar(out=wq_s[:, c, :], in0=wq_f[:, c, :], scalar1=qn_t[:, c:c + 1], scalar2=None,
                                                   op0=ALU.mult), reads=[r_wqf, r_qn], writes=[r_wq])
    P.op("dve", lambda e: e.tensor_scalar(out=wkv_s[:], in0=wkv_f[:], scalar1=kvn_t[:, 0:1], scalar2=None, op0=ALU.mult),
         reads=[r_wkvf, r_kvn], writes=[r_wkv])
    sc0, r_sc0 = C.tile([1, 8], F32)
    sc64, r_sc64 = C.tile([128, 8], F32)
    P.dma("sp", lambda e: e.dma_start(out=sc0[:], in_=scal), writes=[r_sc0], sem=P.dsem())
    P.dma("sp", lambda e: e.dma_start(out=sc64[64:65, :], in_=scal), writes=[r_sc64], sem=P.dsem())
    P.op("dve", lambda e: e.tensor_scalar(out=sc0[:, 0:1], in0=sc0[:, 0:1], scalar1=-1.0, scalar2=None, op0=ALU.mult),
         reads=[r_sc0], writes=[r_sc0])
    P.op("act", lambda e: e.activation(out=sc64[64:65, 2:3], in_=sc64[64:65, 1:2], func=AF.Exp), reads=[r_sc64],
         writes=[r_sc64])
    ones_f, r_ones = C.tile([128, 64], F32)
    P.op("dve", lambda e: e.memset(ones_f[:], 1.0), writes=[r_ones])
    cst_t, r_cst = C.tile([128, 32], F32)
    P.dma("sp", lambda e: e.dma_start(out=cst_t[:], in_=cst), writes=[r_cst], sem=P.dsem())
    pos_i, r_posi = C.tile([128, 64], I32)
    pos_f, r_posf = C.tile([128, 64], F32)
    P.dma("sp", lambda e: e.dma_start(out=pos_i[:], in_=pos), writes=[r_posi], sem=P.dsem())
    P.op("dve", lambda e: e.tensor_copy(pos_f[:], pos_i[:]), reads=[r_posi], writes=[r_posf])
    ang, r_ang = C.tile([128, 64 * 16], F32)
    sin_t, r_sin = C.tile([128, 64 * 16], F32)
    cos_t, r_cos = C.tile([128, 64 * 16], F32)
    tkf, r_tkf = C.tile([128, 64 * 16], F32)
    tki, r_tki = C.tile([128, 64 * 16], I32)
    tfx, r_tfx = tkf, r_tkf
    for blk in range(64):
        P.op("dve", lambda e, blk=blk: e.tensor_scalar(out=ang[:, blk * 16:(blk + 1) * 16], in0=cst_t[:, 0:16],
                                                       scalar1=pos_f[:, blk:blk + 1], scalar2=None, op0=ALU.mult),
             reads=[r_cst, r_posf], writes=[r_ang])

    def emit_sin(dst, r_dst, off):
        md = dst
        P.op("dve", lambda e: e.tensor_scalar(out=tkf[:], in0=ang[:], scalar1=off, scalar2=1.0 / (2 * PI), op0=ALU.add,
                                              op1=ALU.mult), reads=[r_ang], writes=[r_tkf])
        P.op("dve", lambda e: e.tensor_copy(tki[:], tkf[:]), reads=[r_tkf], writes=[r_tki])
        P.op("dve", lambda e: e.tensor_copy(tkf[:], tki[:]), reads=[r_tki], writes=[r_tkf])
        P.op("dve", lambda e: e.scalar_tensor_tensor(out=md[:], in0=tkf[:], scalar=-2 * PI, in1=ang[:], op0=ALU.mult,
                                                     op1=ALU.add), reads=[r_tkf, r_ang], writes=[r_dst])
        if off != 0.0:
            P.op("dve", lambda e: e.tensor_scalar(out=md[:], in0=md[:], scalar1=off, scalar2=None, op0=ALU.add),
                 reads=[r_dst], writes=[r_dst])
        P.op("dve", lambda e: e.tensor_scalar(out=tfx[:], in0=md[:], scalar1=PI, scalar2=-2 * PI, op0=ALU.is_gt,
                                              op1=ALU.mult), reads=[r_dst], writes=[r_tfx])
        P.op("dve", lambda e: e.tensor_tensor(out=md[:], in0=md[:], in1=tfx[:], op=ALU.add), reads=[r_dst, r_tfx],
             writes=[r_dst])
        P.op("dve", lambda e: e.tensor_scalar(out=tfx[:], in0=md[:], scalar1=-PI, scalar2=2 * PI, op0=ALU.is_lt,
                                              op1=ALU.mult), reads=[r_dst], writes=[r_tfx])
        P.op("dve", lambda e: e.tensor_tensor(out=md[:], in0=md[:], in1=tfx[:], op=ALU.add), reads=[r_dst, r_tfx],
             writes=[r_dst])
        P.op("act", lambda e: e.activation(out=md[:], in_=md[:], func=AF.Sin), reads=[r_dst], writes=[r_dst])

    emit_sin(sin_t, r_sin, 0.0)
    emit_sin(cos_t, r_cos, PI / 2)
    trif, r_trif = C.tile([128, 128], F32)
    tri, r_tri = C.tile([128, 128], BF16)
    P.op("pool", lambda e: e.memset(trif[:], 1.0), writes=[r_trif])
    P.op("pool", lambda e: e.affine_select(out=trif[:], in_=trif[:], pattern=[[1, 128]], compare_op=ALU.is_ge, fill=0.0,
                                           base=0, channel_multiplier=-1), reads=[r_trif], writes=[r_trif])
    P.op("dve", lambda e: e.tensor_copy(tri[:], trif[:]), reads=[r_trif], writes=[r_tri])
    jmp_i, r_jmpi = C.tile([128, 256], I32)
    jmp_f, r_jmpf = C.tile([128, 256], F32)
    P.op("pool", lambda e: e.iota(jmp_i[:], pattern=[[1, 256]], base=0, channel_multiplier=-1), writes=[r_jmpi])
    P.op("dve", lambda e: e.tensor_copy(jmp_f[:], jmp_i[:]), reads=[r_jmpi], writes=[r_jmpf])
    Mb, r_Mb = [], []
    for m in range(4):
        t, r = C.tile([128, 256], F32)
        span = 127 if m == 0 else 128
        P.op("act", lambda e, t=t, m=m: e.activation(out=t[:], in_=jmp_f[:], func=AF.Exp, scale=cst_t[:, 16 + m:17 + m]),
             reads=[r_jmpf, r_cst], writes=[r])
        P.op("pool", lambda e, t=t: e.affine_select(out=t[:], in_=t[:], pattern=[[1, 256]], compare_op=ALU.is_ge, fill=0.0,
                                                    base=0, channel_multiplier=-1), reads=[r], writes=[r])
        P.op("pool", lambda e, t=t, span=span: e.affine_select(out=t[:], in_=t[:], pattern=[[-1, 256]], compare_op=ALU.is_ge,
                                                               fill=0.0, base=span, channel_multiplier=1), reads=[r], writes=[r])
        Mb.append(t)
        r_Mb.append(r)

    QT, _ = C.tile([128, S], BF16)
    KT, _ = C.tile([128, S], BF16)
    r_QT = [Res(f"QT{i}") for i in range(NT)]
    r_KT = [Res(f"KT{i}") for i in range(NT)]
    V = []
    r_V = []
    for i in range(3):
        t, _ = C.tile([128, 64, 65], BF16)
        V.append(t)
        r_V.append([Res(f"V{i}_{j}") for j in range(NT)])
        P.op("pool", lambda e, t=t: e.memset(t[:, :, 64:65], 1.0), writes=r_V[i])
    ysb, _ = C.tile([64, S], BF16)
    r_ysb = [Res(f"ysb{i}") for i in range(NT)]
    s_y = [P.dsem() for _ in range(NT)]
    acc, _ = C.tile([65, S], F32)
    r_acc = [Res(f"acc{i}") for i in range(NT)]
    hring = C.ring("hT", 2, [128, 8, 512], BF16, with_sem=True)
    pring = C.ring("pt", 4, [128, 512], BF16)
    ering = C.ring("et", 2, [128, 256], F32)
    oring = C.ring("osb", 3, [128, 512], F32)

    def load_hT(tt):
        sh = (tt * 512) // TSH
        tl0 = (tt * 512) % TSH
        h, r_h, s_h = hring()
        if "hT_fn" in T:
            src = T["hT_fn"](sh, tl0)
        else:
            src = hT_in[sh].rearrange("(kc p) t -> p kc t", p=128)[:, :, tl0:tl0 + 512]
        P.dma("sp", lambda e: e.dma_start(out=h[:], in_=src), writes=[r_h], sem=s_h)
        return h, r_h

    prot = {"i": 0}
    PROJ_BANKS = [0, 1, 2, 3, 5, 6]

    def next_bank():
        b = PROJ_BANKS[prot["i"] % len(PROJ_BANKS)]
        prot["i"] += 1
        return b

    def proj_fm(h, r_h, col0, m, dst_ap, r_dst, scale, bank):
        bank = next_bank()
        for kc in range(8):
            P.op("pe", lambda e, kc=kc: e.matmul(C.pb[bank][0:m, :], lhsT=w_sb[:, kc, col0:col0 + m], rhs=h[:, kc, :],
                                                 start=(kc == 0), stop=(kc == 7)), reads=[r_w, r_h], writes=[C.rpb[bank]])
        P.op("act", lambda e: e.mul(dst_ap, C.pb[bank][0:m, :], scale), reads=[C.rpb[bank]], writes=[r_dst])

    def proj_v(h, r_h, col0, tt, vt, r_vt, bank):
        bank = next_bank()
        for sb in range(4):
            for kc in range(8):
                P.op("pe", lambda e, kc=kc, sb=sb: e.matmul(
                    C.pb[bank][:, sb * 64:(sb + 1) * 64], lhsT=h[:, kc, sb * 128:(sb + 1) * 128],
                    rhs=w_sb[:, kc, col0:col0 + 64], start=(kc == 0), stop=(kc == 7)),
                    reads=[r_w, r_h], writes=[C.rpb[bank]])
        P.op("dve", lambda e: e.tensor_copy(vt[:, tt * 4:(tt + 1) * 4, 0:64],
                                            C.pb[bank][:, 0:256].rearrange("p (b d) -> p b d", b=4)),
             reads=[C.rpb[bank]], writes=[r_vt[tt]])

    def finalize(ob, src_ap, r_src, tq, sink):
        osb, r_osb = oring()
        P.op("act", lambda e: e.copy(osb[0:65, :], src_ap), reads=r_src, writes=[r_osb])
        if sink:
            P.op("dve", lambda e: e.tensor_scalar(out=osb[64:65, :], in0=osb[64:65, :], scalar1=sc64[64:65, 2:3],
                                                  scalar2=None, op0=ALU.add), reads=[r_osb, r_sc64], writes=[r_osb])
        P.op("dve", lambda e: e.reciprocal(osb[64:65, :], osb[64:65, :]), reads=[r_osb], writes=[r_osb])
        P.op("pe", lambda e: e.matmul(C.pb[4][0:64, :], lhsT=ones_f[64:65, 0:64], rhs=osb[64:65, :], start=True,
                                      stop=True), reads=[r_ones, r_osb], writes=[C.rpb[4]])
        P.op("dve", lambda e: e.tensor_tensor(out=ysb[0:64, tq * 512:(tq + 1) * 512], in0=osb[0:64, :],
                                              in1=C.pb[4][0:64, :], op=ALU.mult), reads=[r_osb, C.rpb[4]],
             writes=[r_ysb[tq]])

    def store_y(mixer):
        r_st = Res(f"yst{mixer}")
        P.dma("sp", lambda e: e.dma_start(out=yT[mixer * 64:(mixer + 1) * 64, :], in_=ysb[0:64, :]),
              reads=r_ysb, writes=[d_out, r_st], sem=s_y[0])
        if "after_store" in T:
            T["after_store"](mixer, r_st)

    def attn_causal(kdim):
        for qt in range(NT):
            t0 = qt * 512
            nkb = (t0 + 512) // 128
            ob = 2 + qt % 2

            SB = (0, 1, 5, 6)
            LA = 3

            def s_mm(kb):
                o = max(0, kb * 128 - t0)
                sb_ = SB[kb % 4]
                P.op("pe", lambda e, t0=t0, o=o, kb=kb, sb_=sb_: e.matmul(
                    C.pb[sb_][:, o:512], lhsT=KT[0:kdim, kb * 128:(kb + 1) * 128],
                    rhs=QT[0:kdim, t0 + o:t0 + 512], start=True, stop=True),
                     reads=[r_KT[kb // 4], r_QT[qt]], writes=[C.rpb[sb_]])
            for kb in range(min(LA, nkb)):
                s_mm(kb)
            for kb in range(nkb):
                if kb + LA < nkb:
                    s_mm(kb + LA)
                o = max(0, kb * 128 - t0)
                sb_ = SB[kb % 4]
                pt, r_pt = pring()
                P.op("act", lambda e, o=o, pt=pt, sb_=sb_: e.activation(out=pt[:, o:512], in_=C.pb[sb_][:, o:512],
                                                                        func=AF.Exp), reads=[C.rpb[sb_]], writes=[r_pt])
                if kb * 128 >= t0:
                    P.op("pool", lambda e, o=o, pt=pt: e.tensor_tensor(out=pt[:, o:o + 128], in0=pt[:, o:o + 128],
                                                                       in1=tri[:], op=ALU.mult), reads=[r_pt, r_tri],
                         writes=[r_pt])
                P.op("pe", lambda e, o=o, pt=pt, kb=kb, ob=ob, nkb=nkb: e.matmul(
                    C.pb[ob][0:65, o:512], lhsT=V[0][:, kb, 0:65], rhs=pt[:, o:512], start=(kb == 0),
                    stop=(kb == nkb - 1), skip_group_check=True), reads=[r_V[0][kb // 4], r_pt], writes=[C.rpb[ob]])
            finalize(ob, C.pb[ob][0:65, :], [C.rpb[ob]], qt, False)

    cnt = [0]

    def attn_banded(dil, m, vt, r_vt, evac):
        L = S // dil
        for r in range(dil):
            for qt in range(L // 512):
                n0 = qt * 512
                ob = 2 + cnt[0] % 2
                cnt[0] += 1
                kbs = [kb for kb in range(n0 // 128 - 1, n0 // 128 + 4) if kb >= 0]
                tl_lo = (dil * n0) // 512
                tl_hi = min(NT - 1, (dil * (n0 + 511) + r) // 512)
                rq = [r_QT[i] for i in range(tl_lo, tl_hi + 1)]
                SBK = (0, 1, 5, 6, 7)
                geo = []
                for i, kb in enumerate(kbs):
                    qa = max(kb * 128, n0)
                    qb = min(kb * 128 + 256, n0 + 512)
                    jo, w, co = qa - kb * 128, qb - qa, qa - n0
                    kc0 = r + dil * kb * 128
                    kcols = slice(kc0, kc0 + dil * 127 + 1, dil)
                    qcols = slice(r + dil * qa, r + dil * (qb - 1) + 1, dil)
                    ktl = sorted(set([(kc0) // 512, min(NT - 1, (kc0 + dil * 127) // 512)]))
                    rk = [r_KT[j] for j in range(ktl[0], ktl[-1] + 1)]
                    sbk = SBK[i]
                    geo.append((jo, w, co, sbk))
                    P.op("pe", lambda e, w=w, kcols=kcols, qcols=qcols, sbk=sbk: e.matmul(
                        C.pb[sbk][:, 0:w], lhsT=KT[0:64, kcols], rhs=QT[0:64, qcols], start=True, stop=True),
                        reads=rk + rq, writes=[C.rpb[sbk]])
                for i, kb in enumerate(kbs):
                    jo, w, co, sbk = geo[i]
                    et, r_et = ering()
                    pt, r_pt = pring()
                    P.op("act", lambda e, w=w, et=et, sbk=sbk: e.activation(out=et[:, 0:w], in_=C.pb[sbk][:, 0:w],
                                                                            func=AF.Exp), reads=[C.rpb[sbk]], writes=[r_et])
                    P.op("dve", lambda e, w=w, et=et, pt=pt, jo=jo: e.tensor_tensor(
                        out=pt[:, 0:w], in0=et[:, 0:w], in1=Mb[m][:, jo:jo + w], op=ALU.mult),
                        reads=[r_et, r_Mb[m]], writes=[r_pt])
                    ch = r * (L // 128) + kb
                    P.op("pe", lambda e, w=w, co=co, pt=pt, ch=ch, i=i, ob=ob, nk=len(kbs): e.matmul(
                        C.pb[ob][0:65, co:co + w], lhsT=vt[:, ch, 0:65], rhs=pt[:, 0:w], start=(i == 0),
                        stop=(i == nk - 1), skip_group_check=True), reads=[r_vt[ch // 4], r_pt], writes=[C.rpb[ob]])
                evac(ob, r, n0, tl_lo, tl_hi)

    P.op("dve", lambda e: e.memset(QT[64:70, :], 1.0), writes=r_QT)
    P.op("dve", lambda e: e.memset(KT[64:70, :], 1.0), writes=r_KT)
    s_augq = [P.dsem() for _ in range(NT)]
    s_augk = [P.dsem() for _ in range(NT)]
    cumr = C.ring("cum", 2, [1, 512], F32)
    one1, r_one1 = C.tile([1, 512], F32)
    P.op("dve", lambda e: e.memset(one1[:], 1.0), writes=[r_one1])
    zero1, r_zero1 = C.tile([1, 1], F32)
    P.op("dve", lambda e: e.memset(zero1[:], 0.0), writes=[r_zero1])
    spr = C.ring("sp", 1, [1, 512], F32)
    augr = C.ring("aug", 1, [1, 6, 512], BF16)
    rr = C.ring("rr", 1, [1, 2, 512], F32)
    prev = (zero1[:, 0:1], r_zero1)
    for tt in range(NT):
        h, r_h = load_hT(tt)
        cols = slice(tt * 512, (tt + 1) * 512)
        proj_fm(h, r_h, C_FQ, 64, QT[0:64, cols], r_QT[tt], 0.125, 5)
        proj_fm(h, r_h, C_FK, 64, KT[0:64, cols], r_KT[tt], 1.0, 6)
        proj_v(h, r_h, C_FV, tt, V[0], r_V[0], 5)
        for kc in range(8):
            P.op("pe", lambda e, kc=kc, h=h: e.matmul(C.pb[4][0:1, :], lhsT=w_sb[:, kc, C_FF:C_FF + 1], rhs=h[:, kc, :],
                                                      start=(kc == 0), stop=(kc == 7)), reads=[r_w, r_h], writes=[C.rpb[4]])
        sp_, r_sp = spr()
        P.op("act", lambda e, sp_=sp_: e.activation(out=sp_[:], in_=C.pb[4][0:1, :], func=AF.Exp, scale=-1.0,
                                                    bias=sc0[0:1, 0:1]), reads=[C.rpb[4], r_sc0], writes=[r_sp])
        P.op("act", lambda e, sp_=sp_: e.activation(out=sp_[:], in_=sp_[:], func=AF.Ln, bias=1.0), reads=[r_sp],
             writes=[r_sp])
        cm, r_cm = cumr()
        pv_ap, r_pv = prev
        P.op("dve", lambda e, cm=cm, sp_=sp_, pv_ap=pv_ap: e.tensor_tensor_scan(
            out=cm[:], data0=one1[:], data1=sp_[:], initial=pv_ap, op0=ALU.mult, op1=ALU.add),
            reads=[r_one1, r_sp, r_pv], writes=[r_cm])
        prev = (cm[:, 511:512], r_cm)
        ag, r_ag = augr()
        rs_, r_rs = rr()
        P.op("dve", lambda e, ag=ag, cm=cm: e.tensor_copy(ag[:, 3, :], cm[:]), reads=[r_cm], writes=[r_ag])
        P.op("dve", lambda e, ag=ag, cm=cm, rs_=rs_: e.tensor_tensor(out=rs_[:, 0, :], in0=cm[:], in1=ag[:, 3, :],
                                                                     op=ALU.subtract), reads=[r_cm, r_ag], writes=[r_rs])
        P.op("dve", lambda e, ag=ag, rs_=rs_: e.tensor_copy(ag[:, 4, :], rs_[:, 0, :]), reads=[r_rs], writes=[r_ag])
        P.op("dve", lambda e, ag=ag, rs_=rs_: e.tensor_tensor(out=rs_[:, 1, :], in0=rs_[:, 0, :], in1=ag[:, 4, :],
                                                              op=ALU.subtract), reads=[r_rs, r_ag], writes=[r_rs])
        P.op("dve", lambda e, ag=ag, rs_=rs_: e.tensor_copy(ag[:, 5, :], rs_[:, 1, :]), reads=[r_rs], writes=[r_ag])
        P.op("dve", lambda e, ag=ag: e.tensor_scalar(out=ag[:, 0:3, :], in0=ag[:, 3:6, :], scalar1=-1.0, scalar2=None,
                                                     op0=ALU.mult), reads=[r_ag], writes=[r_ag])
        for i in range(3):
            P.dma("pool", lambda e, i=i, ag=ag, cols=cols: e.dma_start(out=QT[64 + i:65 + i, cols], in_=ag[0:1, i, :]),
                  reads=[r_ag], writes=[r_QT[tt]], sem=s_augq[tt])
            P.dma("pool", lambda e, i=i, ag=ag, cols=cols: e.dma_start(out=KT[67 + i:68 + i, cols], in_=ag[0:1, 3 + i, :]),
                  reads=[r_ag], writes=[r_KT[tt]], sem=s_augk[tt])
    attn_causal(70)
    store_y(0)

    for tt in range(NT):
        h, r_h = load_hT(tt)
        cols = slice(tt * 512, (tt + 1) * 512)
        proj_fm(h, r_h, C_SQ, 64, QT[0:64, cols], r_QT[tt], 0.125, 5)
        proj_fm(h, r_h, C_SK, 64, KT[0:64, cols], r_KT[tt], 1.0, 6)
        proj_v(h, r_h, C_SV, tt, V[0], r_V[0], 5)
    attn_banded(1, 0, V[0], r_V[0],
                lambda ob, r, n0, lo, hi: finalize(ob, C.pb[ob][0:65, :], [C.rpb[ob]], n0 // 512, True))
    store_y(1)

    ctr = C.ring("ctm", 3, [128, 384], BF16)
    cTr = C.ring("cT", 3, [128, 3, 128], BF16)
    ssr = C.ring("ss2", 3, [128, 2], F32)
    qkf = C.ring("qkf", 3, [128, 2, 32], F32)
    qkb = C.ring("qkb", 3, [128, 2, 96], BF16)
    rtmp = C.ring("rtmp", 4, [128, 4, 16], F32)
    mla_state = {}

    def mla_A(blk):
        tt, sb = blk // 4, blk % 4
        if sb == 0:
            mla_state["h"] = load_hT(tt)
        h, r_h = mla_state["h"]
        bT, bQ, bC, bF = (0, 1)[blk % 2], (2, 3)[blk % 2], (5, 6)[blk % 2], (7, 4)[blk % 2]
        for kc in range(8):
            P.op("pe", lambda e, kc=kc, sb=sb, h=h, bT=bT: e.matmul(
                C.pb[bT][:, 0:416], lhsT=h[:, kc, sb * 128:(sb + 1) * 128], rhs=w_sb[:, kc, C_CQ:C_CQ + 416],
                start=(kc == 0), stop=(kc == 7)), reads=[r_w, r_h], writes=[C.rpb[bT]])
        ss, r_ss = ssr()
        P.op("dve", lambda e, ss=ss: e.memset(ss[:], 0.0), writes=[r_ss])
        P.op("act", lambda e, ss=ss, bT=bT: e.activation(out=C.junk[:, 0:256], in_=C.pb[bT][:, 0:256], func=AF.Square,
                                                  scale=1.0 / 16, accum_out=ss[:, 0:1]),
             reads=[C.rpb[bT], r_ss], writes=[C.r_junk, r_ss])
        P.op("act", lambda e, ss=ss, bT=bT: e.activation(out=C.junk[:, 0:128], in_=C.pb[bT][:, 256:384], func=AF.Square,
                                                  scale=float(128 ** -0.5), accum_out=ss[:, 1:2]),
             reads=[C.rpb[bT], r_ss], writes=[C.r_junk, r_ss])
        P.op("dve", lambda e, ss=ss: e.tensor_scalar(out=ss[:], in0=ss[:], scalar1=EPS, scalar2=None, op0=ALU.add),
             reads=[r_ss], writes=[r_ss])
        P.op("act", lambda e, ss=ss: e.activation(out=ss[:], in_=ss[:], func=AF.Sqrt), reads=[r_ss], writes=[r_ss])
        P.op("dve", lambda e, ss=ss: e.reciprocal(ss[:], ss[:]), reads=[r_ss], writes=[r_ss])
        ct, r_ct = ctr()
        P.op("dve", lambda e, ct=ct, bT=bT: e.tensor_copy(ct[:], C.pb[bT][:, 0:384]), reads=[C.rpb[bT]], writes=[r_ct])
        qf, r_qf = qkf()
        P.op("act", lambda e, qf=qf, bT=bT: e.copy(qf[:, 1, :], C.pb[bT][:, 384:416]), reads=[C.rpb[bT]], writes=[r_qf])
        mla_state[blk] = dict(ss=ss, r_ss=r_ss, ct=ct, r_ct=r_ct, qf=qf, r_qf=r_qf, bQ=bQ, bC=bC, bF=bF)

    def mla_B(blk):
        tt = blk // 4
        st = mla_state[blk]
        ss, r_ss, ct, r_ct, qf, r_qf, bQ, bC, bF = (st[k] for k in ("ss", "r_ss", "ct", "r_ct", "qf", "r_qf", "bQ", "bC", "bF"))
        cT, r_cT = cTr()
        emit_transposes_to(C, ct, r_ct, cT[:], r_cT, 3, bank=bC)
        for c in range(2):
            P.op("pe", lambda e, c=c, cT=cT, bQ=bQ: e.matmul(C.pb[bQ][:, 0:96], lhsT=cT[:, c, :], rhs=wq_s[:, c, :],
                                                      start=(c == 0), stop=(c == 1)), reads=[r_cT, r_wq],
                 writes=[C.rpb[bQ]])
        P.op("pe", lambda e, cT=cT, bQ=bQ: e.matmul(C.pb[bQ][:, 128:256], lhsT=cT[:, 2, :], rhs=wkv_s[:], start=True,
                                             stop=True), reads=[r_cT, r_wkv], writes=[C.rpb[bQ]])
        qb_, r_qb = qkb()
        sq = float(96 ** -0.5)
        P.op("dve", lambda e, qb_=qb_, ss=ss, bQ=bQ: e.tensor_scalar(out=qb_[:, 0, 0:64], in0=C.pb[bQ][:, 0:64],
                                                              scalar1=ss[:, 0:1], scalar2=sq, op0=ALU.mult, op1=ALU.mult),
             reads=[C.rpb[bQ], r_ss], writes=[r_qb])
        P.op("dve", lambda e, qf=qf, ss=ss, bQ=bQ: e.tensor_scalar(out=qf[:, 0, :], in0=C.pb[bQ][:, 64:96],
                                                            scalar1=ss[:, 0:1], scalar2=sq, op0=ALU.mult, op1=ALU.mult),
             reads=[C.rpb[bQ], r_ss, r_qf], writes=[r_qf])
        P.op("dve", lambda e, qb_=qb_, ss=ss, bQ=bQ: e.tensor_scalar(out=qb_[:, 1, 0:64], in0=C.pb[bQ][:, 128:192],
                                                              scalar1=ss[:, 1:2], scalar2=None, op0=ALU.mult),
             reads=[C.rpb[bQ], r_ss, r_qb], writes=[r_qb])
        P.op("dve", lambda e, ss=ss, blk=blk, bQ=bQ: e.tensor_scalar(out=V[0][:, blk, 0:64], in0=C.pb[bQ][:, 192:256],
                                                              scalar1=ss[:, 1:2], scalar2=None, op0=ALU.mult),
             reads=[C.rpb[bQ], r_ss], writes=[r_V[0][tt]])
        cs = cos_t[:, blk * 16:(blk + 1) * 16]
        sn = sin_t[:, blk * 16:(blk + 1) * 16]
        for w_ in range(2):
            tm_, r_tm = rtmp()
            t1 = qf[:, w_, 0:16]
            t2 = qf[:, w_, 16:32]
            eng = "pool" if w_ == 0 else "dve"
            P.op(eng, lambda e, t1=t1, tm_=tm_, cs=cs: e.tensor_tensor(out=tm_[:, 0, :], in0=t1, in1=cs, op=ALU.mult),
                 reads=[r_qf, r_cos], writes=[r_tm])
            P.op(eng, lambda e, t2=t2, tm_=tm_, sn=sn: e.tensor_tensor(out=tm_[:, 1, :], in0=t2, in1=sn, op=ALU.mult),
                 reads=[r_qf, r_sin, r_tm], writes=[r_tm])
            P.op(eng, lambda e, t2=t2, tm_=tm_, cs=cs: e.tensor_tensor(out=tm_[:, 2, :], in0=t2, in1=cs, op=ALU.mult),
                 reads=[r_qf, r_cos, r_tm], writes=[r_tm])
            P.op(eng, lambda e, t1=t1, tm_=tm_, sn=sn: e.tensor_tensor(out=tm_[:, 3, :], in0=t1, in1=sn, op=ALU.mult),
                 reads=[r_qf, r_sin, r_tm], writes=[r_tm])
            P.op(eng, lambda e, w_=w_, tm_=tm_, qb_=qb_: e.tensor_tensor(out=qb_[:, w_, 64:80], in0=tm_[:, 0, :],
                                                                         in1=tm_[:, 1, :], op=ALU.subtract),
                 reads=[r_tm, r_qb], writes=[r_qb])
            P.op(eng, lambda e, w_=w_, tm_=tm_, qb_=qb_: e.tensor_tensor(out=qb_[:, w_, 80:96], in0=tm_[:, 2, :],
                                                                         in1=tm_[:, 3, :], op=ALU.add),
                 reads=[r_tm, r_qb], writes=[r_qb])
        st.update(qb_=qb_, r_qb=r_qb)

    def mla_C(blk):
        tt = blk // 4
        st = mla_state.pop(blk)
        qb_, r_qb, bF = st["qb_"], st["r_qb"], st["bF"]
        for w_, (dst, r_d) in enumerate(((QT, r_QT[tt]), (KT, r_KT[tt]))):
            P.op("pe", lambda e, w_=w_, qb_=qb_, bF=bF: e.transpose(C.psbv[bF][0:96, w_ * 128:(w_ + 1) * 128], qb_[:, w_, :],
                                                             C.ident[:]), reads=[r_qb, C.r_ident], writes=[C.rpb[bF]])
            P.op("act", lambda e, w_=w_, dst=dst, blk=blk, bF=bF: e.copy(dst[0:96, blk * 128:(blk + 1) * 128],
                                                                  C.psbv[bF][0:96, w_ * 128:(w_ + 1) * 128]),
                 reads=[C.rpb[bF]], writes=[r_d])

    for i in range(64 + 2):
        if i < 64:
            mla_A(i)
        if 0 <= i - 1 < 64:
            mla_B(i - 1)
        if 0 <= i - 2 < 64:
            mla_C(i - 2)
    attn_causal(96)
    store_y(2)

    for tt in range(NT):
        h, r_h = load_hT(tt)
        cols = slice(tt * 512, (tt + 1) * 512)
        proj_fm(h, r_h, C_DQ, 64, QT[0:64, cols], r_QT[tt], 0.125, 5)
        proj_fm(h, r_h, C_DK, 64, KT[0:64, cols], r_KT[tt], 1.0, 6)
        proj_v(h, r_h, C_DV, tt, V[0], r_V[0], 5)
    s_v = P.dsem()
    s_vp = {1: P.dsem(), 2: P.dsem()}
    P.dma("sp", lambda e: e.dma_start(out=vscr.rearrange("(b p) d -> p b d", p=128), in_=V[0][:, :, 0:64]),
          reads=r_V[0], writes=[r_vscr], sem=s_v)
    for pi, (win, dil) in enumerate(DIL[1:], start=1):
        L = S // dil
        src = vscr.rearrange("(cc i r) d -> i r cc d", i=128, r=dil)
        dstv = V[pi][:, :, 0:64].rearrange("p (r cc) d -> p r cc d", r=dil)
        for r in range(dil):
            P.dma("sp", lambda e, r=r, src=src, dstv=dstv: e.dma_start(out=dstv[:, r, :, :], in_=src[:, r, :, :]),
                  reads=[r_vscr], writes=r_V[pi], sem=s_vp[pi])

    def evac_dil(first):
        def f(ob, r, n0, lo, hi, first=first):
            raise NotImplementedError
        return f

    for pi, (win, dil) in enumerate(DIL):
        def evac(ob, r, n0, lo, hi, pi=pi, dil=dil):
            dst = acc[0:65, slice(r + dil * n0, r + dil * (n0 + 511) + 1, dil)]
            ra = [r_acc[i] for i in range(lo, hi + 1)]
            if pi == 0:
                P.op("act", lambda e: e.copy(dst, C.pb[ob][0:65, :]), reads=[C.rpb[ob]], writes=ra)
            else:
                P.op("dve", lambda e: e.tensor_tensor(out=dst, in0=dst, in1=C.pb[ob][0:65, :], op=ALU.add),
                     reads=[C.rpb[ob]] + ra, writes=ra)
        attn_banded(dil, 1 + pi, V[pi], r_V[pi], evac)
    for tq in range(NT):
        finalize(None, acc[0:65, tq * 512:(tq + 1) * 512], [r_acc[tq]], tq, False)
    store_y(3)
    return


def build_AB():
    nc = bass.Bass("TRN2", target_bir_lowering=False)
    C = Ctx(nc)
    T = {"hT": dram_in(nc, "hT", [4, D, TSH], BF16), "w_in": dram_in(nc, "w_in", [D, NCOL], F32),
         "wq_up": dram_in(nc, "wq_up", [256, 96], F32), "wkv_up": dram_in(nc, "wkv_up", [128, 128], F32),
         "qn": dram_in(nc, "qn", [256], F32), "kvn": dram_in(nc, "kvn", [128], F32),
         "scal": dram_in(nc, "scal", [1, 8], F32), "pos": dram_in(nc, "pos", [128, 64], I32),
         "cst": dram_in(nc, "cst", [128, 32], F32), "yT": dram_out(nc, "yT", [256, S], BF16)}
    emit_AB(C, T)
    C.P.wait_all("sp", [])
    C.P.emit()
    return nc


def ab_inputs_common(head):
    slopes = 2.0 ** (-8.0 * np.arange(1, 9, dtype=np.float64) / 8.0)
    c = np.zeros((128, 32), np.float32)
    c[:, 0:16] = (10000.0 ** (-np.arange(16, dtype=np.float32) / np.float32(16))).astype(np.float32)[None, :]
    c[:, 16] = -slopes[head]
    for pi, (win, dil) in enumerate(DIL):
        c[:, 17 + pi] = -slopes[4 + head] * dil
    return c


GROUPS = [[0, 1, 2, 3], [4, 5, 6, 7]]


def build_fused():
    nc = bass.Bass("TRN2", target_bir_lowering=False, num_devices=NCORE)
    C = Ctx(nc)
    P = C.P
    x = dram_in(nc, "x", [TSH, D], F32)
    idx = dram_in(nc, "idx", [1, 1], I32)
    pos = dram_in(nc, "pos", [128, 64], I32)
    cst = dram_in(nc, "cst", [128, 32], F32)
    g0 = dram_in(nc, "g0", [D], F32)
    L = []
    for l in range(2):
        L.append({"w_in": dram_in(nc, f"w_in{l}", [D, NCOL], F32), "wq_up": dram_in(nc, f"wq_up{l}", [256, 96], F32),
                  "wkv_up": dram_in(nc, f"wkv_up{l}", [128, 128], F32), "qn": dram_in(nc, f"qn{l}", [256], F32),
                  "kvn": dram_in(nc, f"kvn{l}", [128], F32), "scal": dram_in(nc, f"scal{l}", [1, 8], F32),
                  "w_out": dram_in(nc, f"w_out{l}", [D, D], F32), "g_ffn": dram_in(nc, f"g_ffn{l}", [D], F32),
                  "g_next": dram_in(nc, f"g_next{l}", [D], F32)})
    ffn = [{"wg": dram_in(nc, "wg0", [1, D, FF], F32), "wu": dram_in(nc, "wu0", [1, D, FF], F32),
            "wd": dram_in(nc, "wd0", [1, FF, D], F32)},
           {"wg": dram_in(nc, "wg1", [NE, D, FF], F32), "wu": dram_in(nc, "wu1", [NE, D, FF], F32),
            "wd": dram_in(nc, "wd1", [NE, FF, D], F32), "router": dram_in(nc, "router", [D, NE], F32)}]
    y = dram_out(nc, "y", [TSH, D], F32)
    hT_loc = nc.dram_tensor("hT_loc", [D, TSH], BF16)
    hT_all = nc.dram_tensor("hT_all", [4 * D, TSH], BF16)
    y_loc = nc.dram_tensor("y_loc", [256, S], BF16)
    y_all = nc.dram_tensor("y_all", [1024, S], BF16)
    x_mid = nc.dram_tensor("x_mid", [TSH, D], F32)
    mix_own = nc.dram_tensor("mix_own", [D, TSH], BF16)
    reg_tok = P.es.enter_context(nc.gpsimd.register("rtok"))
    reg_tmp = P.es.enter_context(nc.gpsimd.register("rtmp"))
    idx_sb, r_idx = C.tile([1, 1], I32)
    C.base = C.off
    P.dma("sp", lambda e: e.dma_start(out=idx_sb[:], in_=idx), writes=[r_idx], sem=P.dsem())

    def ld_idx(e):
        e.reg_load(reg_tok, idx_sb[0:1, 0:1])
        return e.reg_mul(reg_tok, reg_tok, TSH)
    P.op("pool", ld_idx, reads=[r_idx])
    dmy, r_dmy = C.tile([128, 16], F32)
    C.base = C.off
    P.op("pool", lambda e: e.memset(dmy[:], 0.0), writes=[r_dmy])
    FSTOP = int(os.environ.get("K_FSTOP", "99"))

    def finish():
        P.wait_all("sp", [])
        P.emit()
        return nc
    ncc = [0]

    ccs = [P.newsem("DCC1"), P.newsem("DCC2")]

    def allgather(src_t, dst_t, nrows, rc):
        P.barrier()
        r_cc = Res("cc")
        for i in range(nrows // rc):
            P.dma("pool", lambda e, i=i: e.collective_compute(
                "AllGather", ALU.bypass, replica_groups=GROUPS, ins=[src_t.ap()[i * rc:(i + 1) * rc, :]],
                outs=[dst_t.ap()[i * 4 * rc:(i + 1) * 4 * rc, :]]), writes=[r_cc], sem=ccs[i % 2], inc=1)
        P.barrier()
        C.reset()

    hT_view = hT_all.ap().rearrange("(kc s p) t -> s p kc t", kc=8, s=4)

    def hT_fn(sh, tl0):
        return hT_view[sh][:, :, tl0:tl0 + 512]

    emit_C(C, {"x": x, "g_next": g0, "hT": hT_loc.ap(), "x_out": None}, "N", False)
    if FSTOP == 0:
        return finish()
    allgather(hT_loc, hT_all, D, 128)
    if FSTOP == 1:
        return finish()
    for l in range(2):
        T = dict(L[l])
        r_ycc = Res("ycc")

        def after_store(mixer, r_st):
            for i in (2 * mixer, 2 * mixer + 1):
                P.dma("pool", lambda e, i=i: e.collective_compute(
                    "AllGather", ALU.bypass, replica_groups=GROUPS, ins=[y_loc.ap()[i * 32:(i + 1) * 32, :]],
                    outs=[y_all.ap()[i * 128:(i + 1) * 128, :]]), reads=[r_st], writes=[r_ycc], sem=ccs[i % 2], inc=1)
        T.update(hT=None, hT_fn=hT_fn, pos=pos, cst=cst, yT=y_loc.ap(), after_store=after_store)
        emit_AB(C, T)
        if FSTOP == 2:
            return finish()
        P.barrier()
        C.reset()
        if FSTOP == 3:
            return finish()
        T = dict(L[l])
        T.update(ffn[l])
        for half in range(2):
            def ld_own(e, half=half):
                e.reg_add(reg_tmp, reg_tok, half * 512 * S)
                src = bass.AP(y_all, reg_tmp, [[S, 512], [1, TSH]])
                return e.dma_start(out=mix_own.ap()[half * 512:(half + 1) * 512, :], in_=src)
            P.dma("pool", ld_own, sem=P.dsem())
        P.barrier()
        if FSTOP == 4:
            return finish()
        T.update(x=(x if l == 0 else x_mid.ap()), mixT=mix_own.ap())
        if l == 0:
            T.update(x_out=x_mid.ap(), hT=hT_loc.ap())
            emit_C(C, T, "dense", False)
            if FSTOP == 5:
                return finish()
            allgather(hT_loc, hT_all, D, 128)
        else:
            T["y"] = y
            emit_C(C, T, "moe", True)
    P.wait_all("sp", [])
    P.emit()
    return nc


_PROGS = {}


def _prog(key, fn):
    if key not in _PROGS:
        _PROGS[key] = fn()
    return _PROGS[key]


def _run(nc, maps):
    res = run_bass_kernel_spmd(nc, maps, core_ids=list(range(NCORE)))
    return res.results


def kernel_unfused(x, positions, attn_norm, w_in, b_forget, mla_q_norm, w_q_up, mla_kv_norm, w_kv_up, sinks, w_out, ffn_norm,
           dense_w_gate, dense_w_up, dense_w_down, router, moe_w_gate, moe_w_up, moe_w_down, final_norm):
    f32 = np.float32
    x = np.asarray(x, f32)
    cores = list(range(NCORE))
    bt = [(c // 4, c % 4) for c in cores]
    maps = [{"x": np.ascontiguousarray(x[b, j * TSH:(j + 1) * TSH]), "g_next": np.asarray(attn_norm[0], f32)}
            for (b, j) in bt]
    r = _run(_prog("N", lambda: build_C("N", False)), maps)
    x_cur = [rr["x_out"] for rr in r]
    hT = [rr["hT"] for rr in r]
    o_fq, o_fk, o_fv, o_ff, o_sq, o_sk, o_sv, o_cq, o_dq, o_dk, o_dv = 0, 256, 512, 768, 772, 1028, 1156, 1284, 1700, 1956, 2212
    pos = np.asarray(positions, np.int32)
    out = None
    for layer in range(2):
        wl = np.asarray(w_in[layer], f32)
        maps = []
        for (b, j) in bt:
            kv = j // 2
            cols = np.concatenate([
                np.arange(o_fq + j * 64, o_fq + (j + 1) * 64), np.arange(o_fk + j * 64, o_fk + (j + 1) * 64),
                np.arange(o_fv + j * 64, o_fv + (j + 1) * 64), np.arange(o_ff + j, o_ff + j + 1),
                np.arange(o_sq + j * 64, o_sq + (j + 1) * 64), np.arange(o_sk + kv * 64, o_sk + (kv + 1) * 64),
                np.arange(o_sv + kv * 64, o_sv + (kv + 1) * 64), np.arange(o_cq, o_cq + 416),
                np.arange(o_dq + j * 64, o_dq + (j + 1) * 64), np.arange(o_dk + j * 64, o_dk + (j + 1) * 64),
                np.arange(o_dv + j * 64, o_dv + (j + 1) * 64)])
            scal = np.zeros((1, 8), f32)
            scal[0, 0] = b_forget[layer][j]
            scal[0, 1] = sinks[layer][j]
            maps.append({
                "hT": np.ascontiguousarray(np.stack([hT[b * 4 + s] for s in range(4)], 0)),
                "w_in": np.ascontiguousarray(wl[:, cols]),
                "wq_up": np.ascontiguousarray(np.asarray(w_q_up[layer], f32)[:, j * 96:(j + 1) * 96]),
                "wkv_up": np.ascontiguousarray(np.asarray(w_kv_up[layer], f32)[:, j * 128:(j + 1) * 128]),
                "qn": np.asarray(mla_q_norm[layer], f32), "kvn": np.asarray(mla_kv_norm[layer], f32),
                "scal": scal, "pos": np.ascontiguousarray(pos[b].reshape(64, 128).T),
                "cst": ab_inputs_common(j)})
        r = _run(_prog("AB", build_AB), maps)
        yT = [rr["yT"] for rr in r]
        perm = np.array([m * 256 + j * 64 + d for j in range(4) for m in range(4) for d in range(64)])
        wo = np.ascontiguousarray(np.asarray(w_out[layer], f32)[perm])
        last = layer == 1
        maps = []
        for (b, j) in bt:
            mixT = np.ascontiguousarray(np.concatenate([yT[b * 4 + jj][:, j * TSH:(j + 1) * TSH] for jj in range(4)], 0))
            m = {"x": x_cur[b * 4 + j], "mixT": mixT, "w_out": wo, "g_ffn": np.asarray(ffn_norm[layer], f32),
                 "g_next": np.asarray(final_norm if last else attn_norm[layer + 1], f32)}
            if layer == 0:
                m.update(wg=np.asarray(dense_w_gate, f32), wu=np.asarray(dense_w_up, f32), wd=np.asarray(dense_w_down, f32))
            else:
                m.update(wg=np.asarray(moe_w_gate[0], f32), wu=np.asarray(moe_w_up[0], f32),
                         wd=np.asarray(moe_w_down[0], f32), router=np.asarray(router[0], f32))
            maps.append(m)
        if layer == 0:
            r = _run(_prog("Cd", lambda: build_C("dense", False)), maps)
            x_cur = [rr["x_out"] for rr in r]
            hT = [rr["hT"] for rr in r]
        else:
            r = _run(_prog("Cm", lambda: build_C("moe", True)), maps)
            out = np.zeros((NB, S, D), f32)
            for (b, j), rr in zip(bt, r):
                out[b, j * TSH:(j + 1) * TSH] = rr["y"]
    return out


def _core_cols(j):
    o_fq, o_fk, o_fv, o_ff, o_sq, o_sk, o_sv, o_cq, o_dq, o_dk, o_dv = 0, 256, 512, 768, 772, 1028, 1156, 1284, 1700, 1956, 2212
    kv = j // 2
    return np.concatenate([
        np.arange(o_fq + j * 64, o_fq + (j + 1) * 64), np.arange(o_fk + j * 64, o_fk + (j + 1) * 64),
        np.arange(o_fv + j * 64, o_fv + (j + 1) * 64), np.arange(o_ff + j, o_ff + j + 1),
        np.arange(o_sq + j * 64, o_sq + (j + 1) * 64), np.arange(o_sk + kv * 64, o_sk + (kv + 1) * 64),
        np.arange(o_sv + kv * 64, o_sv + (kv + 1) * 64), np.arange(o_cq, o_cq + 416),
        np.arange(o_dq + j * 64, o_dq + (j + 1) * 64), np.arange(o_dk + j * 64, o_dk + (j + 1) * 64),
        np.arange(o_dv + j * 64, o_dv + (j + 1) * 64)])


def kernel(x, positions, attn_norm, w_in, b_forget, mla_q_norm, w_q_up, mla_kv_norm, w_kv_up, sinks, w_out, ffn_norm,
           dense_w_gate, dense_w_up, dense_w_down, router, moe_w_gate, moe_w_up, moe_w_down, final_norm):
    f32 = np.float32
    x = np.asarray(x, f32)
    pos = np.asarray(positions, np.int32)
    perm = np.array([((c8 * 32 + r) // 64) * 256 + j * 64 + (c8 * 32 + r) % 64
                     for c8 in range(8) for j in range(4) for r in range(32)])
    wo = [np.ascontiguousarray(np.asarray(w_out[l], f32)[perm]) for l in range(2)]
    shared = {"g0": np.asarray(attn_norm[0], f32),
              "wg0": np.asarray(dense_w_gate, f32), "wu0": np.asarray(dense_w_up, f32), "wd0": np.asarray(dense_w_down, f32),
              "wg1": np.asarray(moe_w_gate[0], f32), "wu1": np.asarray(moe_w_up[0], f32), "wd1": np.asarray(moe_w_down[0], f32),
              "router": np.asarray(router[0], f32)}
    for l in range(2):
        shared[f"qn{l}"] = np.asarray(mla_q_norm[l], f32)
        shared[f"kvn{l}"] = np.asarray(mla_kv_norm[l], f32)
        shared[f"w_out{l}"] = wo[l]
        shared[f"g_ffn{l}"] = np.asarray(ffn_norm[l], f32)
        shared[f"g_next{l}"] = np.asarray(attn_norm[1] if l == 0 else final_norm, f32)
    maps = []
    for c in range(NCORE):
        b, j = c // 4, c % 4
        m = dict(shared)
        m["x"] = np.ascontiguousarray(x[b, j * TSH:(j + 1) * TSH])
        m["idx"] = np.array([[j]], np.int32)
        m["pos"] = np.ascontiguousarray(pos[b].reshape(64, 128).T)
        m["cst"] = ab_inputs_common(j)
        cols = _core_cols(j)
        for l in range(2):
            m[f"w_in{l}"] = np.ascontiguousarray(np.asarray(w_in[l], f32)[:, cols])
            m[f"wq_up{l}"] = np.ascontiguousarray(np.asarray(w_q_up[l], f32)[:, j * 96:(j + 1) * 96])
            m[f"wkv_up{l}"] = np.ascontiguousarray(np.asarray(w_kv_up[l], f32)[:, j * 128:(j + 1) * 128])
            sc = np.zeros((1, 8), f32)
            sc[0, 0] = b_forget[l][j]
            sc[0, 1] = sinks[l][j]
            m[f"scal{l}"] = sc
        maps.append(m)
    r = _run(_prog("fused", build_fused), maps)
    out = np.zeros((NB, S, D), f32)
    for c in range(NCORE):
        out[c // 4, (c % 4) * TSH:(c % 4 + 1) * TSH] = r[c]["y"]
    return out
```

```python
import contextlib
import os
import numpy as np
import ml_dtypes
import concourse.bass as bass
import concourse.mybir as mybir
from concourse.bass_utils import run_bass_kernel_spmd

F32 = mybir.dt.float32
BF16 = mybir.dt.bfloat16
I32 = mybir.dt.int32
AF = mybir.ActivationFunctionType
ALU = mybir.AluOpType
AX = mybir.AxisListType

D = 1024
S = 8192
NB = 2
NCORE = 8
TSH = 2048
FF = 3584
NFC = FF // 128
NE = 8
EPS = 1e-6
NCOL = 993
C_FQ, C_FK, C_FV, C_FF = 0, 64, 128, 192
C_SQ, C_SK, C_SV = 193, 257, 321
C_CQ = 385
C_DQ, C_DK, C_DV = 801, 865, 929
DIL = ((128, 1), (512, 4), (2048, 16))
PI = float(np.pi)
DBG2 = int(os.environ.get('K_DBG2', '0'))


class Res:
    __slots__ = ("name", "writer", "readers")

    def __init__(self, name):
        self.name = name
        self.writer = None
        self.readers = {}


class Prog:
    ENGS = ("pe", "act", "dve", "pool", "sp")

    def __init__(self, nc):
        self.nc = nc
        self.es = contextlib.ExitStack()
        self.streams = {e: [] for e in self.ENGS}
        self.count = {}
        self.sem = {}
        self.waited = {e: {} for e in self.ENGS}
        for e in self.ENGS:
            self.newsem("E_" + e)
        self.n_ops = 0
        self.nds = 0

    def newsem(self, name):
        self.sem[name] = self.es.enter_context(self.nc.semaphore(name))
        self.count[name] = 0
        return name

    def dsem(self):
        if getattr(self, "free_d", None):
            return self.free_d.pop()
        self.nds += 1
        return self.newsem(f"D{self.nds}")

    def cond_begin(self, flag_ap, r_flag):
        if not hasattr(self, "flag_regs"):
            nc = self.nc
            engs = {"pe": nc.tensor, "act": nc.scalar, "dve": nc.vector, "pool": nc.gpsimd, "sp": nc.sync}
            self.flag_regs = {k: self.es.enter_context(v.register(f"flag_{k}")) for k, v in engs.items()}
        self._cond_snap = {e: dict(self.waited[e]) for e in self.ENGS}
        for e in self.ENGS:
            if r_flag.writer:
                self._waits(e, {r_flag.writer})
            self.streams[e].append(("if", flag_ap))

    def cond_end(self):
        for e in self.ENGS:
            self.streams[e].append(("endif",))
            self.waited[e] = self._cond_snap[e]

    def barrier(self):
        toks = set((k, v) for k, v in self.count.items() if v > 0)
        for e in self.ENGS:
            self._waits(e, toks)
        self.free_d = sorted([k for k in self.count if k.startswith("D") and not k.startswith("DCC")], reverse=True)

    def sbuf(self, name, shape, dtype):
        return self.es.enter_context(self.nc.sbuf_tensor(name, list(shape), dtype))

    def psum(self, name, shape, dtype):
        return self.es.enter_context(self.nc.psum_tensor(name, list(shape), dtype))

    def _waits(self, eng, toks):
        for (s, v) in sorted(toks):
            if s == "E_pe" and eng == "pe":
                continue
            if self.waited[eng].get(s, 0) >= v:
                continue
            self.waited[eng][s] = v
            self.streams[eng].append(("wait", s, v))

    def _deps(self, reads, writes, own=None):
        toks = set()
        for r in reads:
            if r.writer:
                toks.add(r.writer)
        for w in writes:
            if w.writer and w.writer[0] != own:
                toks.add(w.writer)
            for t in w.readers.values():
                toks.add(t)
        return toks

    def op(self, eng, fn, reads=(), writes=(), signal=True):
        self._waits(eng, self._deps(reads, writes))
        s = "E_" + eng
        self.count[s] += 1
        tok = (s, self.count[s])
        self.streams[eng].append(("op", fn, s, self.count[s]))
        for r in reads:
            r.readers[s] = tok
        for w in writes:
            w.writer = tok
            w.readers = {}
        self.n_ops += 1

    def dma(self, q, fn, reads=(), writes=(), sem=None, inc=16):
        self._waits(q, self._deps(reads, writes, own=sem))
        self.count[sem] += inc
        tok = (sem, self.count[sem])
        self.streams[q].append(("op", fn, sem, inc, self.count[sem]))
        for r in reads:
            r.readers[sem] = tok
        for w in writes:
            w.writer = tok
            w.readers = {}
        self.n_ops += 1

    def wait_all(self, eng, resources):
        toks = set()
        for r in resources:
            if r.writer:
                toks.add(r.writer)
        self._waits(eng, toks)
        self._waits(eng, set((k, v) for k, v in self.count.items() if k.startswith("D") and v > 0))

    def emit(self):
        nc = self.nc
        streams = self.streams
        sem = self.sem

        import bisect
        needed = {}
        for lst in streams.values():
            for it in lst:
                if it[0] == "wait" and it[1].startswith("E_"):
                    needed.setdefault(it[1], set()).add(it[2])
        order = {k: sorted(v) for k, v in needed.items()}

        def phys(sname, v):
            return bisect.bisect_right(order[sname], v)

        def run_items(e, ename, lst):
            i = 0
            while i < len(lst):
                it = lst[i]
                if it[0] == "wait":
                    if it[1].startswith("E_"):
                        e.wait_ge(sem[it[1]], phys(it[1], it[2]))
                    else:
                        e.wait_ge(sem[it[1]], it[2])
                elif it[0] == "if":
                    depth, j = 1, i + 1
                    while depth:
                        if lst[j][0] == "if":
                            depth += 1
                        elif lst[j][0] == "endif":
                            depth -= 1
                        j += 1
                    body = lst[i + 1:j - 1]
                    incs = {}
                    before = {}
                    for b in body:
                        if b[0] == "op":
                            if b[2].startswith("E_"):
                                if b[3] in needed.get(b[2], ()):
                                    incs[b[2]] = incs.get(b[2], 0) + 1
                                    if b[2] not in before:
                                        before[b[2]] = bisect.bisect_left(order[b[2]], b[3])
                            else:
                                incs[b[2]] = incs.get(b[2], 0) + b[3]
                                if b[2] not in before:
                                    before[b[2]] = b[4] - b[3]
                    if any(b[0] == "op" for b in body):
                        reg = self.flag_regs[ename]
                        e.reg_load(reg, it[1])
                        with e.If(reg):
                            run_items(e, ename, body)
                        if incs:
                            with e.Else():
                                for k in sorted(incs):
                                    if before[k] > 0:
                                        e.wait_ge(sem[k], before[k])
                                for k in sorted(incs):
                                    e.sem_inc(sem[k], incs[k])
                    i = j - 1
                elif it[0] == "op":
                    ins = it[1](e)
                    if it[2].startswith("E_"):
                        if it[3] in needed.get(it[2], ()):
                            ins.then_inc(sem[it[2]], 1)
                    else:
                        ins.then_inc(sem[it[2]], it[3])
                i += 1

        def replay(e, lst):
            run_items(e, self._cur, lst)

        with nc.Block() as block:
            @block.tensor
            def _(e):
                self._cur = "pe"
                replay(e, streams["pe"])

            @block.scalar
            def _(e):
                self._cur = "act"
                replay(e, streams["act"])

            @block.vector
            def _(e):
                self._cur = "dve"
                replay(e, streams["dve"])

            @block.gpsimd
            def _(e):
                self._cur = "pool"
                replay(e, streams["pool"])

            @block.sync
            def _(e):
                self._cur = "sp"
                replay(e, streams["sp"])
        self.es.close()


ARENA_BYTES = 211968
_ESZ = {F32: 4, BF16: 2, I32: 4}


class Ctx:
    def __init__(self, nc):
        self.nc = nc
        self.P = Prog(nc)
        P = self.P
        self.pb = [P.psum(f"pb{i}", [128, 512], F32) for i in range(8)]
        self.rpb = [Res(f"pb{i}") for i in range(8)]
        self.psb = self.pb[7][:].bitcast(BF16)
        self.psbv = [self.pb[i][:].bitcast(BF16) for i in range(8)]
        self.nt = 0
        self.arena = P.sbuf("arena", [128, ARENA_BYTES // 2], BF16)
        self.off = 0
        self.uid = 0
        idf, r_idf = self.tile([128, 128], F32)
        self.ident, self.r_ident = self.tile([128, 128], BF16)
        P.op("pool", lambda e: e.memset(idf[:], 1.0), writes=[r_idf])
        P.op("pool", lambda e: e.affine_select(out=idf[:], in_=idf[:], pattern=[[-1, 128]], compare_op=ALU.is_equal,
                                               fill=0.0, base=0, channel_multiplier=1), reads=[r_idf], writes=[r_idf])
        P.op("dve", lambda e: e.tensor_copy(self.ident[:], idf[:]), reads=[r_idf], writes=[self.r_ident])
        self.junk, self.r_junk = self.tile([128, 1024], BF16)
        self.base = self.off

    def reset(self):
        self.off = self.base

    def tile(self, shape, dt, name=None):
        self.nt += 1
        nm = f"{name or 't'}{self.nt}"
        n = 1
        for d_ in shape[1:]:
            n *= d_
        nbytes = n * _ESZ[dt]
        off = (self.off + 63) // 64 * 64
        self.off = off + nbytes
        assert self.off <= ARENA_BYTES, f"SBUF arena overflow allocating {nm} {shape}: {self.off}"
        ap = self.arena[0:shape[0], off // 2:(off + nbytes) // 2]
        if dt != BF16:
            ap = ap.bitcast(dt)
        if len(shape) == 3:
            ap = ap.rearrange("p (a b) -> p a b", a=shape[1])
        elif len(shape) == 4:
            ap = ap.rearrange("p (a b c) -> p a b c", a=shape[1], b=shape[2])
        return ap, Res(nm)

    def ring(self, key, n, shape, dt, with_sem=False):
        bufs = []
        for i in range(n):
            t, r = self.tile(shape, dt, name=key)
            bufs.append((t, r, self.P.dsem()) if with_sem else (t, r))
        state = {"i": 0}

        def nxt():
            b = bufs[state["i"] % n]
            state["i"] += 1
            return b
        return nxt


def dram_in(nc, name, shape, dt):
    return nc.dram_tensor(name, list(shape), dt, kind="ExternalInput").ap()


def dram_out(nc, name, shape, dt):
    return nc.dram_tensor(name, list(shape), dt, kind="ExternalOutput").ap()


def emit_rstd(C, x_ap, r_x, ss, r_ss, n, width_ap=None):
    P = C.P
    P.op("dve", lambda e: e.memset(ss, 0.0), writes=[r_ss])
    P.op("act", lambda e: e.activation(out=C.junk[:, 0:n], in_=x_ap, func=AF.Square, accum_out=ss),
         reads=[r_x, r_ss], writes=[C.r_junk, r_ss])
    P.op("dve", lambda e: e.tensor_scalar(out=ss, in0=ss, scalar1=1.0 / n, scalar2=EPS, op0=ALU.mult, op1=ALU.add),
         reads=[r_ss], writes=[r_ss])
    P.op("act", lambda e: e.activation(out=ss, in_=ss, func=AF.Sqrt), reads=[r_ss], writes=[r_ss])
    P.op("dve", lambda e: e.reciprocal(ss, ss), reads=[r_ss], writes=[r_ss])


def emit_transposes_to(C, hb, r_hb, dst_ap, r_dst, nchunk, eng="act", bank=7):
    P = C.P
    psb = C.psbv[bank]
    for k in range(nchunk):
        P.op("pe", lambda e, k=k: e.transpose(psb[:, k * 128:(k + 1) * 128], hb[:, k * 128:(k + 1) * 128], C.ident[:]),
             reads=[r_hb, C.r_ident], writes=[C.rpb[bank]], signal=(k == nchunk - 1))
    src = psb[:, 0:nchunk * 128].rearrange("p (k t) -> p k t", k=nchunk)
    if eng == "act":
        P.op("act", lambda e: e.copy(dst_ap, src), reads=[C.rpb[bank]], writes=[r_dst])
    else:
        P.op("dve", lambda e: e.tensor_copy(dst_ap, src), reads=[C.rpb[bank]], writes=[r_dst])


def emit_C(C, T, mode, last):
    nc = C.nc
    P = C.P
    x_in = T["x"]
    g_next = T["g_next"]
    full = mode != "N"
    sparse = mode == "moes"
    if sparse:
        mode = "moe"
    ne = NE if mode == "moe" else 1
    if full:
        mixT = T.get("mixT")
        w_out = T["w_out"]
        g_ffn = T["g_ffn"]
        wg, wu, wd = T["wg"], T["wu"], T["wd"]
        if mode == "moe":
            router = T["router"]
    if last:
        y_out = T["y"]
    else:
        x_out = T.get("x_out")
        hT_out = T["hT"]
    d_out = Res("d_out")
    x_blk = x_in.rearrange("(b p) d -> p b d", p=128)
    NTB = TSH // 128

    gbn, r_gbn = C.tile([128, D], F32)
    s_c = P.dsem()
    P.dma("sp", lambda e: e.dma_start(out=gbn[:], in_=g_next.partition_broadcast(128)), writes=[r_gbn], sem=P.dsem())
    ssr = C.ring("ss", 4, [128, 1], F32)
    hbr = C.ring("hb", 3, [128, D], BF16)
    stg = None if last else C.ring("stg", 2, [128, 8, 128], BF16, with_sem=True)
    hT_dram = None if last else hT_out.rearrange("(kc p) t -> p kc t", p=128)

    def emit_next(xa, r_xa, tb):
        ss, r_ss = ssr()
        emit_rstd(C, xa, r_xa, ss[:], r_ss, D)
        if last:
            yo, r_yo, s_yo = outr()
            P.op("dve", lambda e: e.scalar_tensor_tensor(out=yo[:], in0=xa, scalar=ss[:], in1=gbn[:], op0=ALU.mult,
                                                          op1=ALU.mult), reads=[r_xa, r_ss, r_gbn], writes=[r_yo])
            P.dma("sp", lambda e: e.dma_start(out=y_out[tb * 128:(tb + 1) * 128, :], in_=yo[:]), reads=[r_yo],
                  writes=[d_out], sem=s_yo)
        else:
            hb, r_hb = hbr()
            P.op("dve", lambda e: e.scalar_tensor_tensor(out=hb[:], in0=xa, scalar=ss[:], in1=gbn[:], op0=ALU.mult,
                                                          op1=ALU.mult), reads=[r_xa, r_ss, r_gbn], writes=[r_hb])
            st, r_st, s_st = stg()
            emit_transposes_to(C, hb, r_hb, st[:], r_st, 8)
            P.dma("sp", lambda e: e.dma_start(out=hT_dram[:, :, tb * 128:(tb + 1) * 128], in_=st[:]), reads=[r_st],
                  writes=[d_out], sem=s_st)

    if last:
        outr = C.ring("yo", 2, [128, D], F32, with_sem=True)

    if not full:
        xr = C.ring("xs", 3, [128, D], F32, with_sem=True)
        for tb in range(NTB):
            xt, r_xt, s_xt = xr()
            P.dma("sp", lambda e, tb=tb, xt=xt: e.dma_start(out=xt[:], in_=x_blk[:, tb, :]), writes=[r_xt], sem=s_xt)
            emit_next(xt[:], r_xt, tb)
            if x_out is not None:
                P.dma("sp", lambda e, tb=tb, xt=xt: e.dma_start(out=x_out[tb * 128:(tb + 1) * 128, :], in_=xt[:]),
                      reads=[r_xt], writes=[d_out], sem=s_xt)
        return

    C.uid += 1
    x1s = nc.dram_tensor(f"x1s{C.uid}", [TSH, D], F32).ap()
    r_x1s = [Res(f"x1s{i}") for i in range(NTB)]
    if sparse:
        h_tm, _ = C.tile([128, NTB, D], BF16)
        r_htm = [Res(f"htm{i}") for i in range(NTB)]
        comb, _ = C.tile([128, NTB, NE], F32)
        r_comb = [Res(f"comb{i}") for i in range(NTB)]
        rkm, r_rkm = C.tile([128, NTB, NE], F32)
        chh, r_chh = C.tile([128, NTB, NE], BF16)
        cll, r_cll = C.tile([128, NTB, NE], BF16)
        io_f, r_iof = C.tile([128, 128], F32)
        flags_i, r_flags = C.tile([1, 32], I32)
        C.mark = C.off
    gbf, r_gbf = C.tile([128, D], F32)
    P.dma("sp", lambda e: e.dma_start(out=gbf[:], in_=g_ffn.partition_broadcast(128)), writes=[r_gbf], sem=P.dsem())
    wo, r_wo = C.tile([128, 8, D], BF16)
    s_wo = P.dsem()
    P.dma("pool", lambda e: e.dma_start(out=wo[:], in_=w_out.rearrange("(kc p) n -> p kc n", p=128)), writes=[r_wo],
          sem=s_wo)
    if not sparse:
        hT, _ = C.tile([128, 8, TSH], BF16)
        r_hT = [Res(f"hT{i}") for i in range(NTB)]
    else:
        hir = C.ring("hiT", 2, [128, 8, 128], BF16)
    if mode == "moe":
        rt_f, r_rtf = C.tile([128, 8, NE], F32)
        rt_hi, r_rthi = C.tile([128, 8, NE], BF16)
        rt_lo, r_rtlo = C.tile([128, 8, NE], BF16)
        P.dma("sp", lambda e: e.dma_start(out=rt_f[:], in_=router.rearrange("(kc p) n -> p kc n", p=128)),
              writes=[r_rtf], sem=P.dsem())
        P.op("dve", lambda e: e.tensor_copy(rt_hi[:], rt_f[:]), reads=[r_rtf], writes=[r_rthi])
        P.op("dve", lambda e: e.tensor_tensor(out=rt_lo[:], in0=rt_f[:], in1=rt_hi[:], op=ALU.subtract),
             reads=[r_rtf, r_rthi], writes=[r_rtlo])
        if not sparse:
            comb, _ = C.tile([128, NTB, NE], F32)
            r_comb = [Res(f"comb{i}") for i in range(NTB)]
        hfr = C.ring("hf", 1, [128, D], F32)
        hlr = C.ring("hl", 2, [128, D], BF16)
        lor = C.ring("loT", 1, [128, 8, 128], BF16)
        m8r = C.ring("m8", 2, [128, 8], F32)
        smr = C.ring("sm", 2, [128, 8], F32)

    xr = C.ring("xs", 2, [128, D], F32, with_sem=True)
    mxr = C.ring("mx", 2, [128, 8, 128], BF16, with_sem=True)
    mix_v = None if mixT is None else mixT.rearrange("(kc p) t -> p kc t", p=128)

    st1 = {}

    def stage1_a(tb):
        xt, r_xt, s_xt = xr()
        P.dma("sp", lambda e, tb=tb, xt=xt: e.dma_start(out=xt[:], in_=x_blk[:, tb, :]), writes=[r_xt], sem=s_xt)
        mx, r_mx, s_mx = mxr()
        if mix_v is not None:
            P.dma("sp", lambda e, tb=tb, mx=mx: e.dma_start(out=mx[:], in_=mix_v[:, :, tb * 128:(tb + 1) * 128]),
                  writes=[r_mx], sem=s_mx)
        else:
            def ld_mix(e, tb=tb, mx=mx):
                e.reg_add(T["reg_tmp"], T["reg_tok"], tb * 128)
                src = bass.AP(T["mix_all"], T["reg_tmp"], [[S, 128], [128 * S, 8], [1, 128]])
                return e.dma_start(out=mx[:], in_=src)
            P.dma("pool", ld_mix, reads=T["mix_reads"], writes=[r_mx], sem=s_mx)
        for hf_ in range(2):
            pbk = tb % 2 * 2 + hf_
            for kc in range(8):
                P.op("pe", lambda e, kc=kc, hf_=hf_, pbk=pbk, mx=mx: e.matmul(
                    C.pb[pbk][:, :], lhsT=mx[:, kc, :], rhs=wo[:, kc, hf_ * 512:(hf_ + 1) * 512],
                    start=(kc == 0), stop=(kc == 7)), reads=[r_mx, r_wo], writes=[C.rpb[pbk]], signal=(kc == 7))
            P.op("dve", lambda e, hf_=hf_, pbk=pbk, xt=xt: e.tensor_tensor(
                out=xt[:, hf_ * 512:(hf_ + 1) * 512], in0=xt[:, hf_ * 512:(hf_ + 1) * 512], in1=C.pb[pbk][:, :],
                op=ALU.add), reads=[r_xt, C.rpb[pbk]], writes=[r_xt])
        P.dma("sp", lambda e, tb=tb, xt=xt: e.dma_start(out=x1s[tb * 128:(tb + 1) * 128, :], in_=xt[:]),
              reads=[r_xt], writes=[r_x1s[tb]], sem=s_xt)
        ss, r_ss = ssr()
        emit_rstd(C, xt[:], r_xt, ss[:], r_ss, D)
        hb, r_hb = hbr() if not sparse else (h_tm[:, tb, :], r_htm[tb])
        if mode != "moe":
            P.op("dve", lambda e, xt=xt, ss=ss, hb=hb: e.scalar_tensor_tensor(
                out=hb[:], in0=xt[:], scalar=ss[:], in1=gbf[:], op0=ALU.mult, op1=ALU.mult),
                reads=[r_xt, r_ss, r_gbf], writes=[r_hb])
            st1[tb] = (hb, r_hb, None, None)
        else:
            hf, r_hf = hfr()
            hl, r_hl = hlr()
            P.op("dve", lambda e, xt=xt, ss=ss, hf=hf: e.scalar_tensor_tensor(
                out=hf[:], in0=xt[:], scalar=ss[:], in1=gbf[:], op0=ALU.mult, op1=ALU.mult),
                reads=[r_xt, r_ss, r_gbf], writes=[r_hf])
            P.op("dve", lambda e, hf=hf, hb=hb: e.tensor_copy(hb[:], hf[:]), reads=[r_hf], writes=[r_hb])
            P.op("dve", lambda e, hf=hf, hb=hb, hl=hl: e.tensor_tensor(out=hl[:], in0=hf[:], in1=hb[:], op=ALU.subtract),
                 reads=[r_hf, r_hb], writes=[r_hl])
            st1[tb] = (hb, r_hb, hl, r_hl)

    def stage1_b(tb):
        hb, r_hb, hl, r_hl = st1.pop(tb)
        if sparse:
            hi_t, r_hi = hir()
            emit_transposes_to(C, hb, r_hb, hi_t[:], r_hi, 8)
        else:
            emit_transposes_to(C, hb, r_hb, hT[:, :, tb * 128:(tb + 1) * 128], r_hT[tb], 8)
            hi_t, r_hi = hT[:, :, tb * 128:(tb + 1) * 128], r_hT[tb]
        if mode == "moe":
            lo, r_lo = lor()
            emit_transposes_to(C, hl, r_hl, lo[:], r_lo, 8, eng="dve")
            n = 0
            for kc in range(8):
                for (a, ra, b_, rb) in ((hi_t[:, kc, :], r_hi, rt_hi, r_rthi),
                                        (lo[:, kc, :], r_lo, rt_hi, r_rthi),
                                        (hi_t[:, kc, :], r_hi, rt_lo, r_rtlo)):
                    P.op("pe", lambda e, a=a, b_=b_, kc=kc, n=n: e.matmul(
                        C.pb[4][:, 0:NE], lhsT=a, rhs=b_[:, kc, :], start=(n == 0), stop=(n == 23)),
                        reads=[ra, rb], writes=[C.rpb[4]], signal=(n == 23))
                    n += 1
            lg, r_lg = smr()
            m8, r_m8 = m8r()
            cb = comb[:, tb, :]
            P.op("act", lambda e, lg=lg: e.copy(lg[:], C.pb[4][:, 0:NE]), reads=[C.rpb[4]], writes=[r_lg])
            P.op("dve", lambda e, lg=lg, m8=m8: e.max(out=m8[:], in_=lg[:]), reads=[r_lg], writes=[r_m8])
            P.op("dve", lambda e, lg=lg, m8=m8, cb=cb: e.tensor_scalar(
                out=cb, in0=lg[:], scalar1=m8[:, 1:2], scalar2=None, op0=ALU.is_ge),
                reads=[r_lg, r_m8], writes=[r_comb[tb]])
            P.op("dve", lambda e, lg=lg, m8=m8: e.tensor_scalar(
                out=lg[:], in0=lg[:], scalar1=m8[:, 0:1], scalar2=None, op0=ALU.subtract),
                reads=[r_lg, r_m8], writes=[r_lg])
            P.op("act", lambda e, lg=lg: e.activation(out=lg[:], in_=lg[:], func=AF.Exp), reads=[r_lg], writes=[r_lg])
            P.op("dve", lambda e, lg=lg, cb=cb: e.tensor_tensor(out=cb, in0=cb, in1=lg[:], op=ALU.mult),
                 reads=[r_lg, r_comb[tb]], writes=[r_comb[tb]])
            P.op("dve", lambda e, m8=m8, cb=cb: e.tensor_reduce(out=m8[:, 2:3], in_=cb, axis=AX.X, op=ALU.add),
                 reads=[r_comb[tb], r_m8], writes=[r_m8])
            P.op("dve", lambda e, m8=m8: e.reciprocal(m8[:, 2:3], m8[:, 2:3]), reads=[r_m8], writes=[r_m8])
            P.op("dve", lambda e, m8=m8, cb=cb: e.tensor_scalar(
                out=cb, in0=cb, scalar1=m8[:, 2:3], scalar2=None, op0=ALU.mult),
                reads=[r_comb[tb], r_m8], writes=[r_comb[tb]])


    for i in range(NTB + 1):
        if i < NTB:
            stage1_a(i)
        if i >= 1:
            stage1_b(i - 1)

    if sparse:
        cflat = comb[:].rearrange("p t e -> p (t e)")
        rkf = rkm[:].rearrange("p t e -> p (t e)")
        maskf, r_maskf = C.tile([128, 128], F32)
        maskb, r_maskb = C.tile([128, 128], BF16)
        Uf, r_Uf = C.tile([128, 128], F32)
        Ub, r_Ub = C.tile([128, 128], BF16)
        oneb, r_oneb = C.tile([128, 128], BF16)
        tot, r_tot = C.tile([128, NTB, NE], F32)
        off, r_off = C.tile([128, NTB, NE], F32)
        tmpm, r_tmpm = C.tile([128, 128], F32)
        ne_t, r_net = C.tile([128, NE], F32)
        flagf, r_flagf = C.tile([1, 32], F32)
        io_i, r_ioi = C.tile([128, 128], I32)
        P.op("dve", lambda e: e.tensor_scalar(out=maskf[:], in0=cflat, scalar1=0.0, scalar2=None, op0=ALU.is_gt),
             reads=r_comb, writes=[r_maskf])
        P.op("dve", lambda e: e.tensor_copy(maskb[:], maskf[:]), reads=[r_maskf], writes=[r_maskb])
        P.op("pool", lambda e: e.memset(Uf[:], 1.0), writes=[r_Uf])
        P.op("pool", lambda e: e.affine_select(out=Uf[:], in_=Uf[:], pattern=[[1, 128]], compare_op=ALU.is_ge, fill=0.0,
                                               base=-1, channel_multiplier=-1), reads=[r_Uf], writes=[r_Uf])
        P.op("dve", lambda e: e.tensor_copy(Ub[:], Uf[:]), reads=[r_Uf], writes=[r_Ub])
        P.op("dve", lambda e: e.memset(oneb[:], 1.0), writes=[r_oneb])
        P.op("pe", lambda e: e.matmul(C.pb[0][:, 0:128], lhsT=Ub[:], rhs=maskb[:], start=True, stop=True),
             reads=[r_Ub, r_maskb], writes=[C.rpb[0]])
        P.op("pe", lambda e: e.matmul(C.pb[1][:, 0:128], lhsT=oneb[:], rhs=maskb[:], start=True, stop=True),
             reads=[r_oneb, r_maskb], writes=[C.rpb[1]])
        P.op("act", lambda e: e.copy(rkf, C.pb[0][:, 0:128]), reads=[C.rpb[0]], writes=[r_rkm])
        P.op("act", lambda e: e.copy(tot[:].rearrange("p t e -> p (t e)"), C.pb[1][:, 0:128]), reads=[C.rpb[1]],
             writes=[r_tot])
        P.op("dve", lambda e: e.memset(off[:, 0, :], 0.0), writes=[r_off])
        for tb in range(1, NTB):
            P.op("dve", lambda e, tb=tb: e.tensor_tensor(out=off[:, tb, :], in0=off[:, tb - 1, :], in1=tot[:, tb - 1, :],
                                                         op=ALU.add), reads=[r_off, r_tot], writes=[r_off])
        P.op("dve", lambda e: e.tensor_tensor(out=rkf, in0=rkf, in1=off[:].rearrange("p t e -> p (t e)"), op=ALU.add),
             reads=[r_rkm, r_off], writes=[r_rkm])
        P.op("dve", lambda e: e.tensor_scalar(out=tmpm[:], in0=maskf[:], scalar1=-1.0, scalar2=1e9, op0=ALU.add,
                                              op1=ALU.mult), reads=[r_maskf], writes=[r_tmpm])
        P.op("dve", lambda e: e.tensor_tensor(out=rkf, in0=rkf, in1=maskf[:], op=ALU.mult), reads=[r_rkm, r_maskf],
             writes=[r_rkm])
        P.op("dve", lambda e: e.tensor_tensor(out=rkf, in0=rkf, in1=tmpm[:], op=ALU.add), reads=[r_rkm, r_tmpm],
             writes=[r_rkm])
        P.op("dve", lambda e: e.tensor_tensor(out=ne_t[:], in0=off[:, NTB - 1, :], in1=tot[:, NTB - 1, :], op=ALU.add),
             reads=[r_off, r_tot], writes=[r_net])
        PC = 512
        for p_ in range(4):
            P.op("dve", lambda e, p_=p_: e.tensor_scalar(out=flagf[0:1, p_ * 8:(p_ + 1) * 8], in0=ne_t[0:1, :],
                                                         scalar1=float(PC * p_), scalar2=None, op0=ALU.is_gt),
                 reads=[r_net], writes=[r_flagf])
        P.op("dve", lambda e: e.tensor_copy(flags_i[:], flagf[:]), reads=[r_flagf], writes=[r_flags])
        P.op("dve", lambda e: e.tensor_copy(chh[:].rearrange("p t e -> p (t e)"), cflat), reads=r_comb, writes=[r_chh])
        P.op("dve", lambda e: e.tensor_tensor(out=tmpm[:], in0=cflat, in1=chh[:].rearrange("p t e -> p (t e)"),
                                              op=ALU.subtract), reads=r_comb + [r_chh, r_tmpm], writes=[r_tmpm])
        P.op("dve", lambda e: e.tensor_copy(cll[:].rearrange("p t e -> p (t e)"), tmpm[:]), reads=[r_tmpm], writes=[r_cll])
        P.op("pool", lambda e: e.iota(io_i[:], pattern=[[1, 128]], base=0, channel_multiplier=0), writes=[r_ioi])
        P.op("dve", lambda e: e.tensor_copy(io_f[:], io_i[:]), reads=[r_ioi], writes=[r_iof])
        P.barrier()
        C.off = C.mark
        NSB = PC // 128
        xacc, _ = C.tile([128, NTB, D], F32)
        r_xacc = [Res(f"xacc{i}") for i in range(NTB)]
        for tb in range(NTB):
            P.dma("sp", lambda e, tb=tb: e.dma_start(out=xacc[:, tb, :], in_=x1s[tb * 128:(tb + 1) * 128, :]),
                  reads=[r_x1s[tb]], writes=[r_xacc[tb]], sem=P.dsem())
        hTe, r_hTe = C.tile([128, 8, PC], BF16)
        actT, _ = C.tile([128, NFC, PC], BF16)
        r_act = [Res(f"act{f}") for f in range(NFC)]
        PmT, _ = C.tile([128, NSB, TSH], BF16)
        r_PmT = [Res(f"PmT{i}") for i in range(NSB)]
        yg, _ = C.tile([128, NSB, D], BF16)
        r_yg = [Res(f"yg{i}") for i in range(NSB)]
        gs, r_gs = C.tile([128, NSB], F32)
        gtr = C.ring("gt", 2, [128, 2], F32)
        pmr = C.ring("Pm", 2, [128, NTB, 128], BF16)
        wgr = C.ring("wg", 3, [128, 8, 128], BF16, with_sem=True)
        wur = C.ring("wu", 2, [128, 8, 128], BF16, with_sem=True)
        wdr = C.ring("wd", 2, [128, D], BF16, with_sem=True)
        sgr = C.ring("sg", 2, [128, 512], F32)
        for ex in range(NE):
            wg_v = wg[ex].rearrange("(kc p) f -> p kc f", p=128)
            wu_v = wu[ex].rearrange("(kc p) f -> p kc f", p=128)
            for p_ in range(4):
                P.cond_begin(flags_i[0:1, p_ * 8 + ex:p_ * 8 + ex + 1], r_flags)
                for sb in range(NSB):
                    s0 = PC * p_ + 128 * sb
                    Pm, r_Pm = pmr()
                    for tb in range(NTB):
                        P.op("dve", lambda e, tb=tb, Pm=Pm, s0=s0, ex=ex: e.tensor_scalar(
                            out=Pm[:, tb, :], in0=io_f[:], scalar1=float(s0), scalar2=rkm[:, tb, ex:ex + 1],
                            op0=ALU.add, op1=ALU.is_equal), reads=[r_iof, r_rkm], writes=[r_Pm])
                    gb = (0, 1) if sb % 2 == 0 else (2, 3)
                    for kc in range(8):
                        bank, c0 = gb[kc // 4], (kc % 4) * 128
                        for tb in range(NTB):
                            P.op("pe", lambda e, tb=tb, kc=kc, bank=bank, c0=c0, Pm=Pm: e.matmul(
                                C.pb[bank][:, c0:c0 + 128], lhsT=h_tm[:, tb, kc * 128:(kc + 1) * 128], rhs=Pm[:, tb, :],
                                start=(tb == 0), stop=(tb == NTB - 1)), reads=[r_htm[tb], r_Pm], writes=[C.rpb[bank]])
                    for hh in range(2):
                        P.op("act", lambda e, hh=hh, sb=sb, gb=gb: e.copy(
                            hTe[:, hh * 4:(hh + 1) * 4, sb * 128:(sb + 1) * 128],
                            C.pb[gb[hh]][:, 0:512].rearrange("p (k s) -> p k s", k=4)),
                            reads=[C.rpb[gb[hh]]], writes=[r_hTe])
                    for col, cc in ((0, chh), (1, cll)):
                        for tb in range(NTB):
                            P.op("pe", lambda e, tb=tb, col=col, cc=cc, Pm=Pm, ex=ex: e.matmul(
                                C.pb[4][:, col:col + 1], lhsT=Pm[:, tb, :], rhs=cc[:, tb, ex:ex + 1],
                                start=(tb == 0), stop=(tb == NTB - 1)), reads=[r_Pm, r_chh, r_cll], writes=[C.rpb[4]])
                    gt, r_gt = gtr()
                    P.op("act", lambda e, gt=gt: e.copy(gt[:], C.pb[4][:, 0:2]), reads=[C.rpb[4]], writes=[r_gt])
                    P.op("dve", lambda e, gt=gt, sb=sb: e.tensor_tensor(out=gs[:, sb:sb + 1], in0=gt[:, 0:1], in1=gt[:, 1:2],
                                                                        op=ALU.add), reads=[r_gt, r_gs], writes=[r_gs])
                    for g8 in range(2):
                        bank = (5, 6)[g8]
                        for t8 in range(8):
                            tb = g8 * 8 + t8
                            P.op("pe", lambda e, tb=tb, t8=t8, bank=bank, Pm=Pm: e.transpose(
                                C.psbv[bank][:, t8 * 128:(t8 + 1) * 128], Pm[:, tb, :], C.ident[:]),
                                reads=[r_Pm, C.r_ident], writes=[C.rpb[bank]])
                        if g8 == 0:
                            P.op("act", lambda e, sb=sb, bank=bank: e.copy(PmT[:, sb, 0:1024], C.psbv[bank][:, 0:1024]),
                                 reads=[C.rpb[bank]], writes=[r_PmT[sb]])
                        else:
                            P.op("dve", lambda e, sb=sb, bank=bank: e.tensor_copy(PmT[:, sb, 1024:2048],
                                                                                  C.psbv[bank][:, 0:1024]),
                                 reads=[C.rpb[bank], r_PmT[sb]], writes=[r_PmT[sb]])
                for fc in range(NFC):
                    g_t, r_g, s_g = wgr()
                    u_t, r_u, s_u = wur()
                    P.dma("pool", lambda e, fc=fc, g_t=g_t, wg_v=wg_v: e.dma_start(
                        out=g_t[:], in_=wg_v[:, :, fc * 128:(fc + 1) * 128]), writes=[r_g], sem=s_g)
                    P.dma("pool", lambda e, fc=fc, u_t=u_t, wu_v=wu_v: e.dma_start(
                        out=u_t[:], in_=wu_v[:, :, fc * 128:(fc + 1) * 128]), writes=[r_u], sem=s_u)
                    pg, pu = (0, 1) if fc % 2 == 0 else (2, 3)
                    for kc in range(8):
                        P.op("pe", lambda e, kc=kc, g_t=g_t, pg=pg: e.matmul(
                            C.pb[pg][:, :], lhsT=g_t[:, kc, :], rhs=hTe[:, kc, :], start=(kc == 0), stop=(kc == 7)),
                            reads=[r_g, r_hTe], writes=[C.rpb[pg]])
                    for kc in range(8):
                        P.op("pe", lambda e, kc=kc, u_t=u_t, pu=pu: e.matmul(
                            C.pb[pu][:, :], lhsT=u_t[:, kc, :], rhs=hTe[:, kc, :], start=(kc == 0), stop=(kc == 7)),
                            reads=[r_u, r_hTe], writes=[C.rpb[pu]])
                    sg, r_sg = sgr()
                    P.op("act", lambda e, sg=sg, pg=pg: e.activation(out=sg[:], in_=C.pb[pg][:, :], func=AF.Silu),
                         reads=[C.rpb[pg]], writes=[r_sg])
                    P.op("dve", lambda e, sg=sg, pu=pu, fc=fc: e.tensor_tensor(
                        out=actT[:, fc, :], in0=sg[:], in1=C.pb[pu][:, :], op=ALU.mult),
                        reads=[r_sg, C.rpb[pu]], writes=[r_act[fc]])
                for fc in range(NFC):
                    d_t, r_d, s_d = wdr()
                    P.dma("pool", lambda e, fc=fc, d_t=d_t, ex=ex: e.dma_start(
                        out=d_t[:], in_=wd[ex, fc * 128:(fc + 1) * 128, :]), writes=[r_d], sem=s_d)
                    for sb in range(NSB):
                        for hf_ in range(2):
                            bk = sb * 2 + hf_
                            P.op("pe", lambda e, fc=fc, sb=sb, hf_=hf_, bk=bk, d_t=d_t: e.matmul(
                                C.pb[bk][:, :], lhsT=actT[:, fc, sb * 128:(sb + 1) * 128],
                                rhs=d_t[:, hf_ * 512:(hf_ + 1) * 512], start=(fc == 0), stop=(fc == NFC - 1)),
                                reads=[r_d, r_act[fc]], writes=[C.rpb[bk]])
                for sb in range(NSB):
                    for hf_ in range(2):
                        bk = sb * 2 + hf_
                        P.op("dve", lambda e, sb=sb, hf_=hf_, bk=bk: e.tensor_scalar(
                            out=yg[:, sb, hf_ * 512:(hf_ + 1) * 512], in0=C.pb[bk][:, :], scalar1=gs[:, sb:sb + 1],
                            scalar2=None, op0=ALU.mult), reads=[C.rpb[bk], r_gs], writes=[r_yg[sb]])
                for tb in range(NTB):
                    for hf_ in range(2):
                        bk = (tb * 2 + hf_) % 4
                        for sb in range(NSB):
                            P.op("pe", lambda e, tb=tb, hf_=hf_, bk=bk, sb=sb: e.matmul(
                                C.pb[bk][:, :], lhsT=PmT[:, sb, tb * 128:(tb + 1) * 128],
                                rhs=yg[:, sb, hf_ * 512:(hf_ + 1) * 512], start=(sb == 0), stop=(sb == NSB - 1)),
                                reads=[r_PmT[sb], r_yg[sb]], writes=[C.rpb[bk]])
                        P.op("dve", lambda e, tb=tb, hf_=hf_, bk=bk: e.tensor_tensor(
                            out=xacc[:, tb, hf_ * 512:(hf_ + 1) * 512], in0=xacc[:, tb, hf_ * 512:(hf_ + 1) * 512],
                            in1=C.pb[bk][:, :], op=ALU.add), reads=[C.rpb[bk], r_xacc[tb]], writes=[r_xacc[tb]])
                P.cond_end()
        for tb in range(NTB):
            if not last and x_out is not None:
                P.dma("sp", lambda e, tb=tb: e.dma_start(out=x_out[tb * 128:(tb + 1) * 128, :], in_=xacc[:, tb, :]),
                      reads=[r_xacc[tb]], writes=[d_out], sem=P.dsem())
            emit_next(xacc[:, tb, :], r_xacc[tb], tb)
        return

    HT = 1024
    NHB = HT // 128
    actT, _ = C.tile([128, NFC, HT], BF16)
    r_act = [[Res(f"act{f}_{s}") for s in range(HT // 512)] for f in range(NFC)]
    xacc, _ = C.tile([128, NHB, D], F32)
    r_xacc = [Res(f"xacc{i}") for i in range(NHB)]
    s_xa = [P.dsem() for _ in range(NHB)]
    wgr = C.ring("wg", 3, [128, 8, 128], BF16, with_sem=True)
    wur = C.ring("wu", 3, [128, 8, 128], BF16, with_sem=True)
    wdr = C.ring("wd", 3, [128, D], BF16, with_sem=True)
    sgr = C.ring("sg", 2, [128, 512], F32)
    for half in range(TSH // HT):
        for j in range(NHB):
            tb = half * NHB + j
            P.dma("sp", lambda e, tb=tb, j=j: e.dma_start(out=xacc[:, j, :], in_=x1s[tb * 128:(tb + 1) * 128, :]),
                  reads=[r_x1s[tb]], writes=[r_xacc[j]], sem=s_xa[j])
        for ex in range(0 if os.environ.get('K_SKIP2') else ne):
            wg_v = wg[ex].rearrange("(kc p) f -> p kc f", p=128)
            wu_v = wu[ex].rearrange("(kc p) f -> p kc f", p=128)
            for fc in range(NFC):
                g_t, r_g, s_g = wgr()
                u_t, r_u, s_u = wur()
                P.dma("pool", lambda e, fc=fc, g_t=g_t, wg_v=wg_v: e.dma_start(
                    out=g_t[:], in_=wg_v[:, :, fc * 128:(fc + 1) * 128]), writes=[r_g], sem=s_g)
                P.dma("pool", lambda e, fc=fc, u_t=u_t, wu_v=wu_v: e.dma_start(
                    out=u_t[:], in_=wu_v[:, :, fc * 128:(fc + 1) * 128]), writes=[r_u], sem=s_u)
                if DBG2 == 1:
                    continue
                for st_ in range(HT // 512):
                    t0 = half * HT + st_ * 512
                    rh = [r_hT[(t0 // 128) + i] for i in range(4)]
                    pg, pu = (0, 1) if (fc * 2 + st_) % 2 == 0 else (2, 3)
                    for kc in range(8):
                        P.op("pe", lambda e, kc=kc, g_t=g_t, t0=t0, pg=pg: e.matmul(
                            C.pb[pg][:, :], lhsT=g_t[:, kc, :], rhs=hT[:, kc, t0:t0 + 512], start=(kc == 0),
                            stop=(kc == 7)), reads=[r_g] + rh, writes=[C.rpb[pg]], signal=(kc == 7))
                    for kc in range(8):
                        P.op("pe", lambda e, kc=kc, u_t=u_t, t0=t0, pu=pu: e.matmul(
                            C.pb[pu][:, :], lhsT=u_t[:, kc, :], rhs=hT[:, kc, t0:t0 + 512], start=(kc == 0),
                            stop=(kc == 7)), reads=[r_u] + rh, writes=[C.rpb[pu]], signal=(kc == 7))
                    sg, r_sg = sgr()
                    P.op("act", lambda e, sg=sg, pg=pg: e.activation(out=sg[:], in_=C.pb[pg][:, :], func=AF.Silu),
                         reads=[C.rpb[pg]], writes=[r_sg])
                    P.op("dve", lambda e, sg=sg, pu=pu, fc=fc, st_=st_: e.tensor_tensor(
                        out=actT[:, fc, st_ * 512:(st_ + 1) * 512], in0=sg[:], in1=C.pb[pu][:, :], op=ALU.mult),
                        reads=[r_sg, C.rpb[pu]], writes=[r_act[fc][st_]])
            for jg in range(0 if DBG2 in (1, 2) else HT // 512):
                for fc in range(NFC):
                    d_t, r_d, s_d = wdr()
                    P.dma("pool", lambda e, fc=fc, d_t=d_t, ex=ex: e.dma_start(
                        out=d_t[:], in_=wd[ex, fc * 128:(fc + 1) * 128, :]), writes=[r_d], sem=s_d)
                    for jj in range(4):
                        j = jg * 4 + jj
                        for hf_ in range(2):
                            bk = jj * 2 + hf_
                            P.op("pe", lambda e, fc=fc, j=j, hf_=hf_, bk=bk, d_t=d_t: e.matmul(
                                C.pb[bk][:, :], lhsT=actT[:, fc, j * 128:(j + 1) * 128],
                                rhs=d_t[:, hf_ * 512:(hf_ + 1) * 512], start=(fc == 0), stop=(fc == NFC - 1)),
                                reads=[r_d, r_act[fc][jg]], writes=[C.rpb[bk]], signal=(fc == NFC - 1 or (jj == 3 and hf_ == 1)))
                for jj in range(4):
                    j = jg * 4 + jj
                    tb = half * NHB + j
                    for hf_ in range(2):
                        bk = jj * 2 + hf_
                        if mode == "moe":
                            P.op("dve", lambda e, j=j, hf_=hf_, bk=bk, tb=tb, ex=ex: e.scalar_tensor_tensor(
                                out=xacc[:, j, hf_ * 512:(hf_ + 1) * 512], in0=C.pb[bk][:, :],
                                scalar=comb[:, tb, ex:ex + 1], in1=xacc[:, j, hf_ * 512:(hf_ + 1) * 512],
                                op0=ALU.mult, op1=ALU.add), reads=[C.rpb[bk], r_comb[tb], r_xacc[j]],
                                writes=[r_xacc[j]])
                        else:
                            P.op("dve", lambda e, j=j, hf_=hf_, bk=bk: e.tensor_tensor(
                                out=xacc[:, j, hf_ * 512:(hf_ + 1) * 512], in0=xacc[:, j, hf_ * 512:(hf_ + 1) * 512],
                                in1=C.pb[bk][:, :], op=ALU.add), reads=[C.rpb[bk], r_xacc[j]], writes=[r_xacc[j]])
        for j in range(NHB):
            tb = half * NHB + j
            if not last and x_out is not None:
                P.dma("sp", lambda e, tb=tb, j=j: e.dma_start(out=x_out[tb * 128:(tb + 1) * 128, :], in_=xacc[:, j, :]),
                      reads=[r_xacc[j]], writes=[d_out], sem=s_xa[j])
            emit_next(xacc[:, j, :], r_xacc[j], tb)
    return


def build_C(mode, last):
    nc = bass.Bass("TRN2", target_bir_lowering=False)
    C = Ctx(nc)
    ne = NE if mode in ("moe", "moes") else 1
    T = {"x": dram_in(nc, "x", [TSH, D], F32), "g_next": dram_in(nc, "g_next", [D], F32)}
    if mode != "N":
        T.update(mixT=dram_in(nc, "mixT", [D, TSH], BF16), w_out=dram_in(nc, "w_out", [D, D], F32),
                 g_ffn=dram_in(nc, "g_ffn", [D], F32), wg=dram_in(nc, "wg", [ne, D, FF], F32),
                 wu=dram_in(nc, "wu", [ne, D, FF], F32), wd=dram_in(nc, "wd", [ne, FF, D], F32))
        if mode in ("moe", "moes"):
            T["router"] = dram_in(nc, "router", [D, NE], F32)
    if last:
        T["y"] = dram_out(nc, "y", [TSH, D], F32)
    else:
        T["x_out"] = dram_out(nc, "x_out", [TSH, D], F32)
        T["hT"] = dram_out(nc, "hT", [D, TSH], BF16)
    emit_C(C, T, mode, last)
    C.P.wait_all("sp", [])
    C.P.emit()
    return nc


def emit_AB(C, T):
    nc = C.nc
    P = C.P
    hT_in = T["hT"]
    w_in, wq_up, wkv_up, qn, kvn = T["w_in"], T["wq_up"], T["wkv_up"], T["qn"], T["kvn"]
    scal, pos, cst, yT = T["scal"], T["pos"], T["cst"], T["yT"]
    C.uid += 1
    vscr = nc.dram_tensor(f"vscr{C.uid}", [S, 64], BF16).ap()
    d_out = Res("d_out")
    r_vscr = Res("vscr")
    NT = S // 512
    s_c = P.dsem()

    w_sb, r_w = C.tile([128, 8, NCOL], BF16)
    s_w = P.dsem()
    w_v = w_in.rearrange("(kc p) n -> p kc n", p=128)
    for kc in range(8):
        P.dma("pool", lambda e, kc=kc: e.dma_start(out=w_sb[:, kc, :], in_=w_v[:, kc, :]), writes=[r_w], sem=s_w)
    wq_f, r_wqf = C.tile([128, 2, 96], F32)
    wq_s, r_wq = C.tile([128, 2, 96], BF16)
    qn_t, r_qn = C.tile([128, 2], F32)
    wkv_f, r_wkvf = C.tile([128, 128], F32)
    wkv_s, r_wkv = C.tile([128, 128], BF16)
    kvn_t, r_kvn = C.tile([128, 1], F32)
    P.dma("sp", lambda e: e.dma_start(out=wq_f[:], in_=wq_up.rearrange("(c p) n -> p c n", p=128)), writes=[r_wqf], sem=P.dsem())
    for c in range(2):
        P.dma("sp", lambda e, c=c: e.dma_start(out=qn_t[:, c:c + 1], in_=qn[c * 128:(c + 1) * 128].rearrange("(p o) -> p o", o=1)),
              writes=[r_qn], sem=P.dsem())
    P.dma("sp", lambda e: e.dma_start(out=wkv_f[:], in_=wkv_up), writes=[r_wkvf], sem=P.dsem())
    P.dma("sp", lambda e: e.dma_start(out=kvn_t[:], in_=kvn.rearrange("(p o) -> p o", o=1)), writes=[r_kvn], sem=P.dsem())
    for c in range(2):
        P.op("dve", lambda e, c=c: e.tensor_scalar(out=wq_s[:, c, :], in0=wq_f[:, c, :], scalar1=qn_t[:, c:c + 1], scalar2=None,
                                                   op0=ALU.mult), reads=[r_wqf, r_qn], writes=[r_wq])
    P.op("dve", lambda e: e.tensor_scalar(out=wkv_s[:], in0=wkv_f[:], scalar1=kvn_t[:, 0:1], scalar2=None, op0=ALU.mult),
         reads=[r_wkvf, r_kvn], writes=[r_wkv])
    sc0, r_sc0 = C.tile([1, 8], F32)
    sc64, r_sc64 = C.tile([128, 8], F32)
    P.dma("sp", lambda e: e.dma_start(out=sc0[:], in_=scal), writes=[r_sc0], sem=P.dsem())
    P.dma("sp", lambda e: e.dma_start(out=sc64[64:65, :], in_=scal), writes=[r_sc64], sem=P.dsem())
    P.op("dve", lambda e: e.tensor_scalar(out=sc0[:, 0:1], in0=sc0[:, 0:1], scalar1=-1.0, scalar2=None, op0=ALU.mult),
         reads=[r_sc0], writes=[r_sc0])
    P.op("act", lambda e: e.activation(out=sc64[64:65, 2:3], in_=sc64[64:65, 1:2], func=AF.Exp), reads=[r_sc64],
         writes=[r_sc64])
    ones_f, r_ones = C.tile([128, 64], F32)
    P.op("dve", lambda e: e.memset(ones_f[:], 1.0), writes=[r_ones])
    cst_t, r_cst = C.tile([128, 32], F32)
    P.dma("sp", lambda e: e.dma_start(out=cst_t[:], in_=cst), writes=[r_cst], sem=P.dsem())
    pos_i, r_posi = C.tile([128, 64], I32)
    pos_f, r_posf = C.tile([128, 64], F32)
    P.dma("sp", lambda e: e.dma_start(out=pos_i[:], in_=pos), writes=[r_posi], sem=P.dsem())
    P.op("dve", lambda e: e.tensor_copy(pos_f[:], pos_i[:]), reads=[r_posi], writes=[r_posf])
    ang, r_ang = C.tile([128, 64 * 16], F32)
    sin_t, r_sin = C.tile([128, 64 * 16], F32)
    cos_t, r_cos = C.tile([128, 64 * 16], F32)
    tkf, r_tkf = C.tile([128, 64 * 16], F32)
    tki, r_tki = C.tile([128, 64 * 16], I32)
    tfx, r_tfx = tkf, r_tkf
    for blk in range(64):
        P.op("dve", lambda e, blk=blk: e.tensor_scalar(out=ang[:, blk * 16:(blk + 1) * 16], in0=cst_t[:, 0:16],
                                                       scalar1=pos_f[:, blk:blk + 1], scalar2=None, op0=ALU.mult),
             reads=[r_cst, r_posf], writes=[r_ang])

    def emit_sin(dst, r_dst, off):
        md = dst
        P.op("dve", lambda e: e.tensor_scalar(out=tkf[:], in0=ang[:], scalar1=off, scalar2=1.0 / (2 * PI), op0=ALU.add,
                                              op1=ALU.mult), reads=[r_ang], writes=[r_tkf])
        P.op("dve", lambda e: e.tensor_copy(tki[:], tkf[:]), reads=[r_tkf], writes=[r_tki])
        P.op("dve", lambda e: e.tensor_copy(tkf[:], tki[:]), reads=[r_tki], writes=[r_tkf])
        P.op("dve", lambda e: e.scalar_tensor_tensor(out=md[:], in0=tkf[:], scalar=-2 * PI, in1=ang[:], op0=ALU.mult,
                                                     op1=ALU.add), reads=[r_tkf, r_ang], writes=[r_dst])
        if off != 0.0:
            P.op("dve", lambda e: e.tensor_scalar(out=md[:], in0=md[:], scalar1=off, scalar2=None, op0=ALU.add),
                 reads=[r_dst], writes=[r_dst])
        P.op("dve", lambda e: e.tensor_scalar(out=tfx[:], in0=md[:], scalar1=PI, scalar2=-2 * PI, op0=ALU.is_gt,
                                              op1=ALU.mult), reads=[r_dst], writes=[r_tfx])
        P.op("dve", lambda e: e.tensor_tensor(out=md[:], in0=md[:], in1=tfx[:], op=ALU.add), reads=[r_dst, r_tfx],
             writes=[r_dst])
        P.op("dve", lambda e: e.tensor_scalar(out=tfx[:], in0=md[:], scalar1=-PI, scalar2=2 * PI, op0=ALU.is_lt,
                                              op1=ALU.mult), reads=[r_dst], writes=[r_tfx])
        P.op("dve", lambda e: e.tensor_tensor(out=md[:], in0=md[:], in1=tfx[:], op=ALU.add), reads=[r_dst, r_tfx],
             writes=[r_dst])
        P.op("act", lambda e: e.activation(out=md[:], in_=md[:], func=AF.Sin), reads=[r_dst], writes=[r_dst])

    emit_sin(sin_t, r_sin, 0.0)
    emit_sin(cos_t, r_cos, PI / 2)
    trif, r_trif = C.tile([128, 128], F32)
    tri, r_tri = C.tile([128, 128], BF16)
    P.op("pool", lambda e: e.memset(trif[:], 1.0), writes=[r_trif])
    P.op("pool", lambda e: e.affine_select(out=trif[:], in_=trif[:], pattern=[[1, 128]], compare_op=ALU.is_ge, fill=0.0,
                                           base=0, channel_multiplier=-1), reads=[r_trif], writes=[r_trif])
    P.op("dve", lambda e: e.tensor_copy(tri[:], trif[:]), reads=[r_trif], writes=[r_tri])
    jmp_i, r_jmpi = C.tile([128, 256], I32)
    jmp_f, r_jmpf = C.tile([128, 256], F32)
    P.op("pool", lambda e: e.iota(jmp_i[:], pattern=[[1, 256]], base=0, channel_multiplier=-1), writes=[r_jmpi])
    P.op("dve", lambda e: e.tensor_copy(jmp_f[:], jmp_i[:]), reads=[r_jmpi], writes=[r_jmpf])
    Mb, r_Mb = [], []
    for m in range(4):
        t, r = C.tile([128, 256], F32)
        span = 127 if m == 0 else 128
        P.op("act", lambda e, t=t, m=m: e.activation(out=t[:], in_=jmp_f[:], func=AF.Exp, scale=cst_t[:, 16 + m:17 + m]),
             reads=[r_jmpf, r_cst], writes=[r])
        P.op("pool", lambda e, t=t: e.affine_select(out=t[:], in_=t[:], pattern=[[1, 256]], compare_op=ALU.is_ge, fill=0.0,
                                                    base=0, channel_multiplier=-1), reads=[r], writes=[r])
        P.op("pool", lambda e, t=t, span=span: e.affine_select(out=t[:], in_=t[:], pattern=[[-1, 256]], compare_op=ALU.is_ge,
                                                               fill=0.0, base=span, channel_multiplier=1), reads=[r], writes=[r])
        Mb.append(t)
        r_Mb.append(r)

    QT, _ = C.tile([128, S], BF16)
    KT, _ = C.tile([128, S], BF16)
    r_QT = [Res(f"QT{i}") for i in range(NT)]
    r_KT = [Res(f"KT{i}") for i in range(NT)]
    V = []
    r_V = []
    for i in range(3):
        t, _ = C.tile([128, 64, 65], BF16)
        V.append(t)
        r_V.append([Res(f"V{i}_{j}") for j in range(NT)])
        P.op("pool", lambda e, t=t: e.memset(t[:, :, 64:65], 1.0), writes=r_V[i])
    ysb, _ = C.tile([64, S], BF16)
    r_ysb = [Res(f"ysb{i}") for i in range(NT)]
    s_y = [P.dsem() for _ in range(NT)]
    acc, _ = C.tile([65, S], F32)
    r_acc = [Res(f"acc{i}") for i in range(NT)]
    hring = C.ring("hT", 2, [128, 8, 512], BF16, with_sem=True)
    pring = C.ring("pt", 4, [128, 512], BF16)
    ering = C.ring("et", 2, [128, 256], F32)
    oring = C.ring("osb", 3, [128, 512], F32)

    def load_hT(tt):
        sh = (tt * 512) // TSH
        tl0 = (tt * 512) % TSH
        h, r_h, s_h = hring()
        if "hT_fn" in T:
            src = T["hT_fn"](sh, tl0)
        else:
            src = hT_in[sh].rearrange("(kc p) t -> p kc t", p=128)[:, :, tl0:tl0 + 512]
        P.dma("sp", lambda e: e.dma_start(out=h[:], in_=src), writes=[r_h], sem=s_h)
        return h, r_h

    prot = {"i": 0}
    PROJ_BANKS = [0, 1, 2, 3, 5, 6]

    def next_bank():
        b = PROJ_BANKS[prot["i"] % len(PROJ_BANKS)]
        prot["i"] += 1
        return b

    def proj_fm(h, r_h, col0, m, dst_ap, r_dst, scale, bank):
        bank = next_bank()
        for kc in range(8):
            P.op("pe", lambda e, kc=kc: e.matmul(C.pb[bank][0:m, :], lhsT=w_sb[:, kc, col0:col0 + m], rhs=h[:, kc, :],
                                                 start=(kc == 0), stop=(kc == 7)), reads=[r_w, r_h], writes=[C.rpb[bank]])
        P.op("act", lambda e: e.mul(dst_ap, C.pb[bank][0:m, :], scale), reads=[C.rpb[bank]], writes=[r_dst])

    def proj_v(h, r_h, col0, tt, vt, r_vt, bank):
        bank = next_bank()
        for sb in range(4):
            for kc in range(8):
                P.op("pe", lambda e, kc=kc, sb=sb: e.matmul(
                    C.pb[bank][:, sb * 64:(sb + 1) * 64], lhsT=h[:, kc, sb * 128:(sb + 1) * 128],
                    rhs=w_sb[:, kc, col0:col0 + 64], start=(kc == 0), stop=(kc == 7)),
                    reads=[r_w, r_h], writes=[C.rpb[bank]])
        P.op("dve", lambda e: e.tensor_copy(vt[:, tt * 4:(tt + 1) * 4, 0:64],
                                            C.pb[bank][:, 0:256].rearrange("p (b d) -> p b d", b=4)),
             reads=[C.rpb[bank]], writes=[r_vt[tt]])

    def finalize(ob, src_ap, r_src, tq, sink):
        osb, r_osb = oring()
        P.op("act", lambda e: e.copy(osb[0:65, :], src_ap), reads=r_src, writes=[r_osb])
        if sink:
            P.op("dve", lambda e: e.tensor_scalar(out=osb[64:65, :], in0=osb[64:65, :], scalar1=sc64[64:65, 2:3],
                                                  scalar2=None, op0=ALU.add), reads=[r_osb, r_sc64], writes=[r_osb])
        P.op("dve", lambda e: e.reciprocal(osb[64:65, :], osb[64:65, :]), reads=[r_osb], writes=[r_osb])
        P.op("pe", lambda e: e.matmul(C.pb[4][0:64, :], lhsT=ones_f[64:65, 0:64], rhs=osb[64:65, :], start=True,
                                      stop=True), reads=[r_ones, r_osb], writes=[C.rpb[4]])
        P.op("dve", lambda e: e.tensor_tensor(out=ysb[0:64, tq * 512:(tq + 1) * 512], in0=osb[0:64, :],
                                              in1=C.pb[4][0:64, :], op=ALU.mult), reads=[r_osb, C.rpb[4]],
             writes=[r_ysb[tq]])

    def store_y(mixer):
        r_st = Res(f"yst{mixer}")
        P.dma("sp", lambda e: e.dma_start(out=yT[mixer * 64:(mixer + 1) * 64, :], in_=ysb[0:64, :]),
              reads=r_ysb, writes=[d_out, r_st], sem=s_y[0])
        if "after_store" in T:
            T["after_store"](mixer, r_st)

    def attn_causal(kdim):
        for qt in range(NT):
            t0 = qt * 512
            nkb = (t0 + 512) // 128
            ob = 2 + qt % 2

            SB = (0, 1, 5, 6)
            LA = 3

            def s_mm(kb):
                o = max(0, kb * 128 - t0)
                sb_ = SB[kb % 4]
                P.op("pe", lambda e, t0=t0, o=o, kb=kb, sb_=sb_: e.matmul(
                    C.pb[sb_][:, o:512], lhsT=KT[0:kdim, kb * 128:(kb + 1) * 128],
                    rhs=QT[0:kdim, t0 + o:t0 + 512], start=True, stop=True),
                     reads=[r_KT[kb // 4], r_QT[qt]], writes=[C.rpb[sb_]])
            for kb in range(min(LA, nkb)):
                s_mm(kb)
            for kb in range(nkb):
                if kb + LA < nkb:
                    s_mm(kb + LA)
                o = max(0, kb * 128 - t0)
                sb_ = SB[kb % 4]
                pt, r_pt = pring()
                P.op("act", lambda e, o=o, pt=pt, sb_=sb_: e.activation(out=pt[:, o:512], in_=C.pb[sb_][:, o:512],
                                                                        func=AF.Exp), reads=[C.rpb[sb_]], writes=[r_pt])
                if kb * 128 >= t0:
                    P.op("pool", lambda e, o=o, pt=pt: e.tensor_tensor(out=pt[:, o:o + 128], in0=pt[:, o:o + 128],
                                                                       in1=tri[:], op=ALU.mult), reads=[r_pt, r_tri],
                         writes=[r_pt])
                P.op("pe", lambda e, o=o, pt=pt, kb=kb, ob=ob, nkb=nkb: e.matmul(
                    C.pb[ob][0:65, o:512], lhsT=V[0][:, kb, 0:65], rhs=pt[:, o:512], start=(kb == 0),
                    stop=(kb == nkb - 1), skip_group_check=True), reads=[r_V[0][kb // 4], r_pt], writes=[C.rpb[ob]])
            finalize(ob, C.pb[ob][0:65, :], [C.rpb[ob]], qt, False)

    cnt = [0]

    def attn_banded(dil, m, vt, r_vt, evac):
        L = S // dil
        for r in range(dil):
            for qt in range(L // 512):
                n0 = qt * 512
                ob = 2 + cnt[0] % 2
                cnt[0] += 1
                kbs = [kb for kb in range(n0 // 128 - 1, n0 // 128 + 4) if kb >= 0]
                tl_lo = (dil * n0) // 512
                tl_hi = min(NT - 1, (dil * (n0 + 511) + r) // 512)
                rq = [r_QT[i] for i in range(tl_lo, tl_hi + 1)]
                SBK = (0, 1, 5, 6, 7)
                geo = []
                for i, kb in enumerate(kbs):
                    qa = max(kb * 128, n0)
                    qb = min(kb * 128 + 256, n0 + 512)
                    jo, w, co = qa - kb * 128, qb - qa, qa - n0
                    kc0 = r + dil * kb * 128
                    kcols = slice(kc0, kc0 + dil * 127 + 1, dil)
                    qcols = slice(r + dil * qa, r + dil * (qb - 1) + 1, dil)
                    ktl = sorted(set([(kc0) // 512, min(NT - 1, (kc0 + dil * 127) // 512)]))
                    rk = [r_KT[j] for j in range(ktl[0], ktl[-1] + 1)]
                    sbk = SBK[i]
                    geo.append((jo, w, co, sbk))
                    P.op("pe", lambda e, w=w, kcols=kcols, qcols=qcols, sbk=sbk: e.matmul(
                        C.pb[sbk][:, 0:w], lhsT=KT[0:64, kcols], rhs=QT[0:64, qcols], start=True, stop=True),
                        reads=rk + rq, writes=[C.rpb[sbk]])
                for i, kb in enumerate(kbs):
                    jo, w, co, sbk = geo[i]
                    et, r_et = ering()
                    pt, r_pt = pring()
                    P.op("act", lambda e, w=w, et=et, sbk=sbk: e.activation(out=et[:, 0:w], in_=C.pb[sbk][:, 0:w],
                                                                            func=AF.Exp), reads=[C.rpb[sbk]], writes=[r_et])
                    P.op("dve", lambda e, w=w, et=et, pt=pt, jo=jo: e.tensor_tensor(
                        out=pt[:, 0:w], in0=et[:, 0:w], in1=Mb[m][:, jo:jo + w], op=ALU.mult),
                        reads=[r_et, r_Mb[m]], writes=[r_pt])
                    ch = r * (L // 128) + kb
                    P.op("pe", lambda e, w=w, co=co, pt=pt, ch=ch, i=i, ob=ob, nk=len(kbs): e.matmul(
                        C.pb[ob][0:65, co:co + w], lhsT=vt[:, ch, 0:65], rhs=pt[:, 0:w], start=(i == 0),
                        stop=(i == nk - 1), skip_group_check=True), reads=[r_vt[ch // 4], r_pt], writes=[C.rpb[ob]])
                evac(ob, r, n0, tl_lo, tl_hi)

    P.op("dve", lambda e: e.memset(QT[64:70, :], 1.0), writes=r_QT)
    P.op("dve", lambda e: e.memset(KT[64:70, :], 1.0), writes=r_KT)
    s_augq = [P.dsem() for _ in range(NT)]
    s_augk = [P.dsem() for _ in range(NT)]
    cumr = C.ring("cum", 2, [1, 512], F32)
    one1, r_one1 = C.tile([1, 512], F32)
    P.op("dve", lambda e: e.memset(one1[:], 1.0), writes=[r_one1])
    zero1, r_zero1 = C.tile([1, 1], F32)
    P.op("dve", lambda e: e.memset(zero1[:], 0.0), writes=[r_zero1])
    spr = C.ring("sp", 1, [1, 512], F32)
    augr = C.ring("aug", 1, [1, 6, 512], BF16)
    rr = C.ring("rr", 1, [1, 2, 512], F32)
    prev = (zero1[:, 0:1], r_zero1)
    for tt in range(NT):
        h, r_h = load_hT(tt)
        cols = slice(tt * 512, (tt + 1) * 512)
        proj_fm(h, r_h, C_FQ, 64, QT[0:64, cols], r_QT[tt], 0.125, 5)
        proj_fm(h, r_h, C_FK, 64, KT[0:64, cols], r_KT[tt], 1.0, 6)
        proj_v(h, r_h, C_FV, tt, V[0], r_V[0], 5)
        for kc in range(8):
            P.op("pe", lambda e, kc=kc, h=h: e.matmul(C.pb[4][0:1, :], lhsT=w_sb[:, kc, C_FF:C_FF + 1], rhs=h[:, kc, :],
                                                      start=(kc == 0), stop=(kc == 7)), reads=[r_w, r_h], writes=[C.rpb[4]])
        sp_, r_sp = spr()
        P.op("act", lambda e, sp_=sp_: e.activation(out=sp_[:], in_=C.pb[4][0:1, :], func=AF.Exp, scale=-1.0,
                                                    bias=sc0[0:1, 0:1]), reads=[C.rpb[4], r_sc0], writes=[r_sp])
        P.op("act", lambda e, sp_=sp_: e.activation(out=sp_[:], in_=sp_[:], func=AF.Ln, bias=1.0), reads=[r_sp],
             writes=[r_sp])
        cm, r_cm = cumr()
        pv_ap, r_pv = prev
        P.op("dve", lambda e, cm=cm, sp_=sp_, pv_ap=pv_ap: e.tensor_tensor_scan(
            out=cm[:], data0=one1[:], data1=sp_[:], initial=pv_ap, op0=ALU.mult, op1=ALU.add),
            reads=[r_one1, r_sp, r_pv], writes=[r_cm])
        prev = (cm[:, 511:512], r_cm)
        ag, r_ag = augr()
        rs_, r_rs = rr()
        P.op("dve", lambda e, ag=ag, cm=cm: e.tensor_copy(ag[:, 3, :], cm[:]), reads=[r_cm], writes=[r_ag])
        P.op("dve", lambda e, ag=ag, cm=cm, rs_=rs_: e.tensor_tensor(out=rs_[:, 0, :], in0=cm[:], in1=ag[:, 3, :],
                                                                     op=ALU.subtract), reads=[r_cm, r_ag], writes=[r_rs])
        P.op("dve", lambda e, ag=ag, rs_=rs_: e.tensor_copy(ag[:, 4, :], rs_[:, 0, :]), reads=[r_rs], writes=[r_ag])
        P.op("dve", lambda e, ag=ag, rs_=rs_: e.tensor_tensor(out=rs_[:, 1, :], in0=rs_[:, 0, :], in1=ag[:, 4, :],
                                                              op=ALU.subtract), reads=[r_rs, r_ag], writes=[r_rs])
        P.op("dve", lambda e, ag=ag, rs_=rs_: e.tensor_copy(ag[:, 5, :], rs_[:, 1, :]), reads=[r_rs], writes=[r_ag])
        P.op("dve", lambda e, ag=ag: e.tensor_scalar(out=ag[:, 0:3, :], in0=ag[:, 3:6, :], scalar1=-1.0, scalar2=None,
                                                     op0=ALU.mult), reads=[r_ag], writes=[r_ag])
        for i in range(3):
            P.dma("pool", lambda e, i=i, ag=ag, cols=cols: e.dma_start(out=QT[64 + i:65 + i, cols], in_=ag[0:1, i, :]),
                  reads=[r_ag], writes=[r_QT[tt]], sem=s_augq[tt])
            P.dma("pool", lambda e, i=i, ag=ag, cols=cols: e.dma_start(out=KT[67 + i:68 + i, cols], in_=ag[0:1, 3 + i, :]),
                  reads=[r_ag], writes=[r_KT[tt]], sem=s_augk[tt])
    attn_causal(70)
    store_y(0)

    for tt in range(NT):
        h, r_h = load_hT(tt)
        cols = slice(tt * 512, (tt + 1) * 512)
        proj_fm(h, r_h, C_SQ, 64, QT[0:64, cols], r_QT[tt], 0.125, 5)
        proj_fm(h, r_h, C_SK, 64, KT[0:64, cols], r_KT[tt], 1.0, 6)
        proj_v(h, r_h, C_SV, tt, V[0], r_V[0], 5)
    attn_banded(1, 0, V[0], r_V[0],
                lambda ob, r, n0, lo, hi: finalize(ob, C.pb[ob][0:65, :], [C.rpb[ob]], n0 // 512, True))
    store_y(1)

    ctr = C.ring("ctm", 3, [128, 384], BF16)
    cTr = C.ring("cT", 3, [128, 3, 128], BF16)
    ssr = C.ring("ss2", 3, [128, 2], F32)
    qkf = C.ring("qkf", 3, [128, 2, 32], F32)
    qkb = C.ring("qkb", 3, [128, 2, 96], BF16)
    rtmp = C.ring("rtmp", 4, [128, 4, 16], F32)
    mla_state = {}

    def mla_A(blk):
        tt, sb = blk // 4, blk % 4
        if sb == 0:
            mla_state["h"] = load_hT(tt)
        h, r_h = mla_state["h"]
        bT, bQ, bC, bF = (0, 1)[blk % 2], (2, 3)[blk % 2], (5, 6)[blk % 2], (7, 4)[blk % 2]
        for kc in range(8):
            P.op("pe", lambda e, kc=kc, sb=sb, h=h, bT=bT: e.matmul(
                C.pb[bT][:, 0:416], lhsT=h[:, kc, sb * 128:(sb + 1) * 128], rhs=w_sb[:, kc, C_CQ:C_CQ + 416],
                start=(kc == 0), stop=(kc == 7)), reads=[r_w, r_h], writes=[C.rpb[bT]])
        ss, r_ss = ssr()
        P.op("dve", lambda e, ss=ss: e.memset(ss[:], 0.0), writes=[r_ss])
        P.op("act", lambda e, ss=ss, bT=bT: e.activation(out=C.junk[:, 0:256], in_=C.pb[bT][:, 0:256], func=AF.Square,
                                                  scale=1.0 / 16, accum_out=ss[:, 0:1]),
             reads=[C.rpb[bT], r_ss], writes=[C.r_junk, r_ss])
        P.op("act", lambda e, ss=ss, bT=bT: e.activation(out=C.junk[:, 0:128], in_=C.pb[bT][:, 256:384], func=AF.Square,
                                                  scale=float(128 ** -0.5), accum_out=ss[:, 1:2]),
             reads=[C.rpb[bT], r_ss], writes=[C.r_junk, r_ss])
        P.op("dve", lambda e, ss=ss: e.tensor_scalar(out=ss[:], in0=ss[:], scalar1=EPS, scalar2=None, op0=ALU.add),
             reads=[r_ss], writes=[r_ss])
        P.op("act", lambda e, ss=ss: e.activation(out=ss[:], in_=ss[:], func=AF.Sqrt), reads=[r_ss], writes=[r_ss])
        P.op("dve", lambda e, ss=ss: e.reciprocal(ss[:], ss[:]), reads=[r_ss], writes=[r_ss])
        ct, r_ct = ctr()
        P.op("dve", lambda e, ct=ct, bT=bT: e.tensor_copy(ct[:], C.pb[bT][:, 0:384]), reads=[C.rpb[bT]], writes=[r_ct])
        qf, r_qf = qkf()
        P.op("act", lambda e, qf=qf, bT=bT: e.copy(qf[:, 1, :], C.pb[bT][:, 384:416]), reads=[C.rpb[bT]], writes=[r_qf])
        mla_state[blk] = dict(ss=ss, r_ss=r_ss, ct=ct, r_ct=r_ct, qf=qf, r_qf=r_qf, bQ=bQ, bC=bC, bF=bF)

    def mla_B(blk):
        tt = blk // 4
        st = mla_state[blk]
        ss, r_ss, ct, r_ct, qf, r_qf, bQ, bC, bF = (st[k] for k in ("ss", "r_ss", "ct", "r_ct", "qf", "r_qf", "bQ", "bC", "bF"))
        cT, r_cT = cTr()
        emit_transposes_to(C, ct, r_ct, cT[:], r_cT, 3, bank=bC)
        for c in range(2):
            P.op("pe", lambda e, c=c, cT=cT, bQ=bQ: e.matmul(C.pb[bQ][:, 0:96], lhsT=cT[:, c, :], rhs=wq_s[:, c, :],
                                                      start=(c == 0), stop=(c == 1)), reads=[r_cT, r_wq],
                 writes=[C.rpb[bQ]])
        P.op("pe", lambda e, cT=cT, bQ=bQ: e.matmul(C.pb[bQ][:, 128:256], lhsT=cT[:, 2, :], rhs=wkv_s[:], start=True,
                                             stop=True), reads=[r_cT, r_wkv], writes=[C.rpb[bQ]])
        qb_, r_qb = qkb()
        sq = float(96 ** -0.5)
        P.op("dve", lambda e, qb_=qb_, ss=ss, bQ=bQ: e.tensor_scalar(out=qb_[:, 0, 0:64], in0=C.pb[bQ][:, 0:64],
                                                              scalar1=ss[:, 0:1], scalar2=sq, op0=ALU.mult, op1=ALU.mult),
             reads=[C.rpb[bQ], r_ss], writes=[r_qb])
        P.op("dve", lambda e, qf=qf, ss=ss, bQ=bQ: e.tensor_scalar(out=qf[:, 0, :], in0=C.pb[bQ][:, 64:96],
                                                            scalar1=ss[:, 0:1], scalar2=sq, op0=ALU.mult, op1=ALU.mult),
             reads=[C.rpb[bQ], r_ss, r_qf], writes=[r_qf])
        P.op("dve", lambda e, qb_=qb_, ss=ss, bQ=bQ: e.tensor_scalar(out=qb_[:, 1, 0:64], in0=C.pb[bQ][:, 128:192],
                                                              scalar1=ss[:, 1:2], scalar2=None, op0=ALU.mult),
             reads=[C.rpb[bQ], r_ss, r_qb], writes=[r_qb])
        P.op("dve", lambda e, ss=ss, blk=blk, bQ=bQ: e.tensor_scalar(out=V[0][:, blk, 0:64], in0=C.pb[bQ][:, 192:256],
                                                              scalar1=ss[:, 1:2], scalar2=None, op0=ALU.mult),
             reads=[C.rpb[bQ], r_ss], writes=[r_V[0][tt]])
        cs = cos_t[:, blk * 16:(blk + 1) * 16]
        sn = sin_t[:, blk * 16:(blk + 1) * 16]
        for w_ in range(2):
            tm_, r_tm = rtmp()
            t1 = qf[:, w_, 0:16]
            t2 = qf[:, w_, 16:32]
            eng = "pool" if w_ == 0 else "dve"
            P.op(eng, lambda e, t1=t1, tm_=tm_, cs=cs: e.tensor_tensor(out=tm_[:, 0, :], in0=t1, in1=cs, op=ALU.mult),
                 reads=[r_qf, r_cos], writes=[r_tm])
            P.op(eng, lambda e, t2=t2, tm_=tm_, sn=sn: e.tensor_tensor(out=tm_[:, 1, :], in0=t2, in1=sn, op=ALU.mult),
                 reads=[r_qf, r_sin, r_tm], writes=[r_tm])
            P.op(eng, lambda e, t2=t2, tm_=tm_, cs=cs: e.tensor_tensor(out=tm_[:, 2, :], in0=t2, in1=cs, op=ALU.mult),
                 reads=[r_qf, r_cos, r_tm], writes=[r_tm])
            P.op(eng, lambda e, t1=t1, tm_=tm_, sn=sn: e.tensor_tensor(out=tm_[:, 3, :], in0=t1, in1=sn, op=ALU.mult),
                 reads=[r_qf, r_sin, r_tm], writes=[r_tm])
            P.op(eng, lambda e, w_=w_, tm_=tm_, qb_=qb_: e.tensor_tensor(out=qb_[:, w_, 64:80], in0=tm_[:, 0, :],
                                                                         in1=tm_[:, 1, :], op=ALU.subtract),
                 reads=[r_tm, r_qb], writes=[r_qb])
            P.op(eng, lambda e, w_=w_, tm_=tm_, qb_=qb_: e.tensor_tensor(out=qb_[:, w_, 80:96], in0=tm_[:, 2, :],
                                                                         in1=tm_[:, 3, :], op=ALU.add),
                 reads=[r_tm, r_qb], writes=[r_qb])
        st.update(qb_=qb_, r_qb=r_qb)

    def mla_C(blk):
        tt = blk // 4
        st = mla_state.pop(blk)
        qb_, r_qb, bF = st["qb_"], st["r_qb"], st["bF"]
        for w_, (dst, r_d) in enumerate(((QT, r_QT[tt]), (KT, r_KT[tt]))):
            P.op("pe", lambda e, w_=w_, qb_=qb_, bF=bF: e.transpose(C.psbv[bF][0:96, w_ * 128:(w_ + 1) * 128], qb_[:, w_, :],
                                                             C.ident[:]), reads=[r_qb, C.r_ident], writes=[C.rpb[bF]])
            P.op("act", lambda e, w_=w_, dst=dst, blk=blk, bF=bF: e.copy(dst[0:96, blk * 128:(blk + 1) * 128],
                                                                  C.psbv[bF][0:96, w_ * 128:(w_ + 1) * 128]),
                 reads=[C.rpb[bF]], writes=[r_d])

    for i in range(64 + 2):
        if i < 64:
            mla_A(i)
        if 0 <= i - 1 < 64:
            mla_B(i - 1)
        if 0 <= i - 2 < 64:
            mla_C(i - 2)
    attn_causal(96)
    store_y(2)

    for tt in range(NT):
        h, r_h = load_hT(tt)
        cols = slice(tt * 512, (tt + 1) * 512)
        proj_fm(h, r_h, C_DQ, 64, QT[0:64, cols], r_QT[tt], 0.125, 5)
        proj_fm(h, r_h, C_DK, 64, KT[0:64, cols], r_KT[tt], 1.0, 6)
        proj_v(h, r_h, C_DV, tt, V[0], r_V[0], 5)
    s_v = P.dsem()
    s_vp = {1: P.dsem(), 2: P.dsem()}
    P.dma("sp", lambda e: e.dma_start(out=vscr.rearrange("(b p) d -> p b d", p=128), in_=V[0][:, :, 0:64]),
          reads=r_V[0], writes=[r_vscr], sem=s_v)
    for pi, (win, dil) in enumerate(DIL[1:], start=1):
        L = S // dil
        src = vscr.rearrange("(cc i r) d -> i r cc d", i=128, r=dil)
        dstv = V[pi][:, :, 0:64].rearrange("p (r cc) d -> p r cc d", r=dil)
        for r in range(dil):
            P.dma("sp", lambda e, r=r, src=src, dstv=dstv: e.dma_start(out=dstv[:, r, :, :], in_=src[:, r, :, :]),
                  reads=[r_vscr], writes=r_V[pi], sem=s_vp[pi])

    def evac_dil(first):
        def f(ob, r, n0, lo, hi, first=first):
            raise NotImplementedError
        return f

    for pi, (win, dil) in enumerate(DIL):
        def evac(ob, r, n0, lo, hi, pi=pi, dil=dil):
            dst = acc[0:65, slice(r + dil * n0, r + dil * (n0 + 511) + 1, dil)]
            ra = [r_acc[i] for i in range(lo, hi + 1)]
            if pi == 0:
                P.op("act", lambda e: e.copy(dst, C.pb[ob][0:65, :]), reads=[C.rpb[ob]], writes=ra)
            else:
                P.op("dve", lambda e: e.tensor_tensor(out=dst, in0=dst, in1=C.pb[ob][0:65, :], op=ALU.add),
                     reads=[C.rpb[ob]] + ra, writes=ra)
        attn_banded(dil, 1 + pi, V[pi], r_V[pi], evac)
    for tq in range(NT):
        finalize(None, acc[0:65, tq * 512:(tq + 1) * 512], [r_acc[tq]], tq, False)
    store_y(3)
    return


def build_AB():
    nc = bass.Bass("TRN2", target_bir_lowering=False)
    C = Ctx(nc)
    T = {"hT": dram_in(nc, "hT", [4, D, TSH], BF16), "w_in": dram_in(nc, "w_in", [D, NCOL], F32),
         "wq_up": dram_in(nc, "wq_up", [256, 96], F32), "wkv_up": dram_in(nc, "wkv_up", [128, 128], F32),
         "qn": dram_in(nc, "qn", [256], F32), "kvn": dram_in(nc, "kvn", [128], F32),
         "scal": dram_in(nc, "scal", [1, 8], F32), "pos": dram_in(nc, "pos", [128, 64], I32),
         "cst": dram_in(nc, "cst", [128, 32], F32), "yT": dram_out(nc, "yT", [256, S], BF16)}
    emit_AB(C, T)
    C.P.wait_all("sp", [])
    C.P.emit()
    return nc


def ab_inputs_common(head):
    slopes = 2.0 ** (-8.0 * np.arange(1, 9, dtype=np.float64) / 8.0)
    c = np.zeros((128, 32), np.float32)
    c[:, 0:16] = (10000.0 ** (-np.arange(16, dtype=np.float32) / np.float32(16))).astype(np.float32)[None, :]
    c[:, 16] = -slopes[head]
    for pi, (win, dil) in enumerate(DIL):
        c[:, 17 + pi] = -slopes[4 + head] * dil
    return c


GROUPS = [[0, 1, 2, 3], [4, 5, 6, 7]]


def build_fused():
    nc = bass.Bass("TRN2", target_bir_lowering=False, num_devices=NCORE)
    C = Ctx(nc)
    P = C.P
    x = dram_in(nc, "x", [TSH, D], F32)
    idx = dram_in(nc, "idx", [1, 1], I32)
    pos = dram_in(nc, "pos", [128, 64], I32)
    cst = dram_in(nc, "cst", [128, 32], F32)
    g0 = dram_in(nc, "g0", [D], F32)
    L = []
    for l in range(2):
        L.append({"w_in": dram_in(nc, f"w_in{l}", [D, NCOL], F32), "wq_up": dram_in(nc, f"wq_up{l}", [256, 96], F32),
                  "wkv_up": dram_in(nc, f"wkv_up{l}", [128, 128], F32), "qn": dram_in(nc, f"qn{l}", [256], F32),
                  "kvn": dram_in(nc, f"kvn{l}", [128], F32), "scal": dram_in(nc, f"scal{l}", [1, 8], F32),
                  "w_out": dram_in(nc, f"w_out{l}", [D, D], F32), "g_ffn": dram_in(nc, f"g_ffn{l}", [D], F32),
                  "g_next": dram_in(nc, f"g_next{l}", [D], F32)})
    ffn = [{"wg": dram_in(nc, "wg0", [1, D, FF], F32), "wu": dram_in(nc, "wu0", [1, D, FF], F32),
            "wd": dram_in(nc, "wd0", [1, FF, D], F32)},
           {"wg": dram_in(nc, "wg1", [NE, D, FF], F32), "wu": dram_in(nc, "wu1", [NE, D, FF], F32),
            "wd": dram_in(nc, "wd1", [NE, FF, D], F32), "router": dram_in(nc, "router", [D, NE], F32)}]
    y = dram_out(nc, "y", [TSH, D], F32)
    hT_loc = nc.dram_tensor("hT_loc", [D, TSH], BF16)
    hT_all = nc.dram_tensor("hT_all", [4 * D, TSH], BF16)
    y_loc = nc.dram_tensor("y_loc", [256, S], BF16)
    y_all = nc.dram_tensor("y_all", [1024, S], BF16)
    x_mid = nc.dram_tensor("x_mid", [TSH, D], F32)
    mix_own = nc.dram_tensor("mix_own", [D, TSH], BF16)
    reg_tok = P.es.enter_context(nc.gpsimd.register("rtok"))
    reg_tmp = P.es.enter_context(nc.gpsimd.register("rtmp"))
    idx_sb, r_idx = C.tile([1, 1], I32)
    C.base = C.off
    P.dma("sp", lambda e: e.dma_start(out=idx_sb[:], in_=idx), writes=[r_idx], sem=P.dsem())

    def ld_idx(e):
        e.reg_load(reg_tok, idx_sb[0:1, 0:1])
        return e.reg_mul(reg_tok, reg_tok, TSH)
    P.op("pool", ld_idx, reads=[r_idx])
    dmy, r_dmy = C.tile([128, 16], F32)
    C.base = C.off
    P.op("pool", lambda e: e.memset(dmy[:], 0.0), writes=[r_dmy])
    FSTOP = int(os.environ.get("K_FSTOP", "99"))

    def finish():
        P.wait_all("sp", [])
        P.emit()
        return nc
    ncc = [0]

    ccs = [P.newsem("DCC1"), P.newsem("DCC2")]

    def allgather(src_t, dst_t, nrows, rc):
        P.barrier()
        r_cc = Res("cc")
        for i in range(nrows // rc):
            P.dma("pool", lambda e, i=i: e.collective_compute(
                "AllGather", ALU.bypass, replica_groups=GROUPS, ins=[src_t.ap()[i * rc:(i + 1) * rc, :]],
                outs=[dst_t.ap()[i * 4 * rc:(i + 1) * 4 * rc, :]]), writes=[r_cc], sem=ccs[i % 2], inc=1)
        P.barrier()
        C.reset()

    hT_view = hT_all.ap().rearrange("(kc s p) t -> s p kc t", kc=8, s=4)

    def hT_fn(sh, tl0):
        return hT_view[sh][:, :, tl0:tl0 + 512]

    emit_C(C, {"x": x, "g_next": g0, "hT": hT_loc.ap(), "x_out": None}, "N", False)
    if FSTOP == 0:
        return finish()
    allgather(hT_loc, hT_all, D, 128)
    if FSTOP == 1:
        return finish()
    for l in range(2):
        T = dict(L[l])
        r_ycc = Res("ycc")

        def after_store(mixer, r_st):
            for i in (2 * mixer, 2 * mixer + 1):
                P.dma("pool", lambda e, i=i: e.collective_compute(
                    "AllGather", ALU.bypass, replica_groups=GROUPS, ins=[y_loc.ap()[i * 32:(i + 1) * 32, :]],
                    outs=[y_all.ap()[i * 128:(i + 1) * 128, :]]), reads=[r_st], writes=[r_ycc], sem=ccs[i % 2], inc=1)
        T.update(hT=None, hT_fn=hT_fn, pos=pos, cst=cst, yT=y_loc.ap(), after_store=after_store)
        emit_AB(C, T)
        if FSTOP == 2:
            return finish()
        P.barrier()
        C.reset()
        if FSTOP == 3:
            return finish()
        T = dict(L[l])
        T.update(ffn[l])
        for half in range(2):
            def ld_own(e, half=half):
                e.reg_add(reg_tmp, reg_tok, half * 512 * S)
                src = bass.AP(y_all, reg_tmp, [[S, 512], [1, TSH]])
                return e.dma_start(out=mix_own.ap()[half * 512:(half + 1) * 512, :], in_=src)
            P.dma("pool", ld_own, sem=P.dsem())
        P.barrier()
        if FSTOP == 4:
            return finish()
        T.update(x=(x if l == 0 else x_mid.ap()), mixT=mix_own.ap())
        if l == 0:
            T.update(x_out=x_mid.ap(), hT=hT_loc.ap())
            emit_C(C, T, "dense", False)
            if FSTOP == 5:
                return finish()
            allgather(hT_loc, hT_all, D, 128)
        else:
            T["y"] = y
            emit_C(C, T, "moes", True)
    P.wait_all("sp", [])
    P.emit()
    return nc


_PROGS = {}


def _prog(key, fn):
    if key not in _PROGS:
        _PROGS[key] = fn()
    return _PROGS[key]


def _run(nc, maps):
    res = run_bass_kernel_spmd(nc, maps, core_ids=list(range(NCORE)))
    return res.results


def kernel_unfused(x, positions, attn_norm, w_in, b_forget, mla_q_norm, w_q_up, mla_kv_norm, w_kv_up, sinks, w_out, ffn_norm,
           dense_w_gate, dense_w_up, dense_w_down, router, moe_w_gate, moe_w_up, moe_w_down, final_norm):
    f32 = np.float32
    x = np.asarray(x, f32)
    cores = list(range(NCORE))
    bt = [(c // 4, c % 4) for c in cores]
    maps = [{"x": np.ascontiguousarray(x[b, j * TSH:(j + 1) * TSH]), "g_next": np.asarray(attn_norm[0], f32)}
            for (b, j) in bt]
    r = _run(_prog("N", lambda: build_C("N", False)), maps)
    x_cur = [rr["x_out"] for rr in r]
    hT = [rr["hT"] for rr in r]
    o_fq, o_fk, o_fv, o_ff, o_sq, o_sk, o_sv, o_cq, o_dq, o_dk, o_dv = 0, 256, 512, 768, 772, 1028, 1156, 1284, 1700, 1956, 2212
    pos = np.asarray(positions, np.int32)
    out = None
    for layer in range(2):
        wl = np.asarray(w_in[layer], f32)
        maps = []
        for (b, j) in bt:
            kv = j // 2
            cols = np.concatenate([
                np.arange(o_fq + j * 64, o_fq + (j + 1) * 64), np.arange(o_fk + j * 64, o_fk + (j + 1) * 64),
                np.arange(o_fv + j * 64, o_fv + (j + 1) * 64), np.arange(o_ff + j, o_ff + j + 1),
                np.arange(o_sq + j * 64, o_sq + (j + 1) * 64), np.arange(o_sk + kv * 64, o_sk + (kv + 1) * 64),
                np.arange(o_sv + kv * 64, o_sv + (kv + 1) * 64), np.arange(o_cq, o_cq + 416),
                np.arange(o_dq + j * 64, o_dq + (j + 1) * 64), np.arange(o_dk + j * 64, o_dk + (j + 1) * 64),
                np.arange(o_dv + j * 64, o_dv + (j + 1) * 64)])
            scal = np.zeros((1, 8), f32)
            scal[0, 0] = b_forget[layer][j]
            scal[0, 1] = sinks[layer][j]
            maps.append({
                "hT": np.ascontiguousarray(np.stack([hT[b * 4 + s] for s in range(4)], 0)),
                "w_in": np.ascontiguousarray(wl[:, cols]),
                "wq_up": np.ascontiguousarray(np.asarray(w_q_up[layer], f32)[:, j * 96:(j + 1) * 96]),
                "wkv_up": np.ascontiguousarray(np.asarray(w_kv_up[layer], f32)[:, j * 128:(j + 1) * 128]),
                "qn": np.asarray(mla_q_norm[layer], f32), "kvn": np.asarray(mla_kv_norm[layer], f32),
                "scal": scal, "pos": np.ascontiguousarray(pos[b].reshape(64, 128).T),
                "cst": ab_inputs_common(j)})
        r = _run(_prog("AB", build_AB), maps)
        yT = [rr["yT"] for rr in r]
        perm = np.array([m * 256 + j * 64 + d for j in range(4) for m in range(4) for d in range(64)])
        wo = np.ascontiguousarray(np.asarray(w_out[layer], f32)[perm])
        last = layer == 1
        maps = []
        for (b, j) in bt:
            mixT = np.ascontiguousarray(np.concatenate([yT[b * 4 + jj][:, j * TSH:(j + 1) * TSH] for jj in range(4)], 0))
            m = {"x": x_cur[b * 4 + j], "mixT": mixT, "w_out": wo, "g_ffn": np.asarray(ffn_norm[layer], f32),
                 "g_next": np.asarray(final_norm if last else attn_norm[layer + 1], f32)}
            if layer == 0:
                m.update(wg=np.asarray(dense_w_gate, f32), wu=np.asarray(dense_w_up, f32), wd=np.asarray(dense_w_down, f32))
            else:
                m.update(wg=np.asarray(moe_w_gate[0], f32), wu=np.asarray(moe_w_up[0], f32),
                         wd=np.asarray(moe_w_down[0], f32), router=np.asarray(router[0], f32))
            maps.append(m)
        if layer == 0:
            r = _run(_prog("Cd", lambda: build_C("dense", False)), maps)
            x_cur = [rr["x_out"] for rr in r]
            hT = [rr["hT"] for rr in r]
        else:
            r = _run(_prog("Cm", lambda: build_C("moe", True)), maps)
            out = np.zeros((NB, S, D), f32)
            for (b, j), rr in zip(bt, r):
                out[b, j * TSH:(j + 1) * TSH] = rr["y"]
    return out


def _core_cols(j):
    o_fq, o_fk, o_fv, o_ff, o_sq, o_sk, o_sv, o_cq, o_dq, o_dk, o_dv = 0, 256, 512, 768, 772, 1028, 1156, 1284, 1700, 1956, 2212
    kv = j // 2
    return np.concatenate([
        np.arange(o_fq + j * 64, o_fq + (j + 1) * 64), np.arange(o_fk + j * 64, o_fk + (j + 1) * 64),
        np.arange(o_fv + j * 64, o_fv + (j + 1) * 64), np.arange(o_ff + j, o_ff + j + 1),
        np.arange(o_sq + j * 64, o_sq + (j + 1) * 64), np.arange(o_sk + kv * 64, o_sk + (kv + 1) * 64),
        np.arange(o_sv + kv * 64, o_sv + (kv + 1) * 64), np.arange(o_cq, o_cq + 416),
        np.arange(o_dq + j * 64, o_dq + (j + 1) * 64), np.arange(o_dk + j * 64, o_dk + (j + 1) * 64),
        np.arange(o_dv + j * 64, o_dv + (j + 1) * 64)])


def kernel(x, positions, attn_norm, w_in, b_forget, mla_q_norm, w_q_up, mla_kv_norm, w_kv_up, sinks, w_out, ffn_norm,
           dense_w_gate, dense_w_up, dense_w_down, router, moe_w_gate, moe_w_up, moe_w_down, final_norm):
    f32 = np.float32
    x = np.asarray(x, f32)
    pos = np.asarray(positions, np.int32)
    perm = np.array([((c8 * 32 + r) // 64) * 256 + j * 64 + (c8 * 32 + r) % 64
                     for c8 in range(8) for j in range(4) for r in range(32)])
    wo = [np.ascontiguousarray(np.asarray(w_out[l], f32)[perm]) for l in range(2)]
    shared = {"g0": np.asarray(attn_norm[0], f32),
              "wg0": np.asarray(dense_w_gate, f32), "wu0": np.asarray(dense_w_up, f32), "wd0": np.asarray(dense_w_down, f32),
              "wg1": np.asarray(moe_w_gate[0], f32), "wu1": np.asarray(moe_w_up[0], f32), "wd1": np.asarray(moe_w_down[0], f32),
              "router": np.asarray(router[0], f32)}
    for l in range(2):
        shared[f"qn{l}"] = np.asarray(mla_q_norm[l], f32)
        shared[f"kvn{l}"] = np.asarray(mla_kv_norm[l], f32)
        shared[f"w_out{l}"] = wo[l]
        shared[f"g_ffn{l}"] = np.asarray(ffn_norm[l], f32)
        shared[f"g_next{l}"] = np.asarray(attn_norm[1] if l == 0 else final_norm, f32)
    maps = []
    for c in range(NCORE):
        b, j = c // 4, c % 4
        m = dict(shared)
        m["x"] = np.ascontiguousarray(x[b, j * TSH:(j + 1) * TSH])
        m["idx"] = np.array([[j]], np.int32)
        m["pos"] = np.ascontiguousarray(pos[b].reshape(64, 128).T)
        m["cst"] = ab_inputs_common(j)
        cols = _core_cols(j)
        for l in range(2):
            m[f"w_in{l}"] = np.ascontiguousarray(np.asarray(w_in[l], f32)[:, cols])
            m[f"wq_up{l}"] = np.ascontiguousarray(np.asarray(w_q_up[l], f32)[:, j * 96:(j + 1) * 96])
            m[f"wkv_up{l}"] = np.ascontiguousarray(np.asarray(w_kv_up[l], f32)[:, j * 128:(j + 1) * 128])
            sc = np.zeros((1, 8), f32)
            sc[0, 0] = b_forget[l][j]
            sc[0, 1] = sinks[l][j]
            m[f"scal{l}"] = sc
        maps.append(m)
    r = _run(_prog("fused", build_fused), maps)
    out = np.zeros((NB, S, D), f32)
    for c in range(NCORE):
        out[c // 4, (c % 4) * TSH:(c % 4 + 1) * TSH] = r[c]["y"]
    return out
```

```python
import contextlib
import os
import numpy as np
import ml_dtypes
import concourse.bass as bass
import concourse.mybir as mybir
from concourse.bass_utils import run_bass_kernel_spmd

F32 = mybir.dt.float32
BF16 = mybir.dt.bfloat16
I32 = mybir.dt.int32
AF = mybir.ActivationFunctionType
ALU = mybir.AluOpType
AX = mybir.AxisListType

D = 1024
S = 8192
NB = 2
NCORE = 8
TSH = 2048
FF = 3584
NFC = FF // 128
NE = 8
EPS = 1e-6
NCOL = 993
C_FQ, C_FK, C_FV, C_FF = 0, 64, 128, 192
C_SQ, C_SK, C_SV = 193, 257, 321
C_CQ = 385
C_DQ, C_DK, C_DV = 801, 865, 929
DIL = ((128, 1), (512, 4), (2048, 16))
PI = float(np.pi)
DBG2 = int(os.environ.get('K_DBG2', '0'))


class Res:
    __slots__ = ("name", "writer", "readers")

    def __init__(self, name):
        self.name = name
        self.writer = None
        self.readers = {}


class Prog:
    ENGS = ("pe", "act", "dve", "pool", "sp")

    def __init__(self, nc):
        self.nc = nc
        self.es = contextlib.ExitStack()
        self.streams = {e: [] for e in self.ENGS}
        self.count = {}
        self.sem = {}
        self.waited = {e: {} for e in self.ENGS}
        for e in self.ENGS:
            self.newsem("E_" + e)
        self.n_ops = 0
        self.nds = 0

    def newsem(self, name):
        self.sem[name] = self.es.enter_context(self.nc.semaphore(name))
        self.count[name] = 0
        return name

    def dsem(self):
        if getattr(self, "free_d", None):
            return self.free_d.pop()
        self.nds += 1
        return self.newsem(f"D{self.nds}")

    def cond_begin(self, flag_ap, r_flag):
        if not hasattr(self, "flag_regs"):
            nc = self.nc
            engs = {"pe": nc.tensor, "act": nc.scalar, "dve": nc.vector, "pool": nc.gpsimd, "sp": nc.sync}
            self.flag_regs = {k: self.es.enter_context(v.register(f"flag_{k}")) for k, v in engs.items()}
        self._cond_snap = {e: dict(self.waited[e]) for e in self.ENGS}
        for e in self.ENGS:
            if r_flag.writer:
                self._waits(e, {r_flag.writer})
            self.streams[e].append(("if", flag_ap))

    def cond_end(self):
        for e in self.ENGS:
            self.streams[e].append(("endif",))
            self.waited[e] = self._cond_snap[e]

    def barrier(self):
        toks = set((k, v) for k, v in self.count.items() if v > 0)
        for e in self.ENGS:
            self._waits(e, toks)
        self.free_d = sorted([k for k in self.count if k.startswith("D") and not k.startswith("DCC")], reverse=True)

    def sbuf(self, name, shape, dtype):
        return self.es.enter_context(self.nc.sbuf_tensor(name, list(shape), dtype))

    def psum(self, name, shape, dtype):
        return self.es.enter_context(self.nc.psum_tensor(name, list(shape), dtype))

    def _waits(self, eng, toks):
        for (s, v) in sorted(toks):
            if s == "E_pe" and eng == "pe":
                continue
            if self.waited[eng].get(s, 0) >= v:
                continue
            self.waited[eng][s] = v
            self.streams[eng].append(("wait", s, v))

    def _deps(self, reads, writes, own=None):
        toks = set()
        for r in reads:
            if r.writer:
                toks.add(r.writer)
        for w in writes:
            if w.writer and w.writer[0] != own:
                toks.add(w.writer)
            for t in w.readers.values():
                toks.add(t)
        return toks

    def op(self, eng, fn, reads=(), writes=(), signal=True):
        self._waits(eng, self._deps(reads, writes))
        s = "E_" + eng
        self.count[s] += 1
        tok = (s, self.count[s])
        self.streams[eng].append(("op", fn, s, self.count[s]))
        for r in reads:
            r.readers[s] = tok
        for w in writes:
            w.writer = tok
            w.readers = {}
        self.n_ops += 1

    def dma(self, q, fn, reads=(), writes=(), sem=None, inc=16):
        self._waits(q, self._deps(reads, writes, own=sem))
        self.count[sem] += inc
        tok = (sem, self.count[sem])
        self.streams[q].append(("op", fn, sem, inc, self.count[sem]))
        for r in reads:
            r.readers[sem] = tok
        for w in writes:
            w.writer = tok
            w.readers = {}
        self.n_ops += 1

    def wait_all(self, eng, resources):
        toks = set()
        for r in resources:
            if r.writer:
                toks.add(r.writer)
        self._waits(eng, toks)
        self._waits(eng, set((k, v) for k, v in self.count.items() if k.startswith("D") and v > 0))

    def emit(self):
        nc = self.nc
        streams = self.streams
        sem = self.sem

        import bisect
        needed = {}
        for lst in streams.values():
            for it in lst:
                if it[0] == "wait" and it[1].startswith("E_"):
                    needed.setdefault(it[1], set()).add(it[2])
        order = {k: sorted(v) for k, v in needed.items()}

        def phys(sname, v):
            return bisect.bisect_right(order[sname], v)

        def run_items(e, ename, lst):
            i = 0
            while i < len(lst):
                it = lst[i]
                if it[0] == "wait":
                    if it[1].startswith("E_"):
                        e.wait_ge(sem[it[1]], phys(it[1], it[2]))
                    else:
                        e.wait_ge(sem[it[1]], it[2])
                elif it[0] == "if":
                    depth, j = 1, i + 1
                    while depth:
                        if lst[j][0] == "if":
                            depth += 1
                        elif lst[j][0] == "endif":
                            depth -= 1
                        j += 1
                    body = lst[i + 1:j - 1]
                    incs = {}
                    before = {}
                    for b in body:
                        if b[0] == "op":
                            if b[2].startswith("E_"):
                                if b[3] in needed.get(b[2], ()):
                                    incs[b[2]] = incs.get(b[2], 0) + 1
                                    if b[2] not in before:
                                        before[b[2]] = bisect.bisect_left(order[b[2]], b[3])
                            else:
                                incs[b[2]] = incs.get(b[2], 0) + b[3]
                                if b[2] not in before:
                                    before[b[2]] = b[4] - b[3]
                    if any(b[0] == "op" for b in body):
                        reg = self.flag_regs[ename]
                        e.reg_load(reg, it[1])
                        with e.If(reg):
                            run_items(e, ename, body)
                        if incs:
                            with e.Else():
                                for k in sorted(incs):
                                    if before[k] > 0:
                                        e.wait_ge(sem[k], before[k])
                                for k in sorted(incs):
                                    e.sem_inc(sem[k], incs[k])
                    i = j - 1
                elif it[0] == "op":
                    ins = it[1](e)
                    if it[2].startswith("E_"):
                        if it[3] in needed.get(it[2], ()):
                            ins.then_inc(sem[it[2]], 1)
                    else:
                        ins.then_inc(sem[it[2]], it[3])
                i += 1

        def replay(e, lst):
            run_items(e, self._cur, lst)

        with nc.Block() as block:
            @block.tensor
            def _(e):
                self._cur = "pe"
                replay(e, streams["pe"])

            @block.scalar
            def _(e):
                self._cur = "act"
                replay(e, streams["act"])

            @block.vector
            def _(e):
                self._cur = "dve"
                replay(e, streams["dve"])

            @block.gpsimd
            def _(e):
                self._cur = "pool"
                replay(e, streams["pool"])

            @block.sync
            def _(e):
                self._cur = "sp"
                replay(e, streams["sp"])
        self.es.close()


ARENA_BYTES = 212736
_ESZ = {F32: 4, BF16: 2, I32: 4}


class Ctx:
    def __init__(self, nc):
        self.nc = nc
        self.P = Prog(nc)
        P = self.P
        self.pb = [P.psum(f"pb{i}", [128, 512], F32) for i in range(8)]
        self.rpb = [Res(f"pb{i}") for i in range(8)]
        self.psb = self.pb[7][:].bitcast(BF16)
        self.psbv = [self.pb[i][:].bitcast(BF16) for i in range(8)]
        self.nt = 0
        self.arena = P.sbuf("arena", [128, ARENA_BYTES // 2], BF16)
        self.off = 0
        self.uid = 0
        self.ident, self.r_ident = self.tile([128, 128], BF16)
        self.junk, self.r_junk = self.tile([128, 1024], BF16)
        self.base = self.off
        idf, r_idf = self.tile([128, 128], F32)
        P.op("pool", lambda e: e.memset(idf[:], 1.0), writes=[r_idf])
        P.op("pool", lambda e: e.affine_select(out=idf[:], in_=idf[:], pattern=[[-1, 128]], compare_op=ALU.is_equal,
                                               fill=0.0, base=0, channel_multiplier=1), reads=[r_idf], writes=[r_idf])
        P.op("dve", lambda e: e.tensor_copy(self.ident[:], idf[:]), reads=[r_idf], writes=[self.r_ident])
        P.barrier()
        self.off = self.base

    def reset(self):
        self.off = self.base

    def tile(self, shape, dt, name=None):
        self.nt += 1
        nm = f"{name or 't'}{self.nt}"
        n = 1
        for d_ in shape[1:]:
            n *= d_
        nbytes = n * _ESZ[dt]
        off = (self.off + 63) // 64 * 64
        self.off = off + nbytes
        assert self.off <= ARENA_BYTES, f"SBUF arena overflow allocating {nm} {shape}: {self.off}"
        ap = self.arena[0:shape[0], off // 2:(off + nbytes) // 2]
        if dt != BF16:
            ap = ap.bitcast(dt)
        if len(shape) == 3:
            ap = ap.rearrange("p (a b) -> p a b", a=shape[1])
        elif len(shape) == 4:
            ap = ap.rearrange("p (a b c) -> p a b c", a=shape[1], b=shape[2])
        return ap, Res(nm)

    def ring(self, key, n, shape, dt, with_sem=False):
        bufs = []
        for i in range(n):
            t, r = self.tile(shape, dt, name=key)
            bufs.append((t, r, self.P.dsem()) if with_sem else (t, r))
        state = {"i": 0}

        def nxt():
            b = bufs[state["i"] % n]
            state["i"] += 1
            return b
        return nxt


def dram_in(nc, name, shape, dt):
    return nc.dram_tensor(name, list(shape), dt, kind="ExternalInput").ap()


def dram_out(nc, name, shape, dt):
    return nc.dram_tensor(name, list(shape), dt, kind="ExternalOutput").ap()


def emit_rstd(C, x_ap, r_x, ss, r_ss, n, width_ap=None):
    P = C.P
    P.op("dve", lambda e: e.memset(ss, 0.0), writes=[r_ss])
    P.op("act", lambda e: e.activation(out=C.junk[:, 0:n], in_=x_ap, func=AF.Square, accum_out=ss),
         reads=[r_x, r_ss], writes=[C.r_junk, r_ss])
    P.op("dve", lambda e: e.tensor_scalar(out=ss, in0=ss, scalar1=1.0 / n, scalar2=EPS, op0=ALU.mult, op1=ALU.add),
         reads=[r_ss], writes=[r_ss])
    P.op("act", lambda e: e.activation(out=ss, in_=ss, func=AF.Sqrt), reads=[r_ss], writes=[r_ss])
    P.op("dve", lambda e: e.reciprocal(ss, ss), reads=[r_ss], writes=[r_ss])


def emit_transposes_to(C, hb, r_hb, dst_ap, r_dst, nchunk, eng="act", bank=7):
    P = C.P
    psb = C.psbv[bank]
    for k in range(nchunk):
        P.op("pe", lambda e, k=k: e.transpose(psb[:, k * 128:(k + 1) * 128], hb[:, k * 128:(k + 1) * 128], C.ident[:]),
             reads=[r_hb, C.r_ident], writes=[C.rpb[bank]], signal=(k == nchunk - 1))
    src = psb[:, 0:nchunk * 128].rearrange("p (k t) -> p k t", k=nchunk)
    if eng == "act":
        P.op("act", lambda e: e.copy(dst_ap, src), reads=[C.rpb[bank]], writes=[r_dst])
    else:
        P.op("dve", lambda e: e.tensor_copy(dst_ap, src), reads=[C.rpb[bank]], writes=[r_dst])


def emit_C(C, T, mode, last):
    nc = C.nc
    P = C.P
    x_in = T["x"]
    g_next = T["g_next"]
    full = mode != "N"
    sparse = mode == "moes"
    if sparse:
        mode = "moe"
    ne = NE if mode == "moe" else 1
    if full:
        mixT = T.get("mixT")
        w_out = T["w_out"]
        g_ffn = T["g_ffn"]
        wg, wu, wd = T["wg"], T["wu"], T["wd"]
        if mode == "moe":
            router = T["router"]
    if last:
        y_out = T["y"]
    else:
        x_out = T.get("x_out")
        hT_out = T["hT"]
    d_out = Res("d_out")
    x_blk = x_in.rearrange("(b p) d -> p b d", p=128)
    NTB = TSH // 128

    NX = {}

    def alloc_next():
        gbn_, r_gbn_ = C.tile([128, D], F32)
        P.dma("sp", lambda e: e.dma_start(out=gbn_[:], in_=g_next.partition_broadcast(128)), writes=[r_gbn_],
              sem=P.dsem())
        NX["gbn"], NX["r_gbn"] = gbn_, r_gbn_
        if last:
            NX["outr"] = C.ring("yo", 2, [128, D], F32, with_sem=True)
    defer_next = (mode == "moes") and last
    if not defer_next:
        alloc_next()
    ssr = C.ring("ss", 3, [128, 1], F32)
    hbr = None if (mode == "moes") else C.ring("hb", 3, [128, D], BF16)
    stg = None if last else C.ring("stg", 2, [128, 8, 128], BF16, with_sem=True)
    hT_dram = None if last else hT_out.rearrange("(kc p) t -> p kc t", p=128)

    def emit_next(xa, r_xa, tb):
        ss, r_ss = ssr()
        emit_rstd(C, xa, r_xa, ss[:], r_ss, D)
        if last:
            yo, r_yo, s_yo = NX["outr"]()
            P.op("dve", lambda e: e.scalar_tensor_tensor(out=yo[:], in0=xa, scalar=ss[:], in1=NX["gbn"][:], op0=ALU.mult,
                                                          op1=ALU.mult), reads=[r_xa, r_ss, NX["r_gbn"]], writes=[r_yo])
            P.dma("sp", lambda e: e.dma_start(out=y_out[tb * 128:(tb + 1) * 128, :], in_=yo[:]), reads=[r_yo],
                  writes=[d_out], sem=s_yo)
        else:
            hb, r_hb = hbr()
            P.op("dve", lambda e: e.scalar_tensor_tensor(out=hb[:], in0=xa, scalar=ss[:], in1=NX["gbn"][:], op0=ALU.mult,
                                                          op1=ALU.mult), reads=[r_xa, r_ss, NX["r_gbn"]], writes=[r_hb])
            st, r_st, s_st = stg()
            emit_transposes_to(C, hb, r_hb, st[:], r_st, 8)
            P.dma("sp", lambda e: e.dma_start(out=hT_dram[:, :, tb * 128:(tb + 1) * 128], in_=st[:]), reads=[r_st],
                  writes=[d_out], sem=s_st)

    if not full:
        xr = C.ring("xs", 3, [128, D], F32, with_sem=True)
        for tb in range(NTB):
            xt, r_xt, s_xt = xr()
            P.dma("sp", lambda e, tb=tb, xt=xt: e.dma_start(out=xt[:], in_=x_blk[:, tb, :]), writes=[r_xt], sem=s_xt)
            emit_next(xt[:], r_xt, tb)
            if x_out is not None:
                P.dma("sp", lambda e, tb=tb, xt=xt: e.dma_start(out=x_out[tb * 128:(tb + 1) * 128, :], in_=xt[:]),
                      reads=[r_xt], writes=[d_out], sem=s_xt)
        return

    C.uid += 1
    x1s = nc.dram_tensor(f"x1s{C.uid}", [TSH, D], F32).ap()
    r_x1s = [Res(f"x1s{i}") for i in range(NTB)]
    if sparse:
        h_tm, _ = C.tile([128, NTB, D], BF16)
        r_htm = [Res(f"htm{i}") for i in range(NTB)]
        comb, _ = C.tile([128, NTB, NE], F32)
        r_comb = [Res(f"comb{i}") for i in range(NTB)]
        rkm, r_rkm = C.tile([128, NTB, NE], F32)
        chh, r_chh = C.tile([128, NTB, NE], BF16)
        cll, r_cll = C.tile([128, NTB, NE], BF16)
        io_f, r_iof = C.tile([128, 128], BF16)
        flags_i, r_flags = C.tile([1, 32], I32)
        C.mark = C.off
    gbf, r_gbf = C.tile([128, D], F32)
    P.dma("sp", lambda e: e.dma_start(out=gbf[:], in_=g_ffn.partition_broadcast(128)), writes=[r_gbf], sem=P.dsem())
    wo, r_wo = C.tile([128, 8, D], BF16)
    s_wo = P.dsem()
    P.dma("pool", lambda e: e.dma_start(out=wo[:], in_=w_out.rearrange("(kc p) n -> p kc n", p=128)), writes=[r_wo],
          sem=s_wo)
    if not sparse:
        hT, _ = C.tile([128, 8, TSH], BF16)
        r_hT = [Res(f"hT{i}") for i in range(NTB)]
    else:
        hir = C.ring("hiT", 2, [128, 8, 128], BF16)
    if mode == "moe":
        rt_f, r_rtf = C.tile([128, 8, NE], F32)
        rt_hi, r_rthi = C.tile([128, 8, NE], BF16)
        rt_lo, r_rtlo = C.tile([128, 8, NE], BF16)
        P.dma("sp", lambda e: e.dma_start(out=rt_f[:], in_=router.rearrange("(kc p) n -> p kc n", p=128)),
              writes=[r_rtf], sem=P.dsem())
        P.op("dve", lambda e: e.tensor_copy(rt_hi[:], rt_f[:]), reads=[r_rtf], writes=[r_rthi])
        P.op("dve", lambda e: e.tensor_tensor(out=rt_lo[:], in0=rt_f[:], in1=rt_hi[:], op=ALU.subtract),
             reads=[r_rtf, r_rthi], writes=[r_rtlo])
        if not sparse:
            comb, _ = C.tile([128, NTB, NE], F32)
            r_comb = [Res(f"comb{i}") for i in range(NTB)]
        hfr = C.ring("hf", 1, [128, D], F32)
        hlr = C.ring("hl", 2, [128, D], BF16)
        lor = C.ring("loT", 1, [128, 8, 128], BF16)
        m8r = C.ring("m8", 2, [128, 8], F32)
        smr = C.ring("sm", 2, [128, 8], F32)

    xr = C.ring("xs", 2, [128, D], F32, with_sem=True)
    mxr = C.ring("mx", 2, [128, 8, 128], BF16, with_sem=True)
    mix_v = None if mixT is None else mixT.rearrange("(kc p) t -> p kc t", p=128)

    st1 = {}

    def stage1_a(tb):
        xt, r_xt, s_xt = xr()
        P.dma("sp", lambda e, tb=tb, xt=xt: e.dma_start(out=xt[:], in_=x_blk[:, tb, :]), writes=[r_xt], sem=s_xt)
        mx, r_mx, s_mx = mxr()
        if mix_v is not None:
            P.dma("sp", lambda e, tb=tb, mx=mx: e.dma_start(out=mx[:], in_=mix_v[:, :, tb * 128:(tb + 1) * 128]),
                  writes=[r_mx], sem=s_mx)
        else:
            def ld_mix(e, tb=tb, mx=mx):
                e.reg_add(T["reg_tmp"], T["reg_tok"], tb * 128)
                src = bass.AP(T["mix_all"], T["reg_tmp"], [[S, 128], [128 * S, 8], [1, 128]])
                return e.dma_start(out=mx[:], in_=src)
            P.dma("pool", ld_mix, reads=T["mix_reads"], writes=[r_mx], sem=s_mx)
        for hf_ in range(2):
            pbk = tb % 2 * 2 + hf_
            for kc in range(8):
                P.op("pe", lambda e, kc=kc, hf_=hf_, pbk=pbk, mx=mx: e.matmul(
                    C.pb[pbk][:, :], lhsT=mx[:, kc, :], rhs=wo[:, kc, hf_ * 512:(hf_ + 1) * 512],
                    start=(kc == 0), stop=(kc == 7)), reads=[r_mx, r_wo], writes=[C.rpb[pbk]], signal=(kc == 7))
            P.op("dve", lambda e, hf_=hf_, pbk=pbk, xt=xt: e.tensor_tensor(
                out=xt[:, hf_ * 512:(hf_ + 1) * 512], in0=xt[:, hf_ * 512:(hf_ + 1) * 512], in1=C.pb[pbk][:, :],
                op=ALU.add), reads=[r_xt, C.rpb[pbk]], writes=[r_xt])
        P.dma("sp", lambda e, tb=tb, xt=xt: e.dma_start(out=x1s[tb * 128:(tb + 1) * 128, :], in_=xt[:]),
              reads=[r_xt], writes=[r_x1s[tb]], sem=s_xt)
        ss, r_ss = ssr()
        emit_rstd(C, xt[:], r_xt, ss[:], r_ss, D)
        hb, r_hb = hbr() if not sparse else (h_tm[:, tb, :], r_htm[tb])
        if mode != "moe":
            P.op("dve", lambda e, xt=xt, ss=ss, hb=hb: e.scalar_tensor_tensor(
                out=hb[:], in0=xt[:], scalar=ss[:], in1=gbf[:], op0=ALU.mult, op1=ALU.mult),
                reads=[r_xt, r_ss, r_gbf], writes=[r_hb])
            st1[tb] = (hb, r_hb, None, None)
        else:
            hf, r_hf = hfr()
            hl, r_hl = hlr()
            P.op("dve", lambda e, xt=xt, ss=ss, hf=hf: e.scalar_tensor_tensor(
                out=hf[:], in0=xt[:], scalar=ss[:], in1=gbf[:], op0=ALU.mult, op1=ALU.mult),
                reads=[r_xt, r_ss, r_gbf], writes=[r_hf])
            P.op("dve", lambda e, hf=hf, hb=hb: e.tensor_copy(hb[:], hf[:]), reads=[r_hf], writes=[r_hb])
            P.op("dve", lambda e, hf=hf, hb=hb, hl=hl: e.tensor_tensor(out=hl[:], in0=hf[:], in1=hb[:], op=ALU.subtract),
                 reads=[r_hf, r_hb], writes=[r_hl])
            st1[tb] = (hb, r_hb, hl, r_hl)

    def stage1_b(tb):
        hb, r_hb, hl, r_hl = st1.pop(tb)
        if sparse:
            hi_t, r_hi = hir()
            emit_transposes_to(C, hb, r_hb, hi_t[:], r_hi, 8)
        else:
            emit_transposes_to(C, hb, r_hb, hT[:, :, tb * 128:(tb + 1) * 128], r_hT[tb], 8)
            hi_t, r_hi = hT[:, :, tb * 128:(tb + 1) * 128], r_hT[tb]
        if mode == "moe":
            lo, r_lo = lor()
            emit_transposes_to(C, hl, r_hl, lo[:], r_lo, 8, eng="dve")
            n = 0
            for kc in range(8):
                for (a, ra, b_, rb) in ((hi_t[:, kc, :], r_hi, rt_hi, r_rthi),
                                        (lo[:, kc, :], r_lo, rt_hi, r_rthi),
                                        (hi_t[:, kc, :], r_hi, rt_lo, r_rtlo)):
                    P.op("pe", lambda e, a=a, b_=b_, kc=kc, n=n: e.matmul(
                        C.pb[4][:, 0:NE], lhsT=a, rhs=b_[:, kc, :], start=(n == 0), stop=(n == 23)),
                        reads=[ra, rb], writes=[C.rpb[4]], signal=(n == 23))
                    n += 1
            lg, r_lg = smr()
            m8, r_m8 = m8r()
            cb = comb[:, tb, :]
            P.op("act", lambda e, lg=lg: e.copy(lg[:], C.pb[4][:, 0:NE]), reads=[C.rpb[4]], writes=[r_lg])
            P.op("dve", lambda e, lg=lg, m8=m8: e.max(out=m8[:], in_=lg[:]), reads=[r_lg], writes=[r_m8])
            P.op("dve", lambda e, lg=lg, m8=m8, cb=cb: e.tensor_scalar(
                out=cb, in0=lg[:], scalar1=m8[:, 1:2], scalar2=None, op0=ALU.is_ge),
                reads=[r_lg, r_m8], writes=[r_comb[tb]])
            P.op("dve", lambda e, lg=lg, m8=m8: e.tensor_scalar(
                out=lg[:], in0=lg[:], scalar1=m8[:, 0:1], scalar2=None, op0=ALU.subtract),
                reads=[r_lg, r_m8], writes=[r_lg])
            P.op("act", lambda e, lg=lg: e.activation(out=lg[:], in_=lg[:], func=AF.Exp), reads=[r_lg], writes=[r_lg])
            P.op("dve", lambda e, lg=lg, cb=cb: e.tensor_tensor(out=cb, in0=cb, in1=lg[:], op=ALU.mult),
                 reads=[r_lg, r_comb[tb]], writes=[r_comb[tb]])
            P.op("dve", lambda e, m8=m8, cb=cb: e.tensor_reduce(out=m8[:, 2:3], in_=cb, axis=AX.X, op=ALU.add),
                 reads=[r_comb[tb], r_m8], writes=[r_m8])
            P.op("dve", lambda e, m8=m8: e.reciprocal(m8[:, 2:3], m8[:, 2:3]), reads=[r_m8], writes=[r_m8])
            P.op("dve", lambda e, m8=m8, cb=cb: e.tensor_scalar(
                out=cb, in0=cb, scalar1=m8[:, 2:3], scalar2=None, op0=ALU.mult),
                reads=[r_comb[tb], r_m8], writes=[r_comb[tb]])


    for i in range(NTB + 1):
        if i < NTB:
            stage1_a(i)
        if i >= 1:
            stage1_b(i - 1)

    if sparse:
        cflat = comb[:].rearrange("p t e -> p (t e)")
        rkf = rkm[:].rearrange("p t e -> p (t e)")
        maskf, r_maskf = C.tile([128, 128], F32)
        maskb, r_maskb = C.tile([128, 128], BF16)
        Uf, r_Uf = C.tile([128, 128], F32)
        Ub, r_Ub = C.tile([128, 128], BF16)
        oneb, r_oneb = C.tile([128, 128], BF16)
        tot, r_tot = C.tile([128, NTB, NE], F32)
        off, r_off = C.tile([128, NTB, NE], F32)
        tmpm, r_tmpm = C.tile([128, 128], F32)
        ne_t, r_net = C.tile([128, NE], F32)
        flagf, r_flagf = C.tile([1, 32], F32)
        io_i, r_ioi = C.tile([128, 128], I32)
        P.op("dve", lambda e: e.tensor_scalar(out=maskf[:], in0=cflat, scalar1=0.0, scalar2=None, op0=ALU.is_gt),
             reads=r_comb, writes=[r_maskf])
        P.op("dve", lambda e: e.tensor_copy(maskb[:], maskf[:]), reads=[r_maskf], writes=[r_maskb])
        P.op("pool", lambda e: e.memset(Uf[:], 1.0), writes=[r_Uf])
        P.op("pool", lambda e: e.affine_select(out=Uf[:], in_=Uf[:], pattern=[[1, 128]], compare_op=ALU.is_ge, fill=0.0,
                                               base=-1, channel_multiplier=-1), reads=[r_Uf], writes=[r_Uf])
        P.op("dve", lambda e: e.tensor_copy(Ub[:], Uf[:]), reads=[r_Uf], writes=[r_Ub])
        P.op("dve", lambda e: e.memset(oneb[:], 1.0), writes=[r_oneb])
        P.op("pe", lambda e: e.matmul(C.pb[0][:, 0:128], lhsT=Ub[:], rhs=maskb[:], start=True, stop=True),
             reads=[r_Ub, r_maskb], writes=[C.rpb[0]])
        P.op("pe", lambda e: e.matmul(C.pb[1][:, 0:128], lhsT=oneb[:], rhs=maskb[:], start=True, stop=True),
             reads=[r_oneb, r_maskb], writes=[C.rpb[1]])
        P.op("act", lambda e: e.copy(rkf, C.pb[0][:, 0:128]), reads=[C.rpb[0]], writes=[r_rkm])
        P.op("act", lambda e: e.copy(tot[:].rearrange("p t e -> p (t e)"), C.pb[1][:, 0:128]), reads=[C.rpb[1]],
             writes=[r_tot])
        P.op("dve", lambda e: e.memset(off[:, 0, :], 0.0), writes=[r_off])
        for tb in range(1, NTB):
            P.op("dve", lambda e, tb=tb: e.tensor_tensor(out=off[:, tb, :], in0=off[:, tb - 1, :], in1=tot[:, tb - 1, :],
                                                         op=ALU.add), reads=[r_off, r_tot], writes=[r_off])
        P.op("dve", lambda e: e.tensor_tensor(out=rkf, in0=rkf, in1=off[:].rearrange("p t e -> p (t e)"), op=ALU.add),
             reads=[r_rkm, r_off], writes=[r_rkm])
        P.op("dve", lambda e: e.tensor_scalar(out=tmpm[:], in0=maskf[:], scalar1=-1.0, scalar2=1e9, op0=ALU.add,
                                              op1=ALU.mult), reads=[r_maskf], writes=[r_tmpm])
        P.op("dve", lambda e: e.tensor_tensor(out=rkf, in0=rkf, in1=maskf[:], op=ALU.mult), reads=[r_rkm, r_maskf],
             writes=[r_rkm])
        P.op("dve", lambda e: e.tensor_tensor(out=rkf, in0=rkf, in1=tmpm[:], op=ALU.add), reads=[r_rkm, r_tmpm],
             writes=[r_rkm])
        P.op("dve", lambda e: e.tensor_tensor(out=ne_t[:], in0=off[:, NTB - 1, :], in1=tot[:, NTB - 1, :], op=ALU.add),
             reads=[r_off, r_tot], writes=[r_net])
        PC = 640
        for p_ in range(4):
            P.op("dve", lambda e, p_=p_: e.tensor_scalar(out=flagf[0:1, p_ * 8:(p_ + 1) * 8], in0=ne_t[0:1, :],
                                                         scalar1=float(PC * p_), scalar2=None, op0=ALU.is_gt),
                 reads=[r_net], writes=[r_flagf])
        P.op("dve", lambda e: e.tensor_copy(flags_i[:], flagf[:]), reads=[r_flagf], writes=[r_flags])
        P.op("dve", lambda e: e.tensor_copy(chh[:].rearrange("p t e -> p (t e)"), cflat), reads=r_comb, writes=[r_chh])
        P.op("dve", lambda e: e.tensor_tensor(out=tmpm[:], in0=cflat, in1=chh[:].rearrange("p t e -> p (t e)"),
                                              op=ALU.subtract), reads=r_comb + [r_chh, r_tmpm], writes=[r_tmpm])
        P.op("dve", lambda e: e.tensor_copy(cll[:].rearrange("p t e -> p (t e)"), tmpm[:]), reads=[r_tmpm], writes=[r_cll])
        P.op("pool", lambda e: e.iota(io_i[:], pattern=[[1, 128]], base=0, channel_multiplier=0), writes=[r_ioi])
        P.op("dve", lambda e: e.tensor_copy(io_f[:], io_i[:]), reads=[r_ioi], writes=[r_iof])
        P.barrier()
        C.off = C.mark
        NSB = PC // 128
        xacc, _ = C.tile([128, NTB, D], F32)
        r_xacc = [Res(f"xacc{i}") for i in range(NTB)]
        mark2 = C.off
        for tb in range(NTB):
            P.dma("sp", lambda e, tb=tb: e.dma_start(out=xacc[:, tb, :], in_=x1s[tb * 128:(tb + 1) * 128, :]),
                  reads=[r_x1s[tb]], writes=[r_xacc[tb]], sem=P.dsem())
        hTe, r_hTe = C.tile([128, 8, PC], BF16)
        actT, _ = C.tile([128, NFC, PC], BF16)
        r_act = [Res(f"act{f}") for f in range(NFC)]
        PmT, _ = C.tile([128, NSB, TSH], BF16)
        r_PmT = [Res(f"PmT{i}") for i in range(NSB)]
        yg, _ = C.tile([128, NSB, D], BF16)
        r_yg = [Res(f"yg{i}") for i in range(NSB)]
        gs, r_gs = C.tile([128, NSB], F32)
        gtr = C.ring("gt", 1, [128, 2], F32)
        pmr = C.ring("Pm", 1, [128, NTB, 128], BF16)
        wgr = C.ring("wg", 2, [128, 8, 128], BF16, with_sem=True)
        wur = C.ring("wu", 2, [128, 8, 128], BF16, with_sem=True)
        wdr = C.ring("wd", 2, [128, 512], BF16, with_sem=True)
        for ex in range(NE):
            wg_v = wg[ex].rearrange("(kc p) f -> p kc f", p=128)
            wu_v = wu[ex].rearrange("(kc p) f -> p kc f", p=128)
            for p_ in range(4):
                P.cond_begin(flags_i[0:1, p_ * 8 + ex:p_ * 8 + ex + 1], r_flags)
                for sb in range(NSB):
                    s0 = PC * p_ + 128 * sb
                    Pm, r_Pm = pmr()
                    for tb in range(NTB):
                        P.op("dve", lambda e, tb=tb, Pm=Pm, s0=s0, ex=ex: e.tensor_scalar(
                            out=Pm[:, tb, :], in0=io_f[:], scalar1=float(s0), scalar2=rkm[:, tb, ex:ex + 1],
                            op0=ALU.add, op1=ALU.is_equal), reads=[r_iof, r_rkm], writes=[r_Pm])
                    gb = (0, 1) if sb % 2 == 0 else (2, 3)
                    for kc in range(8):
                        bank, c0 = gb[kc // 4], (kc % 4) * 128
                        for tb in range(NTB):
                            P.op("pe", lambda e, tb=tb, kc=kc, bank=bank, c0=c0, Pm=Pm: e.matmul(
                                C.pb[bank][:, c0:c0 + 128], lhsT=h_tm[:, tb, kc * 128:(kc + 1) * 128], rhs=Pm[:, tb, :],
                                start=(tb == 0), stop=(tb == NTB - 1)), reads=[r_htm[tb], r_Pm], writes=[C.rpb[bank]])
                    for hh in range(2):
                        P.op("act", lambda e, hh=hh, sb=sb, gb=gb: e.copy(
                            hTe[:, hh * 4:(hh + 1) * 4, sb * 128:(sb + 1) * 128],
                            C.pb[gb[hh]][:, 0:512].rearrange("p (k s) -> p k s", k=4)),
                            reads=[C.rpb[gb[hh]]], writes=[r_hTe])
                    for col, cc in ((0, chh), (1, cll)):
                        for tb in range(NTB):
                            P.op("pe", lambda e, tb=tb, col=col, cc=cc, Pm=Pm, ex=ex: e.matmul(
                                C.pb[4][:, col:col + 1], lhsT=Pm[:, tb, :], rhs=cc[:, tb, ex:ex + 1],
                                start=(tb == 0), stop=(tb == NTB - 1)), reads=[r_Pm, r_chh, r_cll], writes=[C.rpb[4]])
                    gt, r_gt = gtr()
                    P.op("act", lambda e, gt=gt: e.copy(gt[:], C.pb[4][:, 0:2]), reads=[C.rpb[4]], writes=[r_gt])
                    P.op("dve", lambda e, gt=gt, sb=sb: e.tensor_tensor(out=gs[:, sb:sb + 1], in0=gt[:, 0:1], in1=gt[:, 1:2],
                                                                        op=ALU.add), reads=[r_gt, r_gs], writes=[r_gs])
                    for g8 in range(2):
                        bank = (5, 6)[g8]
                        for t8 in range(8):
                            tb = g8 * 8 + t8
                            P.op("pe", lambda e, tb=tb, t8=t8, bank=bank, Pm=Pm: e.transpose(
                                C.psbv[bank][:, t8 * 128:(t8 + 1) * 128], Pm[:, tb, :], C.ident[:]),
                                reads=[r_Pm, C.r_ident], writes=[C.rpb[bank]])
                        if g8 == 0:
                            P.op("act", lambda e, sb=sb, bank=bank: e.copy(PmT[:, sb, 0:1024], C.psbv[bank][:, 0:1024]),
                                 reads=[C.rpb[bank]], writes=[r_PmT[sb]])
                        else:
                            P.op("dve", lambda e, sb=sb, bank=bank: e.tensor_copy(PmT[:, sb, 1024:2048],
                                                                                  C.psbv[bank][:, 0:1024]),
                                 reads=[C.rpb[bank], r_PmT[sb]], writes=[r_PmT[sb]])
                for fc in range(NFC):
                    g_t, r_g, s_g = wgr()
                    u_t, r_u, s_u = wur()
                    P.dma("pool", lambda e, fc=fc, g_t=g_t, wg_v=wg_v: e.dma_start(
                        out=g_t[:], in_=wg_v[:, :, fc * 128:(fc + 1) * 128]), writes=[r_g], sem=s_g)
                    P.dma("pool", lambda e, fc=fc, u_t=u_t, wu_v=wu_v: e.dma_start(
                        out=u_t[:], in_=wu_v[:, :, fc * 128:(fc + 1) * 128]), writes=[r_u], sem=s_u)
                    pg0, pg1, pu0, pu1 = (0, 1, 2, 3) if fc % 2 == 0 else (4, 5, 6, 7)
                    for (bk, w_t, c0, c1, r_w_) in ((pg0, g_t, 0, 512, r_g), (pg1, g_t, 512, PC, r_g),
                                                    (pu0, u_t, 0, 512, r_u), (pu1, u_t, 512, PC, r_u)):
                        for kc in range(8):
                            P.op("pe", lambda e, kc=kc, w_t=w_t, bk=bk, c0=c0, c1=c1: e.matmul(
                                C.pb[bk][:, 0:c1 - c0], lhsT=w_t[:, kc, :], rhs=hTe[:, kc, c0:c1], start=(kc == 0),
                                stop=(kc == 7)), reads=[r_w_, r_hTe], writes=[C.rpb[bk]])
                    P.op("act", lambda e, fc=fc, pg0=pg0: e.activation(out=actT[:, fc, 0:512], in_=C.pb[pg0][:, :],
                                                                       func=AF.Silu), reads=[C.rpb[pg0]], writes=[r_act[fc]])
                    P.op("act", lambda e, fc=fc, pg1=pg1: e.activation(out=actT[:, fc, 512:PC], in_=C.pb[pg1][:, 0:PC - 512],
                                                                       func=AF.Silu), reads=[C.rpb[pg1], r_act[fc]],
                         writes=[r_act[fc]])
                    P.op("dve", lambda e, pu0=pu0, fc=fc: e.tensor_tensor(
                        out=actT[:, fc, 0:512], in0=actT[:, fc, 0:512], in1=C.pb[pu0][:, :], op=ALU.mult),
                        reads=[C.rpb[pu0], r_act[fc]], writes=[r_act[fc]])
                    P.op("dve", lambda e, pu1=pu1, fc=fc: e.tensor_tensor(
                        out=actT[:, fc, 512:PC], in0=actT[:, fc, 512:PC], in1=C.pb[pu1][:, 0:PC - 512], op=ALU.mult),
                        reads=[C.rpb[pu1], r_act[fc]], writes=[r_act[fc]])
                for hf_ in range(2):
                    banks = (0, 1, 2, 3, 4) if hf_ == 0 else (5, 6, 7, 0, 1)
                    for fc in range(NFC):
                        d_t, r_d, s_d = wdr()
                        P.dma("pool", lambda e, fc=fc, d_t=d_t, ex=ex, hf_=hf_: e.dma_start(
                            out=d_t[:], in_=wd[ex, fc * 128:(fc + 1) * 128, hf_ * 512:(hf_ + 1) * 512]),
                            writes=[r_d], sem=s_d)
                        for sb in range(NSB):
                            bk = banks[sb]
                            P.op("pe", lambda e, fc=fc, sb=sb, bk=bk, d_t=d_t: e.matmul(
                                C.pb[bk][:, :], lhsT=actT[:, fc, sb * 128:(sb + 1) * 128], rhs=d_t[:, :],
                                start=(fc == 0), stop=(fc == NFC - 1)), reads=[r_d, r_act[fc]], writes=[C.rpb[bk]])
                    for sb in range(NSB):
                        bk = banks[sb]
                        P.op("dve", lambda e, sb=sb, hf_=hf_, bk=bk: e.tensor_scalar(
                            out=yg[:, sb, hf_ * 512:(hf_ + 1) * 512], in0=C.pb[bk][:, :], scalar1=gs[:, sb:sb + 1],
                            scalar2=None, op0=ALU.mult), reads=[C.rpb[bk], r_gs, r_yg[sb]], writes=[r_yg[sb]])
                for tb in range(NTB):
                    for hf_ in range(2):
                        bk = (tb * 2 + hf_) % 4
                        for sb in range(NSB):
                            P.op("pe", lambda e, tb=tb, hf_=hf_, bk=bk, sb=sb: e.matmul(
                                C.pb[bk][:, :], lhsT=PmT[:, sb, tb * 128:(tb + 1) * 128],
                                rhs=yg[:, sb, hf_ * 512:(hf_ + 1) * 512], start=(sb == 0), stop=(sb == NSB - 1)),
                                reads=[r_PmT[sb], r_yg[sb]], writes=[C.rpb[bk]])
                        P.op("dve", lambda e, tb=tb, hf_=hf_, bk=bk: e.tensor_tensor(
                            out=xacc[:, tb, hf_ * 512:(hf_ + 1) * 512], in0=xacc[:, tb, hf_ * 512:(hf_ + 1) * 512],
                            in1=C.pb[bk][:, :], op=ALU.add), reads=[C.rpb[bk], r_xacc[tb]], writes=[r_xacc[tb]])
                P.cond_end()
        if defer_next:
            P.barrier()
            C.off = mark2
            alloc_next()
        for tb in range(NTB):
            if not last and x_out is not None:
                P.dma("sp", lambda e, tb=tb: e.dma_start(out=x_out[tb * 128:(tb + 1) * 128, :], in_=xacc[:, tb, :]),
                      reads=[r_xacc[tb]], writes=[d_out], sem=P.dsem())
            emit_next(xacc[:, tb, :], r_xacc[tb], tb)
        return

    HT = 1024
    NHB = HT // 128
    actT, _ = C.tile([128, NFC, HT], BF16)
    r_act = [[Res(f"act{f}_{s}") for s in range(HT // 512)] for f in range(NFC)]
    xacc, _ = C.tile([128, NHB, D], F32)
    r_xacc = [Res(f"xacc{i}") for i in range(NHB)]
    s_xa = [P.dsem() for _ in range(NHB)]
    wgr = C.ring("wg", 3, [128, 8, 128], BF16, with_sem=True)
    wur = C.ring("wu", 3, [128, 8, 128], BF16, with_sem=True)
    wdr = C.ring("wd", 3, [128, D], BF16, with_sem=True)
    sgr = C.ring("sg", 2, [128, 512], F32)
    for half in range(TSH // HT):
        for j in range(NHB):
            tb = half * NHB + j
            P.dma("sp", lambda e, tb=tb, j=j: e.dma_start(out=xacc[:, j, :], in_=x1s[tb * 128:(tb + 1) * 128, :]),
                  reads=[r_x1s[tb]], writes=[r_xacc[j]], sem=s_xa[j])
        for ex in range(0 if os.environ.get('K_SKIP2') else ne):
            wg_v = wg[ex].rearrange("(kc p) f -> p kc f", p=128)
            wu_v = wu[ex].rearrange("(kc p) f -> p kc f", p=128)
            for fc in range(NFC):
                g_t, r_g, s_g = wgr()
                u_t, r_u, s_u = wur()
                P.dma("pool", lambda e, fc=fc, g_t=g_t, wg_v=wg_v: e.dma_start(
                    out=g_t[:], in_=wg_v[:, :, fc * 128:(fc + 1) * 128]), writes=[r_g], sem=s_g)
                P.dma("pool", lambda e, fc=fc, u_t=u_t, wu_v=wu_v: e.dma_start(
                    out=u_t[:], in_=wu_v[:, :, fc * 128:(fc + 1) * 128]), writes=[r_u], sem=s_u)
                if DBG2 == 1:
                    continue
                for st_ in range(HT // 512):
                    t0 = half * HT + st_ * 512
                    rh = [r_hT[(t0 // 128) + i] for i in range(4)]
                    pg, pu = (0, 1) if (fc * 2 + st_) % 2 == 0 else (2, 3)
                    for kc in range(8):
                        P.op("pe", lambda e, kc=kc, g_t=g_t, t0=t0, pg=pg: e.matmul(
                            C.pb[pg][:, :], lhsT=g_t[:, kc, :], rhs=hT[:, kc, t0:t0 + 512], start=(kc == 0),
                            stop=(kc == 7)), reads=[r_g] + rh, writes=[C.rpb[pg]], signal=(kc == 7))
                    for kc in range(8):
                        P.op("pe", lambda e, kc=kc, u_t=u_t, t0=t0, pu=pu: e.matmul(
                            C.pb[pu][:, :], lhsT=u_t[:, kc, :], rhs=hT[:, kc, t0:t0 + 512], start=(kc == 0),
                            stop=(kc == 7)), reads=[r_u] + rh, writes=[C.rpb[pu]], signal=(kc == 7))
                    sg, r_sg = sgr()
                    P.op("act", lambda e, sg=sg, pg=pg: e.activation(out=sg[:], in_=C.pb[pg][:, :], func=AF.Silu),
                         reads=[C.rpb[pg]], writes=[r_sg])
                    P.op("dve", lambda e, sg=sg, pu=pu, fc=fc, st_=st_: e.tensor_tensor(
                        out=actT[:, fc, st_ * 512:(st_ + 1) * 512], in0=sg[:], in1=C.pb[pu][:, :], op=ALU.mult),
                        reads=[r_sg, C.rpb[pu]], writes=[r_act[fc][st_]])
            for jg in range(0 if DBG2 in (1, 2) else HT // 512):
                for fc in range(NFC):
                    d_t, r_d, s_d = wdr()
                    P.dma("pool", lambda e, fc=fc, d_t=d_t, ex=ex: e.dma_start(
                        out=d_t[:], in_=wd[ex, fc * 128:(fc + 1) * 128, :]), writes=[r_d], sem=s_d)
                    for jj in range(4):
                        j = jg * 4 + jj
                        for hf_ in range(2):
                            bk = jj * 2 + hf_
                            P.op("pe", lambda e, fc=fc, j=j, hf_=hf_, bk=bk, d_t=d_t: e.matmul(
                                C.pb[bk][:, :], lhsT=actT[:, fc, j * 128:(j + 1) * 128],
                                rhs=d_t[:, hf_ * 512:(hf_ + 1) * 512], start=(fc == 0), stop=(fc == NFC - 1)),
                                reads=[r_d, r_act[fc][jg]], writes=[C.rpb[bk]], signal=(fc == NFC - 1 or (jj == 3 and hf_ == 1)))
                for jj in range(4):
                    j = jg * 4 + jj
                    tb = half * NHB + j
                    for hf_ in range(2):
                        bk = jj * 2 + hf_
                        if mode == "moe":
                            P.op("dve", lambda e, j=j, hf_=hf_, bk=bk, tb=tb, ex=ex: e.scalar_tensor_tensor(
                                out=xacc[:, j, hf_ * 512:(hf_ + 1) * 512], in0=C.pb[bk][:, :],
                                scalar=comb[:, tb, ex:ex + 1], in1=xacc[:, j, hf_ * 512:(hf_ + 1) * 512],
                                op0=ALU.mult, op1=ALU.add), reads=[C.rpb[bk], r_comb[tb], r_xacc[j]],
                                writes=[r_xacc[j]])
                        else:
                            P.op("dve", lambda e, j=j, hf_=hf_, bk=bk: e.tensor_tensor(
                                out=xacc[:, j, hf_ * 512:(hf_ + 1) * 512], in0=xacc[:, j, hf_ * 512:(hf_ + 1) * 512],
                                in1=C.pb[bk][:, :], op=ALU.add), reads=[C.rpb[bk], r_xacc[j]], writes=[r_xacc[j]])
        for j in range(NHB):
            tb = half * NHB + j
            if not last and x_out is not None:
                P.dma("sp", lambda e, tb=tb, j=j: e.dma_start(out=x_out[tb * 128:(tb + 1) * 128, :], in_=xacc[:, j, :]),
                      reads=[r_xacc[j]], writes=[d_out], sem=s_xa[j])
            emit_next(xacc[:, j, :], r_xacc[j], tb)
    return


def build_C(mode, last):
    nc = bass.Bass("TRN2", target_bir_lowering=False)
    C = Ctx(nc)
    ne = NE if mode in ("moe", "moes") else 1
    T = {"x": dram_in(nc, "x", [TSH, D], F32), "g_next": dram_in(nc, "g_next", [D], F32)}
    if mode != "N":
        T.update(mixT=dram_in(nc, "mixT", [D, TSH], BF16), w_out=dram_in(nc, "w_out", [D, D], F32),
                 g_ffn=dram_in(nc, "g_ffn", [D], F32), wg=dram_in(nc, "wg", [ne, D, FF], F32),
                 wu=dram_in(nc, "wu", [ne, D, FF], F32), wd=dram_in(nc, "wd", [ne, FF, D], F32))
        if mode in ("moe", "moes"):
            T["router"] = dram_in(nc, "router", [D, NE], F32)
    if last:
        T["y"] = dram_out(nc, "y", [TSH, D], F32)
    else:
        T["x_out"] = dram_out(nc, "x_out", [TSH, D], F32)
        T["hT"] = dram_out(nc, "hT", [D, TSH], BF16)
    emit_C(C, T, mode, last)
    C.P.wait_all("sp", [])
    C.P.emit()
    return nc


def emit_AB(C, T):
    nc = C.nc
    P = C.P
    hT_in = T["hT"]
    w_in, wq_up, wkv_up, qn, kvn = T["w_in"], T["wq_up"], T["wkv_up"], T["qn"], T["kvn"]
    scal, pos, cst, yT = T["scal"], T["pos"], T["cst"], T["yT"]
    C.uid += 1
    vscr = nc.dram_tensor(f"vscr{C.uid}", [S, 64], BF16).ap()
    d_out = Res("d_out")
    r_vscr = Res("vscr")
    NT = S // 512
    s_c = P.dsem()

    w_sb, r_w = C.tile([128, 8, NCOL], BF16)
    s_w = P.dsem()
    w_v = w_in.rearrange("(kc p) n -> p kc n", p=128)
    for kc in range(8):
        P.dma("pool", lambda e, kc=kc: e.dma_start(out=w_sb[:, kc, :], in_=w_v[:, kc, :]), writes=[r_w], sem=s_w)
    wq_f, r_wqf = C.tile([128, 2, 96], F32)
    wq_s, r_wq = C.tile([128, 2, 96], BF16)
    qn_t, r_qn = C.tile([128, 2], F32)
    wkv_f, r_wkvf = C.tile([128, 128], F32)
    wkv_s, r_wkv = C.tile([128, 128], BF16)
    kvn_t, r_kvn = C.tile([128, 1], F32)
    P.dma("sp", lambda e: e.dma_start(out=wq_f[:], in_=wq_up.rearrange("(c p) n -> p c n", p=128)), writes=[r_wqf], sem=P.dsem())
    for c in range(2):
        P.dma("sp", lambda e, c=c: e.dma_start(out=qn_t[:, c:c + 1], in_=qn[c * 128:(c + 1) * 128].rearrange("(p o) -> p o", o=1)),
              writes=[r_qn], sem=P.dsem())
    P.dma("sp", lambda e: e.dma_start(out=wkv_f[:], in_=wkv_up), writes=[r_wkvf], sem=P.dsem())
    P.dma("sp", lambda e: e.dma_start(out=kvn_t[:], in_=kvn.rearrange("(p o) -> p o", o=1)), writes=[r_kvn], sem=P.dsem())
    for c in range(2):
        P.op("dve", lambda e, c=c: e.tensor_scalar(out=wq_s[:, c, :], in0=wq_f[:, c, :], scalar1=qn_t[:, c:c + 1], scalar2=None,
                                                   op0=ALU.mult), reads=[r_wqf, r_qn], writes=[r_wq])
    P.op("dve", lambda e: e.tensor_scalar(out=wkv_s[:], in0=wkv_f[:], scalar1=kvn_t[:, 0:1], scalar2=None, op0=ALU.mult),
         reads=[r_wkvf, r_kvn], writes=[r_wkv])
    sc0, r_sc0 = C.tile([1, 8], F32)
    sc64, r_sc64 = C.tile([128, 8], F32)
    P.dma("sp", lambda e: e.dma_start(out=sc0[:], in_=scal), writes=[r_sc0], sem=P.dsem())
    P.dma("sp", lambda e: e.dma_start(out=sc64[64:65, :], in_=scal), writes=[r_sc64], sem=P.dsem())
    P.op("dve", lambda e: e.tensor_scalar(out=sc0[:, 0:1], in0=sc0[:, 0:1], scalar1=-1.0, scalar2=None, op0=ALU.mult),
         reads=[r_sc0], writes=[r_sc0])
    P.op("act", lambda e: e.activation(out=sc64[64:65, 2:3], in_=sc64[64:65, 1:2], func=AF.Exp), reads=[r_sc64],
         writes=[r_sc64])
    ones_f, r_ones = C.tile([128, 64], F32)
    P.op("dve", lambda e: e.memset(ones_f[:], 1.0), writes=[r_ones])
    cst_t, r_cst = C.tile([128, 32], F32)
    P.dma("sp", lambda e: e.dma_start(out=cst_t[:], in_=cst), writes=[r_cst], sem=P.dsem())
    pos_i, r_posi = C.tile([128, 64], I32)
    pos_f, r_posf = C.tile([128, 64], F32)
    P.dma("sp", lambda e: e.dma_start(out=pos_i[:], in_=pos), writes=[r_posi], sem=P.dsem())
    P.op("dve", lambda e: e.tensor_copy(pos_f[:], pos_i[:]), reads=[r_posi], writes=[r_posf])
    ang, r_ang = C.tile([128, 64 * 16], F32)
    sin_t, r_sin = C.tile([128, 64 * 16], F32)
    cos_t, r_cos = C.tile([128, 64 * 16], F32)
    tkf, r_tkf = C.tile([128, 64 * 16], F32)
    tki, r_tki = C.tile([128, 64 * 16], I32)
    tfx, r_tfx = tkf, r_tkf
    for blk in range(64):
        P.op("dve", lambda e, blk=blk: e.tensor_scalar(out=ang[:, blk * 16:(blk + 1) * 16], in0=cst_t[:, 0:16],
                                                       scalar1=pos_f[:, blk:blk + 1], scalar2=None, op0=ALU.mult),
             reads=[r_cst, r_posf], writes=[r_ang])

    def emit_sin(dst, r_dst, off):
        md = dst
        P.op("dve", lambda e: e.tensor_scalar(out=tkf[:], in0=ang[:], scalar1=off, scalar2=1.0 / (2 * PI), op0=ALU.add,
                                              op1=ALU.mult), reads=[r_ang], writes=[r_tkf])
        P.op("dve", lambda e: e.tensor_copy(tki[:], tkf[:]), reads=[r_tkf], writes=[r_tki])
        P.op("dve", lambda e: e.tensor_copy(tkf[:], tki[:]), reads=[r_tki], writes=[r_tkf])
        P.op("dve", lambda e: e.scalar_tensor_tensor(out=md[:], in0=tkf[:], scalar=-2 * PI, in1=ang[:], op0=ALU.mult,
                                                     op1=ALU.add), reads=[r_tkf, r_ang], writes=[r_dst])
        if off != 0.0:
            P.op("dve", lambda e: e.tensor_scalar(out=md[:], in0=md[:], scalar1=off, scalar2=None, op0=ALU.add),
                 reads=[r_dst], writes=[r_dst])
        P.op("dve", lambda e: e.tensor_scalar(out=tfx[:], in0=md[:], scalar1=PI, scalar2=-2 * PI, op0=ALU.is_gt,
                                              op1=ALU.mult), reads=[r_dst], writes=[r_tfx])
        P.op("dve", lambda e: e.tensor_tensor(out=md[:], in0=md[:], in1=tfx[:], op=ALU.add), reads=[r_dst, r_tfx],
             writes=[r_dst])
        P.op("dve", lambda e: e.tensor_scalar(out=tfx[:], in0=md[:], scalar1=-PI, scalar2=2 * PI, op0=ALU.is_lt,
                                              op1=ALU.mult), reads=[r_dst], writes=[r_tfx])
        P.op("dve", lambda e: e.tensor_tensor(out=md[:], in0=md[:], in1=tfx[:], op=ALU.add), reads=[r_dst, r_tfx],
             writes=[r_dst])
        P.op("act", lambda e: e.activation(out=md[:], in_=md[:], func=AF.Sin), reads=[r_dst], writes=[r_dst])

    emit_sin(sin_t, r_sin, 0.0)
    emit_sin(cos_t, r_cos, PI / 2)
    trif, r_trif = C.tile([128, 128], F32)
    tri, r_tri = C.tile([128, 128], BF16)
    P.op("pool", lambda e: e.memset(trif[:], 1.0), writes=[r_trif])
    P.op("pool", lambda e: e.affine_select(out=trif[:], in_=trif[:], pattern=[[1, 128]], compare_op=ALU.is_ge, fill=0.0,
                                           base=0, channel_multiplier=-1), reads=[r_trif], writes=[r_trif])
    P.op("dve", lambda e: e.tensor_copy(tri[:], trif[:]), reads=[r_trif], writes=[r_tri])
    jmp_i, r_jmpi = C.tile([128, 256], I32)
    jmp_f, r_jmpf = C.tile([128, 256], F32)
    P.op("pool", lambda e: e.iota(jmp_i[:], pattern=[[1, 256]], base=0, channel_multiplier=-1), writes=[r_jmpi])
    P.op("dve", lambda e: e.tensor_copy(jmp_f[:], jmp_i[:]), reads=[r_jmpi], writes=[r_jmpf])
    Mb, r_Mb = [], []
    for m in range(4):
        t, r = C.tile([128, 256], F32)
        span = 127 if m == 0 else 128
        P.op("act", lambda e, t=t, m=m: e.activation(out=t[:], in_=jmp_f[:], func=AF.Exp, scale=cst_t[:, 16 + m:17 + m]),
             reads=[r_jmpf, r_cst], writes=[r])
        P.op("pool", lambda e, t=t: e.affine_select(out=t[:], in_=t[:], pattern=[[1, 256]], compare_op=ALU.is_ge, fill=0.0,
                                                    base=0, channel_multiplier=-1), reads=[r], writes=[r])
        P.op("pool", lambda e, t=t, span=span: e.affine_select(out=t[:], in_=t[:], pattern=[[-1, 256]], compare_op=ALU.is_ge,
                                                               fill=0.0, base=span, channel_multiplier=1), reads=[r], writes=[r])
        Mb.append(t)
        r_Mb.append(r)

    QT, _ = C.tile([128, S], BF16)
    KT, _ = C.tile([128, S], BF16)
    r_QT = [Res(f"QT{i}") for i in range(NT)]
    r_KT = [Res(f"KT{i}") for i in range(NT)]
    V = []
    r_V = []
    for i in range(3):
        t, _ = C.tile([128, 64, 65], BF16)
        V.append(t)
        r_V.append([Res(f"V{i}_{j}") for j in range(NT)])
        P.op("pool", lambda e, t=t: e.memset(t[:, :, 64:65], 1.0), writes=r_V[i])
    ysb, _ = C.tile([64, S], BF16)
    r_ysb = [Res(f"ysb{i}") for i in range(NT)]
    s_y = [P.dsem() for _ in range(NT)]
    acc, _ = C.tile([65, S], F32)
    r_acc = [Res(f"acc{i}") for i in range(NT)]
    hring = C.ring("hT", 2, [128, 8, 512], BF16, with_sem=True)
    pring = C.ring("pt", 4, [128, 512], BF16)
    ering = C.ring("et", 2, [128, 256], F32)
    oring = C.ring("osb", 3, [128, 512], F32)

    def load_hT(tt):
        sh = (tt * 512) // TSH
        tl0 = (tt * 512) % TSH
        h, r_h, s_h = hring()
        if "hT_fn" in T:
            src = T["hT_fn"](sh, tl0)
        else:
            src = hT_in[sh].rearrange("(kc p) t -> p kc t", p=128)[:, :, tl0:tl0 + 512]
        P.dma("sp", lambda e: e.dma_start(out=h[:], in_=src), writes=[r_h], sem=s_h)
        return h, r_h

    prot = {"i": 0}
    PROJ_BANKS = [0, 1, 2, 3, 5, 6]

    def next_bank():
        b = PROJ_BANKS[prot["i"] % len(PROJ_BANKS)]
        prot["i"] += 1
        return b

    def proj_fm(h, r_h, col0, m, dst_ap, r_dst, scale, bank):
        bank = next_bank()
        for kc in range(8):
            P.op("pe", lambda e, kc=kc: e.matmul(C.pb[bank][0:m, :], lhsT=w_sb[:, kc, col0:col0 + m], rhs=h[:, kc, :],
                                                 start=(kc == 0), stop=(kc == 7)), reads=[r_w, r_h], writes=[C.rpb[bank]])
        P.op("act", lambda e: e.mul(dst_ap, C.pb[bank][0:m, :], scale), reads=[C.rpb[bank]], writes=[r_dst])

    def proj_v(h, r_h, col0, tt, vt, r_vt, bank):
        bank = next_bank()
        for sb in range(4):
            for kc in range(8):
                P.op("pe", lambda e, kc=kc, sb=sb: e.matmul(
                    C.pb[bank][:, sb * 64:(sb + 1) * 64], lhsT=h[:, kc, sb * 128:(sb + 1) * 128],
                    rhs=w_sb[:, kc, col0:col0 + 64], start=(kc == 0), stop=(kc == 7)),
                    reads=[r_w, r_h], writes=[C.rpb[bank]])
        P.op("dve", lambda e: e.tensor_copy(vt[:, tt * 4:(tt + 1) * 4, 0:64],
                                            C.pb[bank][:, 0:256].rearrange("p (b d) -> p b d", b=4)),
             reads=[C.rpb[bank]], writes=[r_vt[tt]])

    def finalize(ob, src_ap, r_src, tq, sink):
        osb, r_osb = oring()
        P.op("act", lambda e: e.copy(osb[0:65, :], src_ap), reads=r_src, writes=[r_osb])
        if sink:
            P.op("dve", lambda e: e.tensor_scalar(out=osb[64:65, :], in0=osb[64:65, :], scalar1=sc64[64:65, 2:3],
                                                  scalar2=None, op0=ALU.add), reads=[r_osb, r_sc64], writes=[r_osb])
        P.op("dve", lambda e: e.reciprocal(osb[64:65, :], osb[64:65, :]), reads=[r_osb], writes=[r_osb])
        P.op("pe", lambda e: e.matmul(C.pb[4][0:64, :], lhsT=ones_f[64:65, 0:64], rhs=osb[64:65, :], start=True,
                                      stop=True), reads=[r_ones, r_osb], writes=[C.rpb[4]])
        P.op("dve", lambda e: e.tensor_tensor(out=ysb[0:64, tq * 512:(tq + 1) * 512], in0=osb[0:64, :],
                                              in1=C.pb[4][0:64, :], op=ALU.mult), reads=[r_osb, C.rpb[4]],
             writes=[r_ysb[tq]])

    def store_y(mixer):
        r_st = Res(f"yst{mixer}")
        P.dma("sp", lambda e: e.dma_start(out=yT[mixer * 64:(mixer + 1) * 64, :], in_=ysb[0:64, :]),
              reads=r_ysb, writes=[d_out, r_st], sem=s_y[0])
        if "after_store" in T:
            T["after_store"](mixer, r_st)

    def attn_causal(kdim):
        for qt in range(NT):
            t0 = qt * 512
            nkb = (t0 + 512) // 128
            ob = 2 + qt % 2

            SB = (0, 1, 5, 6)
            LA = 3

            def s_mm(kb):
                o = max(0, kb * 128 - t0)
                sb_ = SB[kb % 4]
                P.op("pe", lambda e, t0=t0, o=o, kb=kb, sb_=sb_: e.matmul(
                    C.pb[sb_][:, o:512], lhsT=KT[0:kdim, kb * 128:(kb + 1) * 128],
                    rhs=QT[0:kdim, t0 + o:t0 + 512], start=True, stop=True),
                     reads=[r_KT[kb // 4], r_QT[qt]], writes=[C.rpb[sb_]])
            for kb in range(min(LA, nkb)):
                s_mm(kb)
            for kb in range(nkb):
                if kb + LA < nkb:
                    s_mm(kb + LA)
                o = max(0, kb * 128 - t0)
                sb_ = SB[kb % 4]
                pt, r_pt = pring()
                P.op("act", lambda e, o=o, pt=pt, sb_=sb_: e.activation(out=pt[:, o:512], in_=C.pb[sb_][:, o:512],
                                                                        func=AF.Exp), reads=[C.rpb[sb_]], writes=[r_pt])
                if kb * 128 >= t0:
                    P.op("pool", lambda e, o=o, pt=pt: e.tensor_tensor(out=pt[:, o:o + 128], in0=pt[:, o:o + 128],
                                                                       in1=tri[:], op=ALU.mult), reads=[r_pt, r_tri],
                         writes=[r_pt])
                P.op("pe", lambda e, o=o, pt=pt, kb=kb, ob=ob, nkb=nkb: e.matmul(
                    C.pb[ob][0:65, o:512], lhsT=V[0][:, kb, 0:65], rhs=pt[:, o:512], start=(kb == 0),
                    stop=(kb == nkb - 1), skip_group_check=True), reads=[r_V[0][kb // 4], r_pt], writes=[C.rpb[ob]])
            finalize(ob, C.pb[ob][0:65, :], [C.rpb[ob]], qt, False)

    cnt = [0]

    def attn_banded(dil, m, vt, r_vt, evac):
        L = S // dil
        for r in range(dil):
            for qt in range(L // 512):
                n0 = qt * 512
                ob = 2 + cnt[0] % 2
                cnt[0] += 1
                kbs = [kb for kb in range(n0 // 128 - 1, n0 // 128 + 4) if kb >= 0]
                tl_lo = (dil * n0) // 512
                tl_hi = min(NT - 1, (dil * (n0 + 511) + r) // 512)
                rq = [r_QT[i] for i in range(tl_lo, tl_hi + 1)]
                SBK = (0, 1, 5, 6, 7)
                geo = []
                for i, kb in enumerate(kbs):
                    qa = max(kb * 128, n0)
                    qb = min(kb * 128 + 256, n0 + 512)
                    jo, w, co = qa - kb * 128, qb - qa, qa - n0
                    kc0 = r + dil * kb * 128
                    kcols = slice(kc0, kc0 + dil * 127 + 1, dil)
                    qcols = slice(r + dil * qa, r + dil * (qb - 1) + 1, dil)
                    ktl = sorted(set([(kc0) // 512, min(NT - 1, (kc0 + dil * 127) // 512)]))
                    rk = [r_KT[j] for j in range(ktl[0], ktl[-1] + 1)]
                    sbk = SBK[i]
                    geo.append((jo, w, co, sbk))
                    P.op("pe", lambda e, w=w, kcols=kcols, qcols=qcols, sbk=sbk: e.matmul(
                        C.pb[sbk][:, 0:w], lhsT=KT[0:64, kcols], rhs=QT[0:64, qcols], start=True, stop=True),
                        reads=rk + rq, writes=[C.rpb[sbk]])
                for i, kb in enumerate(kbs):
                    jo, w, co, sbk = geo[i]
                    et, r_et = ering()
                    pt, r_pt = pring()
                    P.op("act", lambda e, w=w, et=et, sbk=sbk: e.activation(out=et[:, 0:w], in_=C.pb[sbk][:, 0:w],
                                                                            func=AF.Exp), reads=[C.rpb[sbk]], writes=[r_et])
                    P.op("dve", lambda e, w=w, et=et, pt=pt, jo=jo: e.tensor_tensor(
                        out=pt[:, 0:w], in0=et[:, 0:w], in1=Mb[m][:, jo:jo + w], op=ALU.mult),
                        reads=[r_et, r_Mb[m]], writes=[r_pt])
                    ch = r * (L // 128) + kb
                    P.op("pe", lambda e, w=w, co=co, pt=pt, ch=ch, i=i, ob=ob, nk=len(kbs): e.matmul(
                        C.pb[ob][0:65, co:co + w], lhsT=vt[:, ch, 0:65], rhs=pt[:, 0:w], start=(i == 0),
                        stop=(i == nk - 1), skip_group_check=True), reads=[r_vt[ch // 4], r_pt], writes=[C.rpb[ob]])
                evac(ob, r, n0, tl_lo, tl_hi)

    P.op("dve", lambda e: e.memset(QT[64:70, :], 1.0), writes=r_QT)
    P.op("dve", lambda e: e.memset(KT[64:70, :], 1.0), writes=r_KT)
    s_augq = [P.dsem() for _ in range(NT)]
    s_augk = [P.dsem() for _ in range(NT)]
    cumr = C.ring("cum", 2, [1, 512], F32)
    one1, r_one1 = C.tile([1, 512], F32)
    P.op("dve", lambda e: e.memset(one1[:], 1.0), writes=[r_one1])
    zero1, r_zero1 = C.tile([1, 1], F32)
    P.op("dve", lambda e: e.memset(zero1[:], 0.0), writes=[r_zero1])
    spr = C.ring("sp", 1, [1, 512], F32)
    augr = C.ring("aug", 1, [1, 6, 512], BF16)
    rr = C.ring("rr", 1, [1, 2, 512], F32)
    prev = (zero1[:, 0:1], r_zero1)
    for tt in range(NT):
        h, r_h = load_hT(tt)
        cols = slice(tt * 512, (tt + 1) * 512)
        proj_fm(h, r_h, C_FQ, 64, QT[0:64, cols], r_QT[tt], 0.125, 5)
        proj_fm(h, r_h, C_FK, 64, KT[0:64, cols], r_KT[tt], 1.0, 6)
        proj_v(h, r_h, C_FV, tt, V[0], r_V[0], 5)
        for kc in range(8):
            P.op("pe", lambda e, kc=kc, h=h: e.matmul(C.pb[4][0:1, :], lhsT=w_sb[:, kc, C_FF:C_FF + 1], rhs=h[:, kc, :],
                                                      start=(kc == 0), stop=(kc == 7)), reads=[r_w, r_h], writes=[C.rpb[4]])
        sp_, r_sp = spr()
        P.op("act", lambda e, sp_=sp_: e.activation(out=sp_[:], in_=C.pb[4][0:1, :], func=AF.Exp, scale=-1.0,
                                                    bias=sc0[0:1, 0:1]), reads=[C.rpb[4], r_sc0], writes=[r_sp])
        P.op("act", lambda e, sp_=sp_: e.activation(out=sp_[:], in_=sp_[:], func=AF.Ln, bias=1.0), reads=[r_sp],
             writes=[r_sp])
        cm, r_cm = cumr()
        pv_ap, r_pv = prev
        P.op("dve", lambda e, cm=cm, sp_=sp_, pv_ap=pv_ap: e.tensor_tensor_scan(
            out=cm[:], data0=one1[:], data1=sp_[:], initial=pv_ap, op0=ALU.mult, op1=ALU.add),
            reads=[r_one1, r_sp, r_pv], writes=[r_cm])
        prev = (cm[:, 511:512], r_cm)
        ag, r_ag = augr()
        rs_, r_rs = rr()
        P.op("dve", lambda e, ag=ag, cm=cm: e.tensor_copy(ag[:, 3, :], cm[:]), reads=[r_cm], writes=[r_ag])
        P.op("dve", lambda e, ag=ag, cm=cm, rs_=rs_: e.tensor_tensor(out=rs_[:, 0, :], in0=cm[:], in1=ag[:, 3, :],
                                                                     op=ALU.subtract), reads=[r_cm, r_ag], writes=[r_rs])
        P.op("dve", lambda e, ag=ag, rs_=rs_: e.tensor_copy(ag[:, 4, :], rs_[:, 0, :]), reads=[r_rs], writes=[r_ag])
        P.op("dve", lambda e, ag=ag, rs_=rs_: e.tensor_tensor(out=rs_[:, 1, :], in0=rs_[:, 0, :], in1=ag[:, 4, :],
                                                              op=ALU.subtract), reads=[r_rs, r_ag], writes=[r_rs])
        P.op("dve", lambda e, ag=ag, rs_=rs_: e.tensor_copy(ag[:, 5, :], rs_[:, 1, :]), reads=[r_rs], writes=[r_ag])
        P.op("dve", lambda e, ag=ag: e.tensor_scalar(out=ag[:, 0:3, :], in0=ag[:, 3:6, :], scalar1=-1.0, scalar2=None,
                                                     op0=ALU.mult), reads=[r_ag], writes=[r_ag])
        for i in range(3):
            P.dma("pool", lambda e, i=i, ag=ag, cols=cols: e.dma_start(out=QT[64 + i:65 + i, cols], in_=ag[0:1, i, :]),
                  reads=[r_ag], writes=[r_QT[tt]], sem=s_augq[tt])
            P.dma("pool", lambda e, i=i, ag=ag, cols=cols: e.dma_start(out=KT[67 + i:68 + i, cols], in_=ag[0:1, 3 + i, :]),
                  reads=[r_ag], writes=[r_KT[tt]], sem=s_augk[tt])
    attn_causal(70)
    store_y(0)

    for tt in range(NT):
        h, r_h = load_hT(tt)
        cols = slice(tt * 512, (tt + 1) * 512)
        proj_fm(h, r_h, C_SQ, 64, QT[0:64, cols], r_QT[tt], 0.125, 5)
        proj_fm(h, r_h, C_SK, 64, KT[0:64, cols], r_KT[tt], 1.0, 6)
        proj_v(h, r_h, C_SV, tt, V[0], r_V[0], 5)
    attn_banded(1, 0, V[0], r_V[0],
                lambda ob, r, n0, lo, hi: finalize(ob, C.pb[ob][0:65, :], [C.rpb[ob]], n0 // 512, True))
    store_y(1)

    ctr = C.ring("ctm", 3, [128, 384], BF16)
    cTr = C.ring("cT", 3, [128, 3, 128], BF16)
    ssr = C.ring("ss2", 3, [128, 2], F32)
    qkf = C.ring("qkf", 3, [128, 2, 32], F32)
    qkb = C.ring("qkb", 3, [128, 2, 96], BF16)
    rtmp = C.ring("rtmp", 4, [128, 4, 16], F32)
    mla_state = {}

    def mla_A(blk):
        tt, sb = blk // 4, blk % 4
        if sb == 0:
            mla_state["h"] = load_hT(tt)
        h, r_h = mla_state["h"]
        bT, bQ, bC, bF = (0, 1)[blk % 2], (2, 3)[blk % 2], (5, 6)[blk % 2], (7, 4)[blk % 2]
        for kc in range(8):
            P.op("pe", lambda e, kc=kc, sb=sb, h=h, bT=bT: e.matmul(
                C.pb[bT][:, 0:416], lhsT=h[:, kc, sb * 128:(sb + 1) * 128], rhs=w_sb[:, kc, C_CQ:C_CQ + 416],
                start=(kc == 0), stop=(kc == 7)), reads=[r_w, r_h], writes=[C.rpb[bT]])
        ss, r_ss = ssr()
        P.op("dve", lambda e, ss=ss: e.memset(ss[:], 0.0), writes=[r_ss])
        P.op("act", lambda e, ss=ss, bT=bT: e.activation(out=C.junk[:, 0:256], in_=C.pb[bT][:, 0:256], func=AF.Square,
                                                  scale=1.0 / 16, accum_out=ss[:, 0:1]),
             reads=[C.rpb[bT], r_ss], writes=[C.r_junk, r_ss])
        P.op("act", lambda e, ss=ss, bT=bT: e.activation(out=C.junk[:, 0:128], in_=C.pb[bT][:, 256:384], func=AF.Square,
                                                  scale=float(128 ** -0.5), accum_out=ss[:, 1:2]),
             reads=[C.rpb[bT], r_ss], writes=[C.r_junk, r_ss])
        P.op("dve", lambda e, ss=ss: e.tensor_scalar(out=ss[:], in0=ss[:], scalar1=EPS, scalar2=None, op0=ALU.add),
             reads=[r_ss], writes=[r_ss])
        P.op("act", lambda e, ss=ss: e.activation(out=ss[:], in_=ss[:], func=AF.Sqrt), reads=[r_ss], writes=[r_ss])
        P.op("dve", lambda e, ss=ss: e.reciprocal(ss[:], ss[:]), reads=[r_ss], writes=[r_ss])
        ct, r_ct = ctr()
        P.op("dve", lambda e, ct=ct, bT=bT: e.tensor_copy(ct[:], C.pb[bT][:, 0:384]), reads=[C.rpb[bT]], writes=[r_ct])
        qf, r_qf = qkf()
        P.op("act", lambda e, qf=qf, bT=bT: e.copy(qf[:, 1, :], C.pb[bT][:, 384:416]), reads=[C.rpb[bT]], writes=[r_qf])
        mla_state[blk] = dict(ss=ss, r_ss=r_ss, ct=ct, r_ct=r_ct, qf=qf, r_qf=r_qf, bQ=bQ, bC=bC, bF=bF)

    def mla_B(blk):
        tt = blk // 4
        st = mla_state[blk]
        ss, r_ss, ct, r_ct, qf, r_qf, bQ, bC, bF = (st[k] for k in ("ss", "r_ss", "ct", "r_ct", "qf", "r_qf", "bQ", "bC", "bF"))
        cT, r_cT = cTr()
        emit_transposes_to(C, ct, r_ct, cT[:], r_cT, 3, bank=bC)
        for c in range(2):
            P.op("pe", lambda e, c=c, cT=cT, bQ=bQ: e.matmul(C.pb[bQ][:, 0:96], lhsT=cT[:, c, :], rhs=wq_s[:, c, :],
                                                      start=(c == 0), stop=(c == 1)), reads=[r_cT, r_wq],
                 writes=[C.rpb[bQ]])
        P.op("pe", lambda e, cT=cT, bQ=bQ: e.matmul(C.pb[bQ][:, 128:256], lhsT=cT[:, 2, :], rhs=wkv_s[:], start=True,
                                             stop=True), reads=[r_cT, r_wkv], writes=[C.rpb[bQ]])
        qb_, r_qb = qkb()
        sq = float(96 ** -0.5)
        P.op("dve", lambda e, qb_=qb_, ss=ss, bQ=bQ: e.tensor_scalar(out=qb_[:, 0, 0:64], in0=C.pb[bQ][:, 0:64],
                                                              scalar1=ss[:, 0:1], scalar2=sq, op0=ALU.mult, op1=ALU.mult),
             reads=[C.rpb[bQ], r_ss], writes=[r_qb])
        P.op("dve", lambda e, qf=qf, ss=ss, bQ=bQ: e.tensor_scalar(out=qf[:, 0, :], in0=C.pb[bQ][:, 64:96],
                                                            scalar1=ss[:, 0:1], scalar2=sq, op0=ALU.mult, op1=ALU.mult),
             reads=[C.rpb[bQ], r_ss, r_qf], writes=[r_qf])
        P.op("dve", lambda e, qb_=qb_, ss=ss, bQ=bQ: e.tensor_scalar(out=qb_[:, 1, 0:64], in0=C.pb[bQ][:, 128:192],
                                                              scalar1=ss[:, 1:2], scalar2=None, op0=ALU.mult),
             reads=[C.rpb[bQ], r_ss, r_qb], writes=[r_qb])
        P.op("dve", lambda e, ss=ss, blk=blk, bQ=bQ: e.tensor_scalar(out=V[0][:, blk, 0:64], in0=C.pb[bQ][:, 192:256],
                                                              scalar1=ss[:, 1:2], scalar2=None, op0=ALU.mult),
             reads=[C.rpb[bQ], r_ss], writes=[r_V[0][tt]])
        cs = cos_t[:, blk * 16:(blk + 1) * 16]
        sn = sin_t[:, blk * 16:(blk + 1) * 16]
        for w_ in range(2):
            tm_, r_tm = rtmp()
            t1 = qf[:, w_, 0:16]
            t2 = qf[:, w_, 16:32]
            eng = "pool" if w_ == 0 else "dve"
            P.op(eng, lambda e, t1=t1, tm_=tm_, cs=cs: e.tensor_tensor(out=tm_[:, 0, :], in0=t1, in1=cs, op=ALU.mult),
                 reads=[r_qf, r_cos], writes=[r_tm])
            P.op(eng, lambda e, t2=t2, tm_=tm_, sn=sn: e.tensor_tensor(out=tm_[:, 1, :], in0=t2, in1=sn, op=ALU.mult),
                 reads=[r_qf, r_sin, r_tm], writes=[r_tm])
            P.op(eng, lambda e, t2=t2, tm_=tm_, cs=cs: e.tensor_tensor(out=tm_[:, 2, :], in0=t2, in1=cs, op=ALU.mult),
                 reads=[r_qf, r_cos, r_tm], writes=[r_tm])
            P.op(eng, lambda e, t1=t1, tm_=tm_, sn=sn: e.tensor_tensor(out=tm_[:, 3, :], in0=t1, in1=sn, op=ALU.mult),
                 reads=[r_qf, r_sin, r_tm], writes=[r_tm])
            P.op(eng, lambda e, w_=w_, tm_=tm_, qb_=qb_: e.tensor_tensor(out=qb_[:, w_, 64:80], in0=tm_[:, 0, :],
                                                                         in1=tm_[:, 1, :], op=ALU.subtract),
                 reads=[r_tm, r_qb], writes=[r_qb])
            P.op(eng, lambda e, w_=w_, tm_=tm_, qb_=qb_: e.tensor_tensor(out=qb_[:, w_, 80:96], in0=tm_[:, 2, :],
                                                                         in1=tm_[:, 3, :], op=ALU.add),
                 reads=[r_tm, r_qb], writes=[r_qb])
        st.update(qb_=qb_, r_qb=r_qb)

    def mla_C(blk):
        tt = blk // 4
        st = mla_state.pop(blk)
        qb_, r_qb, bF = st["qb_"], st["r_qb"], st["bF"]
        for w_, (dst, r_d) in enumerate(((QT, r_QT[tt]), (KT, r_KT[tt]))):
            P.op("pe", lambda e, w_=w_, qb_=qb_, bF=bF: e.transpose(C.psbv[bF][0:96, w_ * 128:(w_ + 1) * 128], qb_[:, w_, :],
                                                             C.ident[:]), reads=[r_qb, C.r_ident], writes=[C.rpb[bF]])
            P.op("act", lambda e, w_=w_, dst=dst, blk=blk, bF=bF: e.copy(dst[0:96, blk * 128:(blk + 1) * 128],
                                                                  C.psbv[bF][0:96, w_ * 128:(w_ + 1) * 128]),
                 reads=[C.rpb[bF]], writes=[r_d])

    for i in range(64 + 2):
        if i < 64:
            mla_A(i)
        if 0 <= i - 1 < 64:
            mla_B(i - 1)
        if 0 <= i - 2 < 64:
            mla_C(i - 2)
    attn_causal(96)
    store_y(2)

    for tt in range(NT):
        h, r_h = load_hT(tt)
        cols = slice(tt * 512, (tt + 1) * 512)
        proj_fm(h, r_h, C_DQ, 64, QT[0:64, cols], r_QT[tt], 0.125, 5)
        proj_fm(h, r_h, C_DK, 64, KT[0:64, cols], r_KT[tt], 1.0, 6)
        proj_v(h, r_h, C_DV, tt, V[0], r_V[0], 5)
    s_v = P.dsem()
    s_vp = {1: P.dsem(), 2: P.dsem()}
    P.dma("sp", lambda e: e.dma_start(out=vscr.rearrange("(b p) d -> p b d", p=128), in_=V[0][:, :, 0:64]),
          reads=r_V[0], writes=[r_vscr], sem=s_v)
    for pi, (win, dil) in enumerate(DIL[1:], start=1):
        L = S // dil
        src = vscr.rearrange("(cc i r) d -> i r cc d", i=128, r=dil)
        dstv = V[pi][:, :, 0:64].rearrange("p (r cc) d -> p r cc d", r=dil)
        for r in range(dil):
            P.dma("sp", lambda e, r=r, src=src, dstv=dstv: e.dma_start(out=dstv[:, r, :, :], in_=src[:, r, :, :]),
                  reads=[r_vscr], writes=r_V[pi], sem=s_vp[pi])

    def evac_dil(first):
        def f(ob, r, n0, lo, hi, first=first):
            raise NotImplementedError
        return f

    for pi, (win, dil) in enumerate(DIL):
        def evac(ob, r, n0, lo, hi, pi=pi, dil=dil):
            dst = acc[0:65, slice(r + dil * n0, r + dil * (n0 + 511) + 1, dil)]
            ra = [r_acc[i] for i in range(lo, hi + 1)]
            if pi == 0:
                P.op("act", lambda e: e.copy(dst, C.pb[ob][0:65, :]), reads=[C.rpb[ob]], writes=ra)
            else:
                P.op("dve", lambda e: e.tensor_tensor(out=dst, in0=dst, in1=C.pb[ob][0:65, :], op=ALU.add),
                     reads=[C.rpb[ob]] + ra, writes=ra)
        attn_banded(dil, 1 + pi, V[pi], r_V[pi], evac)
    for tq in range(NT):
        finalize(None, acc[0:65, tq * 512:(tq + 1) * 512], [r_acc[tq]], tq, False)
    store_y(3)
    return


def build_AB():
    nc = bass.Bass("TRN2", target_bir_lowering=False)
    C = Ctx(nc)
    T = {"hT": dram_in(nc, "hT", [4, D, TSH], BF16), "w_in": dram_in(nc, "w_in", [D, NCOL], F32),
         "wq_up": dram_in(nc, "wq_up", [256, 96], F32), "wkv_up": dram_in(nc, "wkv_up", [128, 128], F32),
         "qn": dram_in(nc, "qn", [256], F32), "kvn": dram_in(nc, "kvn", [128], F32),
         "scal": dram_in(nc, "scal", [1, 8], F32), "pos": dram_in(nc, "pos", [128, 64], I32),
         "cst": dram_in(nc, "cst", [128, 32], F32), "yT": dram_out(nc, "yT", [256, S], BF16)}
    emit_AB(C, T)
    C.P.wait_all("sp", [])
    C.P.emit()
    return nc


def ab_inputs_common(head):
    slopes = 2.0 ** (-8.0 * np.arange(1, 9, dtype=np.float64) / 8.0)
    c = np.zeros((128, 32), np.float32)
    c[:, 0:16] = (10000.0 ** (-np.arange(16, dtype=np.float32) / np.float32(16))).astype(np.float32)[None, :]
    c[:, 16] = -slopes[head]
    for pi, (win, dil) in enumerate(DIL):
        c[:, 17 + pi] = -slopes[4 + head] * dil
    return c


GROUPS = [[0, 1, 2, 3], [4, 5, 6, 7]]


def build_fused():
    nc = bass.Bass("TRN2", target_bir_lowering=False, num_devices=NCORE)
    C = Ctx(nc)
    P = C.P
    x = dram_in(nc, "x", [TSH, D], F32)
    idx = dram_in(nc, "idx", [1, 1], I32)
    pos = dram_in(nc, "pos", [128, 64], I32)
    cst = dram_in(nc, "cst", [128, 32], F32)
    g0 = dram_in(nc, "g0", [D], F32)
    L = []
    for l in range(2):
        L.append({"w_in": dram_in(nc, f"w_in{l}", [D, NCOL], F32), "wq_up": dram_in(nc, f"wq_up{l}", [256, 96], F32),
                  "wkv_up": dram_in(nc, f"wkv_up{l}", [128, 128], F32), "qn": dram_in(nc, f"qn{l}", [256], F32),
                  "kvn": dram_in(nc, f"kvn{l}", [128], F32), "scal": dram_in(nc, f"scal{l}", [1, 8], F32),
                  "w_out": dram_in(nc, f"w_out{l}", [D, D], F32), "g_ffn": dram_in(nc, f"g_ffn{l}", [D], F32),
                  "g_next": dram_in(nc, f"g_next{l}", [D], F32)})
    ffn = [{"wg": dram_in(nc, "wg0", [1, D, FF], F32), "wu": dram_in(nc, "wu0", [1, D, FF], F32),
            "wd": dram_in(nc, "wd0", [1, FF, D], F32)},
           {"wg": dram_in(nc, "wg1", [NE, D, FF], F32), "wu": dram_in(nc, "wu1", [NE, D, FF], F32),
            "wd": dram_in(nc, "wd1", [NE, FF, D], F32), "router": dram_in(nc, "router", [D, NE], F32)}]
    y = dram_out(nc, "y", [TSH, D], F32)
    hT_loc = nc.dram_tensor("hT_loc", [D, TSH], BF16)
    hT_all = nc.dram_tensor("hT_all", [4 * D, TSH], BF16)
    y_loc = nc.dram_tensor("y_loc", [256, S], BF16)
    y_all = nc.dram_tensor("y_all", [1024, S], BF16)
    x_mid = nc.dram_tensor("x_mid", [TSH, D], F32)
    mix_own = nc.dram_tensor("mix_own", [D, TSH], BF16)
    reg_tok = P.es.enter_context(nc.gpsimd.register("rtok"))
    reg_tmp = P.es.enter_context(nc.gpsimd.register("rtmp"))
    idx_sb, r_idx = C.tile([1, 1], I32)
    C.base = C.off
    P.dma("sp", lambda e: e.dma_start(out=idx_sb[:], in_=idx), writes=[r_idx], sem=P.dsem())

    def ld_idx(e):
        e.reg_load(reg_tok, idx_sb[0:1, 0:1])
        return e.reg_mul(reg_tok, reg_tok, TSH)
    P.op("pool", ld_idx, reads=[r_idx])
    dmy, r_dmy = C.tile([128, 16], F32)
    C.base = C.off
    P.op("pool", lambda e: e.memset(dmy[:], 0.0), writes=[r_dmy])
    FSTOP = int(os.environ.get("K_FSTOP", "99"))

    def finish():
        P.wait_all("sp", [])
        P.emit()
        return nc
    ncc = [0]

    ccs = [P.newsem("DCC1"), P.newsem("DCC2")]

    def allgather(src_t, dst_t, nrows, rc):
        P.barrier()
        r_cc = Res("cc")
        for i in range(nrows // rc):
            P.dma("pool", lambda e, i=i: e.collective_compute(
                "AllGather", ALU.bypass, replica_groups=GROUPS, ins=[src_t.ap()[i * rc:(i + 1) * rc, :]],
                outs=[dst_t.ap()[i * 4 * rc:(i + 1) * 4 * rc, :]]), writes=[r_cc], sem=ccs[i % 2], inc=1)
        P.barrier()
        C.reset()

    hT_view = hT_all.ap().rearrange("(kc s p) t -> s p kc t", kc=8, s=4)

    def hT_fn(sh, tl0):
        return hT_view[sh][:, :, tl0:tl0 + 512]

    emit_C(C, {"x": x, "g_next": g0, "hT": hT_loc.ap(), "x_out": None}, "N", False)
    if FSTOP == 0:
        return finish()
    allgather(hT_loc, hT_all, D, 128)
    if FSTOP == 1:
        return finish()
    for l in range(2):
        T = dict(L[l])
        r_ycc = Res("ycc")

        def after_store(mixer, r_st):
            for i in (2 * mixer, 2 * mixer + 1):
                P.dma("pool", lambda e, i=i: e.collective_compute(
                    "AllGather", ALU.bypass, replica_groups=GROUPS, ins=[y_loc.ap()[i * 32:(i + 1) * 32, :]],
                    outs=[y_all.ap()[i * 128:(i + 1) * 128, :]]), reads=[r_st], writes=[r_ycc], sem=ccs[i % 2], inc=1)
        T.update(hT=None, hT_fn=hT_fn, pos=pos, cst=cst, yT=y_loc.ap(), after_store=after_store)
        emit_AB(C, T)
        if FSTOP == 2:
            return finish()
        P.barrier()
        C.reset()
        if FSTOP == 3:
            return finish()
        T = dict(L[l])
        T.update(ffn[l])
        for half in range(2):
            def ld_own(e, half=half):
                e.reg_add(reg_tmp, reg_tok, half * 512 * S)
                src = bass.AP(y_all, reg_tmp, [[S, 512], [1, TSH]])
                return e.dma_start(out=mix_own.ap()[half * 512:(half + 1) * 512, :], in_=src)
            P.dma("pool", ld_own, sem=P.dsem())
        P.barrier()
        if FSTOP == 4:
            return finish()
        T.update(x=(x if l == 0 else x_mid.ap()), mixT=mix_own.ap())
        if l == 0:
            T.update(x_out=x_mid.ap(), hT=hT_loc.ap())
            emit_C(C, T, "dense", False)
            if FSTOP == 5:
                return finish()
            allgather(hT_loc, hT_all, D, 128)
        else:
            T["y"] = y
            emit_C(C, T, "moes", True)
    P.wait_all("sp", [])
    P.emit()
    return nc


_PROGS = {}


def _prog(key, fn):
    if key not in _PROGS:
        _PROGS[key] = fn()
    return _PROGS[key]


def _run(nc, maps):
    res = run_bass_kernel_spmd(nc, maps, core_ids=list(range(NCORE)))
    return res.results


def kernel_unfused(x, positions, attn_norm, w_in, b_forget, mla_q_norm, w_q_up, mla_kv_norm, w_kv_up, sinks, w_out, ffn_norm,
           dense_w_gate, dense_w_up, dense_w_down, router, moe_w_gate, moe_w_up, moe_w_down, final_norm):
    f32 = np.float32
    x = np.asarray(x, f32)
    cores = list(range(NCORE))
    bt = [(c // 4, c % 4) for c in cores]
    maps = [{"x": np.ascontiguousarray(x[b, j * TSH:(j + 1) * TSH]), "g_next": np.asarray(attn_norm[0], f32)}
            for (b, j) in bt]
    r = _run(_prog("N", lambda: build_C("N", False)), maps)
    x_cur = [rr["x_out"] for rr in r]
    hT = [rr["hT"] for rr in r]
    o_fq, o_fk, o_fv, o_ff, o_sq, o_sk, o_sv, o_cq, o_dq, o_dk, o_dv = 0, 256, 512, 768, 772, 1028, 1156, 1284, 1700, 1956, 2212
    pos = np.asarray(positions, np.int32)
    out = None
    for layer in range(2):
        wl = np.asarray(w_in[layer], f32)
        maps = []
        for (b, j) in bt:
            kv = j // 2
            cols = np.concatenate([
                np.arange(o_fq + j * 64, o_fq + (j + 1) * 64), np.arange(o_fk + j * 64, o_fk + (j + 1) * 64),
                np.arange(o_fv + j * 64, o_fv + (j + 1) * 64), np.arange(o_ff + j, o_ff + j + 1),
                np.arange(o_sq + j * 64, o_sq + (j + 1) * 64), np.arange(o_sk + kv * 64, o_sk + (kv + 1) * 64),
                np.arange(o_sv + kv * 64, o_sv + (kv + 1) * 64), np.arange(o_cq, o_cq + 416),
                np.arange(o_dq + j * 64, o_dq + (j + 1) * 64), np.arange(o_dk + j * 64, o_dk + (j + 1) * 64),
                np.arange(o_dv + j * 64, o_dv + (j + 1) * 64)])
            scal = np.zeros((1, 8), f32)
            scal[0, 0] = b_forget[layer][j]
            scal[0, 1] = sinks[layer][j]
            maps.append({
                "hT": np.ascontiguousarray(np.stack([hT[b * 4 + s] for s in range(4)], 0)),
                "w_in": np.ascontiguousarray(wl[:, cols]),
                "wq_up": np.ascontiguousarray(np.asarray(w_q_up[layer], f32)[:, j * 96:(j + 1) * 96]),
                "wkv_up": np.ascontiguousarray(np.asarray(w_kv_up[layer], f32)[:, j * 128:(j + 1) * 128]),
                "qn": np.asarray(mla_q_norm[layer], f32), "kvn": np.asarray(mla_kv_norm[layer], f32),
                "scal": scal, "pos": np.ascontiguousarray(pos[b].reshape(64, 128).T),
                "cst": ab_inputs_common(j)})
        r = _run(_prog("AB", build_AB), maps)
        yT = [rr["yT"] for rr in r]
        perm = np.array([m * 256 + j * 64 + d for j in range(4) for m in range(4) for d in range(64)])
        wo = np.ascontiguousarray(np.asarray(w_out[layer], f32)[perm])
        last = layer == 1
        maps = []
        for (b, j) in bt:
            mixT = np.ascontiguousarray(np.concatenate([yT[b * 4 + jj][:, j * TSH:(j + 1) * TSH] for jj in range(4)], 0))
            m = {"x": x_cur[b * 4 + j], "mixT": mixT, "w_out": wo, "g_ffn": np.asarray(ffn_norm[layer], f32),
                 "g_next": np.asarray(final_norm if last else attn_norm[layer + 1], f32)}
            if layer == 0:
                m.update(wg=np.asarray(dense_w_gate, f32), wu=np.asarray(dense_w_up, f32), wd=np.asarray(dense_w_down, f32))
            else:
                m.update(wg=np.asarray(moe_w_gate[0], f32), wu=np.asarray(moe_w_up[0], f32),
                         wd=np.asarray(moe_w_down[0], f32), router=np.asarray(router[0], f32))
            maps.append(m)
        if layer == 0:
            r = _run(_prog("Cd", lambda: build_C("dense", False)), maps)
            x_cur = [rr["x_out"] for rr in r]
            hT = [rr["hT"] for rr in r]
        else:
            r = _run(_prog("Cm", lambda: build_C("moe", True)), maps)
            out = np.zeros((NB, S, D), f32)
            for (b, j), rr in zip(bt, r):
                out[b, j * TSH:(j + 1) * TSH] = rr["y"]
    return out


def _core_cols(j):
    o_fq, o_fk, o_fv, o_ff, o_sq, o_sk, o_sv, o_cq, o_dq, o_dk, o_dv = 0, 256, 512, 768, 772, 1028, 1156, 1284, 1700, 1956, 2212
    kv = j // 2
    return np.concatenate([
        np.arange(o_fq + j * 64, o_fq + (j + 1) * 64), np.arange(o_fk + j * 64, o_fk + (j + 1) * 64),
        np.arange(o_fv + j * 64, o_fv + (j + 1) * 64), np.arange(o_ff + j, o_ff + j + 1),
        np.arange(o_sq + j * 64, o_sq + (j + 1) * 64), np.arange(o_sk + kv * 64, o_sk + (kv + 1) * 64),
        np.arange(o_sv + kv * 64, o_sv + (kv + 1) * 64), np.arange(o_cq, o_cq + 416),
        np.arange(o_dq + j * 64, o_dq + (j + 1) * 64), np.arange(o_dk + j * 64, o_dk + (j + 1) * 64),
        np.arange(o_dv + j * 64, o_dv + (j + 1) * 64)])


def kernel(x, positions, attn_norm, w_in, b_forget, mla_q_norm, w_q_up, mla_kv_norm, w_kv_up, sinks, w_out, ffn_norm,
           dense_w_gate, dense_w_up, dense_w_down, router, moe_w_gate, moe_w_up, moe_w_down, final_norm):
    f32 = np.float32
    x = np.asarray(x, f32)
    pos = np.asarray(positions, np.int32)
    perm = np.array([((c8 * 32 + r) // 64) * 256 + j * 64 + (c8 * 32 + r) % 64
                     for c8 in range(8) for j in range(4) for r in range(32)])
    wo = [np.ascontiguousarray(np.asarray(w_out[l], f32)[perm]) for l in range(2)]
    shared = {"g0": np.asarray(attn_norm[0], f32),
              "wg0": np.asarray(dense_w_gate, f32), "wu0": np.asarray(dense_w_up, f32), "wd0": np.asarray(dense_w_down, f32),
              "wg1": np.asarray(moe_w_gate[0], f32), "wu1": np.asarray(moe_w_up[0], f32), "wd1": np.asarray(moe_w_down[0], f32),
              "router": np.asarray(router[0], f32)}
    for l in range(2):
        shared[f"qn{l}"] = np.asarray(mla_q_norm[l], f32)
        shared[f"kvn{l}"] = np.asarray(mla_kv_norm[l], f32)
        shared[f"w_out{l}"] = wo[l]
        shared[f"g_ffn{l}"] = np.asarray(ffn_norm[l], f32)
        shared[f"g_next{l}"] = np.asarray(attn_norm[1] if l == 0 else final_norm, f32)
    maps = []
    for c in range(NCORE):
        b, j = c // 4, c % 4
        m = dict(shared)
        m["x"] = np.ascontiguousarray(x[b, j * TSH:(j + 1) * TSH])
        m["idx"] = np.array([[j]], np.int32)
        m["pos"] = np.ascontiguousarray(pos[b].reshape(64, 128).T)
        m["cst"] = ab_inputs_common(j)
        cols = _core_cols(j)
        for l in range(2):
            m[f"w_in{l}"] = np.ascontiguousarray(np.asarray(w_in[l], f32)[:, cols])
            m[f"wq_up{l}"] = np.ascontiguousarray(np.asarray(w_q_up[l], f32)[:, j * 96:(j + 1) * 96])
            m[f"wkv_up{l}"] = np.ascontiguousarray(np.asarray(w_kv_up[l], f32)[:, j * 128:(j + 1) * 128])
            sc = np.zeros((1, 8), f32)
            sc[0, 0] = b_forget[l][j]
            sc[0, 1] = sinks[l][j]
            m[f"scal{l}"] = sc
        maps.append(m)
    r = _run(_prog("fused", build_fused), maps)
    out = np.zeros((NB, S, D), f32)
    for c in range(NCORE):
        out[c // 4, (c % 4) * TSH:(c % 4 + 1) * TSH] = r[c]["y"]
    return out
```

```python
import contextlib
import os
import numpy as np
import ml_dtypes
import concourse.bass as bass
import concourse.mybir as mybir
from concourse.bass_utils import run_bass_kernel_spmd

F32 = mybir.dt.float32
BF16 = mybir.dt.bfloat16
I32 = mybir.dt.int32
AF = mybir.ActivationFunctionType
ALU = mybir.AluOpType
AX = mybir.AxisListType

D = 1024
S = 8192
NB = 2
NCORE = 8
TSH = 2048
FF = 3584
NFC = FF // 128
NE = 8
EPS = 1e-6
NCOL = 993
C_FQ, C_FK, C_FV, C_FF = 0, 64, 128, 192
C_SQ, C_SK, C_SV = 193, 257, 321
C_CQ = 385
C_DQ, C_DK, C_DV = 801, 865, 929
DIL = ((128, 1), (512, 4), (2048, 16))
PI = float(np.pi)
DBG2 = int(os.environ.get('K_DBG2', '0'))


class Res:
    __slots__ = ("name", "writer", "readers")

    def __init__(self, name):
        self.name = name
        self.writer = None
        self.readers = {}


class Prog:
    ENGS = ("pe", "act", "dve", "pool", "sp")

    def __init__(self, nc):
        self.nc = nc
        self.es = contextlib.ExitStack()
        self.streams = {e: [] for e in self.ENGS}
        self.count = {}
        self.sem = {}
        self.waited = {e: {} for e in self.ENGS}
        for e in self.ENGS:
            self.newsem("E_" + e)
        self.n_ops = 0
        self.nds = 0

    def newsem(self, name):
        self.sem[name] = self.es.enter_context(self.nc.semaphore(name))
        self.count[name] = 0
        return name

    def dsem(self, sw=False):
        fl = getattr(self, "free_sw" if sw else "free_d", None)
        if fl:
            return fl.pop()
        self.nds += 1
        return self.newsem(f"DS{self.nds}" if sw else f"D{self.nds}")

    def cond_begin(self, flag_ap, r_flag):
        if not hasattr(self, "flag_regs"):
            nc = self.nc
            engs = {"pe": nc.tensor, "act": nc.scalar, "dve": nc.vector, "pool": nc.gpsimd, "sp": nc.sync}
            self.flag_regs = {k: self.es.enter_context(v.register(f"flag_{k}")) for k, v in engs.items()}
        self._cond_snap = {e: dict(self.waited[e]) for e in self.ENGS}
        for e in self.ENGS:
            if r_flag.writer:
                self._waits(e, {r_flag.writer})
            self.streams[e].append(("if", flag_ap))

    def cond_end(self):
        for e in self.ENGS:
            self.streams[e].append(("endif",))
            self.waited[e] = self._cond_snap[e]

    def barrier(self):
        toks = set((k, v) for k, v in self.count.items() if v > 0)
        for e in self.ENGS:
            self._waits(e, toks)
        self.free_d = sorted([k for k in self.count if k.startswith("D") and not k.startswith("DCC")
                              and not k.startswith("DS")], reverse=True)
        self.free_sw = sorted([k for k in self.count if k.startswith("DS")], reverse=True)

    def sbuf(self, name, shape, dtype):
        return self.es.enter_context(self.nc.sbuf_tensor(name, list(shape), dtype))

    def psum(self, name, shape, dtype):
        return self.es.enter_context(self.nc.psum_tensor(name, list(shape), dtype))

    def _waits(self, eng, toks):
        for (s, v) in sorted(toks):
            if s == "E_pe" and eng == "pe":
                continue
            if self.waited[eng].get(s, 0) >= v:
                continue
            self.waited[eng][s] = v
            self.streams[eng].append(("wait", s, v))

    def _deps(self, reads, writes, own=None):
        toks = set()
        for r in reads:
            if r.writer:
                toks.add(r.writer)
        for w in writes:
            if w.writer and w.writer[0] != own:
                toks.add(w.writer)
            for t in w.readers.values():
                toks.add(t)
        return toks

    def op(self, eng, fn, reads=(), writes=(), signal=True):
        self._waits(eng, self._deps(reads, writes))
        s = "E_" + eng
        self.count[s] += 1
        tok = (s, self.count[s])
        self.streams[eng].append(("op", fn, s, self.count[s]))
        for r in reads:
            r.readers[s] = tok
        for w in writes:
            w.writer = tok
            w.readers = {}
        self.n_ops += 1

    def dma(self, q, fn, reads=(), writes=(), sem=None, inc=16):
        self._waits(q, self._deps(reads, writes, own=sem))
        self.count[sem] += inc
        tok = (sem, self.count[sem])
        self.streams[q].append(("op", fn, sem, inc, self.count[sem]))
        for r in reads:
            r.readers[sem] = tok
        for w in writes:
            w.writer = tok
            w.readers = {}
        self.n_ops += 1

    def wait_all(self, eng, resources):
        toks = set()
        for r in resources:
            if r.writer:
                toks.add(r.writer)
        self._waits(eng, toks)
        self._waits(eng, set((k, v) for k, v in self.count.items() if k.startswith("D") and v > 0))

    def emit(self):
        nc = self.nc
        streams = self.streams
        sem = self.sem

        import bisect
        needed = {}
        for lst in streams.values():
            for it in lst:
                if it[0] == "wait" and it[1].startswith("E_"):
                    needed.setdefault(it[1], set()).add(it[2])
        order = {k: sorted(v) for k, v in needed.items()}

        def phys(sname, v):
            return bisect.bisect_right(order[sname], v)

        def run_items(e, ename, lst):
            i = 0
            while i < len(lst):
                it = lst[i]
                if it[0] == "wait":
                    if it[1].startswith("E_"):
                        e.wait_ge(sem[it[1]], phys(it[1], it[2]))
                    else:
                        e.wait_ge(sem[it[1]], it[2])
                elif it[0] == "if":
                    depth, j = 1, i + 1
                    while depth:
                        if lst[j][0] == "if":
                            depth += 1
                        elif lst[j][0] == "endif":
                            depth -= 1
                        j += 1
                    body = lst[i + 1:j - 1]
                    incs = {}
                    before = {}
                    for b in body:
                        if b[0] == "op":
                            if b[2].startswith("E_"):
                                if b[3] in needed.get(b[2], ()):
                                    incs[b[2]] = incs.get(b[2], 0) + 1
                                    if b[2] not in before:
                                        before[b[2]] = bisect.bisect_left(order[b[2]], b[3])
                            else:
                                incs[b[2]] = incs.get(b[2], 0) + b[3]
                                if b[2] not in before:
                                    before[b[2]] = b[4] - b[3]
                    if any(b[0] == "op" for b in body):
                        reg = self.flag_regs[ename]
                        e.reg_load(reg, it[1])
                        with e.If(reg):
                            run_items(e, ename, body)
                        if incs:
                            with e.Else():
                                for k in sorted(incs):
                                    if before[k] > 0:
                                        e.wait_ge(sem[k], before[k])
                                for k in sorted(incs):
                                    e.sem_inc(sem[k], incs[k])
                    i = j - 1
                elif it[0] == "op":
                    ins = it[1](e)
                    if it[2].startswith("E_"):
                        if it[3] in needed.get(it[2], ()):
                            ins.then_inc(sem[it[2]], 1)
                    else:
                        ins.then_inc(sem[it[2]], it[3])
                i += 1

        def replay(e, lst):
            run_items(e, self._cur, lst)

        with nc.Block() as block:
            @block.tensor
            def _(e):
                self._cur = "pe"
                replay(e, streams["pe"])

            @block.scalar
            def _(e):
                self._cur = "act"
                replay(e, streams["act"])

            @block.vector
            def _(e):
                self._cur = "dve"
                replay(e, streams["dve"])

            @block.gpsimd
            def _(e):
                self._cur = "pool"
                replay(e, streams["pool"])

            @block.sync
            def _(e):
                self._cur = "sp"
                replay(e, streams["sp"])
        self.es.close()


ARENA_BYTES = 212736
_ESZ = {F32: 4, BF16: 2, I32: 4}


class Ctx:
    def __init__(self, nc):
        self.nc = nc
        self.P = Prog(nc)
        P = self.P
        self.pb = [P.psum(f"pb{i}", [128, 512], F32) for i in range(8)]
        self.rpb = [Res(f"pb{i}") for i in range(8)]
        self.psb = self.pb[7][:].bitcast(BF16)
        self.psbv = [self.pb[i][:].bitcast(BF16) for i in range(8)]
        self.nt = 0
        self.arena = P.sbuf("arena", [128, ARENA_BYTES // 2], BF16)
        self.off = 0
        self.uid = 0
        self.ident, self.r_ident = self.tile([128, 128], BF16)
        self.junk, self.r_junk = self.tile([128, 1024], BF16)
        self.base = self.off
        idf, r_idf = self.tile([128, 128], F32)
        P.op("pool", lambda e: e.memset(idf[:], 1.0), writes=[r_idf])
        P.op("pool", lambda e: e.affine_select(out=idf[:], in_=idf[:], pattern=[[-1, 128]], compare_op=ALU.is_equal,
                                               fill=0.0, base=0, channel_multiplier=1), reads=[r_idf], writes=[r_idf])
        P.op("dve", lambda e: e.tensor_copy(self.ident[:], idf[:]), reads=[r_idf], writes=[self.r_ident])
        P.barrier()
        self.off = self.base

    def reset(self):
        self.off = self.base

    def tile(self, shape, dt, name=None):
        self.nt += 1
        nm = f"{name or 't'}{self.nt}"
        n = 1
        for d_ in shape[1:]:
            n *= d_
        nbytes = n * _ESZ[dt]
        off = (self.off + 63) // 64 * 64
        self.off = off + nbytes
        assert self.off <= ARENA_BYTES, f"SBUF arena overflow allocating {nm} {shape}: {self.off}"
        ap = self.arena[0:shape[0], off // 2:(off + nbytes) // 2]
        if dt != BF16:
            ap = ap.bitcast(dt)
        if len(shape) == 3:
            ap = ap.rearrange("p (a b) -> p a b", a=shape[1])
        elif len(shape) == 4:
            ap = ap.rearrange("p (a b c) -> p a b c", a=shape[1], b=shape[2])
        return ap, Res(nm)

    def ring(self, key, n, shape, dt, with_sem=False, sw=False):
        bufs = []
        for i in range(n):
            t, r = self.tile(shape, dt, name=key)
            bufs.append((t, r, self.P.dsem(sw=sw)) if with_sem else (t, r))
        state = {"i": 0}

        def nxt():
            b = bufs[state["i"] % n]
            state["i"] += 1
            return b
        return nxt


def dram_in(nc, name, shape, dt):
    return nc.dram_tensor(name, list(shape), dt, kind="ExternalInput").ap()


def dram_out(nc, name, shape, dt):
    return nc.dram_tensor(name, list(shape), dt, kind="ExternalOutput").ap()


def emit_rstd(C, x_ap, r_x, ss, r_ss, n, width_ap=None):
    P = C.P
    P.op("dve", lambda e: e.memset(ss, 0.0), writes=[r_ss])
    P.op("act", lambda e: e.activation(out=C.junk[:, 0:n], in_=x_ap, func=AF.Square, accum_out=ss),
         reads=[r_x, r_ss], writes=[C.r_junk, r_ss])
    P.op("dve", lambda e: e.tensor_scalar(out=ss, in0=ss, scalar1=1.0 / n, scalar2=EPS, op0=ALU.mult, op1=ALU.add),
         reads=[r_ss], writes=[r_ss])
    P.op("act", lambda e: e.activation(out=ss, in_=ss, func=AF.Sqrt), reads=[r_ss], writes=[r_ss])
    P.op("dve", lambda e: e.reciprocal(ss, ss), reads=[r_ss], writes=[r_ss])


def emit_transposes_to(C, hb, r_hb, dst_ap, r_dst, nchunk, eng="act", bank=7):
    P = C.P
    psb = C.psbv[bank]
    for k in range(nchunk):
        P.op("pe", lambda e, k=k: e.transpose(psb[:, k * 128:(k + 1) * 128], hb[:, k * 128:(k + 1) * 128], C.ident[:]),
             reads=[r_hb, C.r_ident], writes=[C.rpb[bank]], signal=(k == nchunk - 1))
    src = psb[:, 0:nchunk * 128].rearrange("p (k t) -> p k t", k=nchunk)
    if eng == "act":
        P.op("act", lambda e: e.copy(dst_ap, src), reads=[C.rpb[bank]], writes=[r_dst])
    else:
        P.op("dve", lambda e: e.tensor_copy(dst_ap, src), reads=[C.rpb[bank]], writes=[r_dst])


def emit_C(C, T, mode, last):
    nc = C.nc
    P = C.P
    x_in = T["x"]
    g_next = T["g_next"]
    full = mode != "N"
    sparse = mode == "moes"
    if sparse:
        mode = "moe"
    ne = NE if mode == "moe" else 1
    if full:
        mixT = T.get("mixT")
        w_out = T["w_out"]
        g_ffn = T["g_ffn"]
        wg, wu, wd = T["wg"], T["wu"], T["wd"]
        if mode == "moe":
            router = T["router"]
    if last:
        y_out = T["y"]
    else:
        x_out = T.get("x_out")
        hT_out = T["hT"]
    d_out = Res("d_out")
    x_blk = x_in.rearrange("(b p) d -> p b d", p=128)
    NTB = TSH // 128

    NX = {}

    def alloc_next():
        gbn_, r_gbn_ = C.tile([128, D], F32)
        P.dma("sp", lambda e: e.dma_start(out=gbn_[:], in_=g_next.partition_broadcast(128)), writes=[r_gbn_],
              sem=P.dsem())
        NX["gbn"], NX["r_gbn"] = gbn_, r_gbn_
        if last:
            NX["outr"] = C.ring("yo", 2, [128, D], F32, with_sem=True)
    defer_next = (mode == "moes") and last
    if not defer_next:
        alloc_next()
    ssr = C.ring("ss", 3, [128, 1], F32)
    hbr = None if (mode == "moes") else C.ring("hb", 3, [128, D], BF16)
    stg = None if last else C.ring("stg", 2, [128, 8, 128], BF16, with_sem=True)
    hT_dram = None if last else hT_out.rearrange("(kc p) t -> p kc t", p=128)

    def emit_next(xa, r_xa, tb):
        ss, r_ss = ssr()
        emit_rstd(C, xa, r_xa, ss[:], r_ss, D)
        if last:
            yo, r_yo, s_yo = NX["outr"]()
            P.op("dve", lambda e: e.scalar_tensor_tensor(out=yo[:], in0=xa, scalar=ss[:], in1=NX["gbn"][:], op0=ALU.mult,
                                                          op1=ALU.mult), reads=[r_xa, r_ss, NX["r_gbn"]], writes=[r_yo])
            P.dma("sp", lambda e: e.dma_start(out=y_out[tb * 128:(tb + 1) * 128, :], in_=yo[:]), reads=[r_yo],
                  writes=[d_out], sem=s_yo)
        else:
            hb, r_hb = hbr()
            P.op("dve", lambda e: e.scalar_tensor_tensor(out=hb[:], in0=xa, scalar=ss[:], in1=NX["gbn"][:], op0=ALU.mult,
                                                          op1=ALU.mult), reads=[r_xa, r_ss, NX["r_gbn"]], writes=[r_hb])
            st, r_st, s_st = stg()
            emit_transposes_to(C, hb, r_hb, st[:], r_st, 8)
            P.dma("sp", lambda e: e.dma_start(out=hT_dram[:, :, tb * 128:(tb + 1) * 128], in_=st[:]), reads=[r_st],
                  writes=[d_out], sem=s_st)

    if not full:
        xr = C.ring("xs", 3, [128, D], F32, with_sem=True)
        for tb in range(NTB):
            xt, r_xt, s_xt = xr()
            P.dma("sp", lambda e, tb=tb, xt=xt: e.dma_start(out=xt[:], in_=x_blk[:, tb, :]), writes=[r_xt], sem=s_xt)
            emit_next(xt[:], r_xt, tb)
            if x_out is not None:
                P.dma("sp", lambda e, tb=tb, xt=xt: e.dma_start(out=x_out[tb * 128:(tb + 1) * 128, :], in_=xt[:]),
                      reads=[r_xt], writes=[d_out], sem=s_xt)
        return

    C.uid += 1
    x1s = nc.dram_tensor(f"x1s{C.uid}", [TSH, D], F32).ap()
    r_x1s = [Res(f"x1s{i}") for i in range(NTB)]
    if sparse:
        h_tm, _ = C.tile([128, NTB, D], BF16)
        r_htm = [Res(f"htm{i}") for i in range(NTB)]
        comb, _ = C.tile([128, NTB, NE], F32)
        r_comb = [Res(f"comb{i}") for i in range(NTB)]
        rkm, r_rkm = C.tile([128, NTB, NE], F32)
        chh, r_chh = C.tile([128, NTB, NE], BF16)
        cll, r_cll = C.tile([128, NTB, NE], BF16)
        io_f, r_iof = C.tile([128, 128], BF16)
        flags_i, r_flags = C.tile([1, 32], I32)
        C.mark = C.off
    gbf, r_gbf = C.tile([128, D], F32)
    P.dma("sp", lambda e: e.dma_start(out=gbf[:], in_=g_ffn.partition_broadcast(128)), writes=[r_gbf], sem=P.dsem())
    wo, r_wo = C.tile([128, 8, D], BF16)
    s_wo = P.dsem(sw=True)
    P.dma("pool", lambda e: e.dma_start(out=wo[:], in_=w_out.rearrange("(kc p) n -> p kc n", p=128)), writes=[r_wo],
          sem=s_wo)
    if not sparse:
        hT, _ = C.tile([128, 8, TSH], BF16)
        r_hT = [Res(f"hT{i}") for i in range(NTB)]
    else:
        hir = C.ring("hiT", 2, [128, 8, 128], BF16)
    if mode == "moe":
        rt_f, r_rtf = C.tile([128, 8, NE], F32)
        rt_hi, r_rthi = C.tile([128, 8, NE], BF16)
        rt_lo, r_rtlo = C.tile([128, 8, NE], BF16)
        P.dma("sp", lambda e: e.dma_start(out=rt_f[:], in_=router.rearrange("(kc p) n -> p kc n", p=128)),
              writes=[r_rtf], sem=P.dsem())
        P.op("dve", lambda e: e.tensor_copy(rt_hi[:], rt_f[:]), reads=[r_rtf], writes=[r_rthi])
        P.op("dve", lambda e: e.tensor_tensor(out=rt_lo[:], in0=rt_f[:], in1=rt_hi[:], op=ALU.subtract),
             reads=[r_rtf, r_rthi], writes=[r_rtlo])
        if not sparse:
            comb, _ = C.tile([128, NTB, NE], F32)
            r_comb = [Res(f"comb{i}") for i in range(NTB)]
        hfr = C.ring("hf", 1, [128, D], F32)
        hlr = C.ring("hl", 2, [128, D], BF16)
        lor = C.ring("loT", 1, [128, 8, 128], BF16)
        m8r = C.ring("m8", 2, [128, 8], F32)
        smr = C.ring("sm", 2, [128, 8], F32)

    xr = C.ring("xs", 2, [128, D], F32, with_sem=True)
    mxr = C.ring("mx", 2, [128, 8, 128], BF16, with_sem=True)
    mix_v = None if mixT is None else mixT.rearrange("(kc p) t -> p kc t", p=128)

    st1 = {}

    def stage1_a(tb):
        xt, r_xt, s_xt = xr()
        P.dma("sp", lambda e, tb=tb, xt=xt: e.dma_start(out=xt[:], in_=x_blk[:, tb, :]), writes=[r_xt], sem=s_xt)
        mx, r_mx, s_mx = mxr()
        if mix_v is not None:
            P.dma("sp", lambda e, tb=tb, mx=mx: e.dma_start(out=mx[:], in_=mix_v[:, :, tb * 128:(tb + 1) * 128]),
                  writes=[r_mx], sem=s_mx)
        else:
            def ld_mix(e, tb=tb, mx=mx):
                e.reg_add(T["reg_tmp"], T["reg_tok"], tb * 128)
                src = bass.AP(T["mix_all"], T["reg_tmp"], [[S, 128], [128 * S, 8], [1, 128]])
                return e.dma_start(out=mx[:], in_=src)
            P.dma("pool", ld_mix, reads=T["mix_reads"], writes=[r_mx], sem=s_mx)
        for hf_ in range(2):
            pbk = tb % 2 * 2 + hf_
            for kc in range(8):
                P.op("pe", lambda e, kc=kc, hf_=hf_, pbk=pbk, mx=mx: e.matmul(
                    C.pb[pbk][:, :], lhsT=mx[:, kc, :], rhs=wo[:, kc, hf_ * 512:(hf_ + 1) * 512],
                    start=(kc == 0), stop=(kc == 7)), reads=[r_mx, r_wo], writes=[C.rpb[pbk]], signal=(kc == 7))
            P.op("dve", lambda e, hf_=hf_, pbk=pbk, xt=xt: e.tensor_tensor(
                out=xt[:, hf_ * 512:(hf_ + 1) * 512], in0=xt[:, hf_ * 512:(hf_ + 1) * 512], in1=C.pb[pbk][:, :],
                op=ALU.add), reads=[r_xt, C.rpb[pbk]], writes=[r_xt])
        P.dma("sp", lambda e, tb=tb, xt=xt: e.dma_start(out=x1s[tb * 128:(tb + 1) * 128, :], in_=xt[:]),
              reads=[r_xt], writes=[r_x1s[tb]], sem=s_xt)
        ss, r_ss = ssr()
        emit_rstd(C, xt[:], r_xt, ss[:], r_ss, D)
        hb, r_hb = hbr() if not sparse else (h_tm[:, tb, :], r_htm[tb])
        if mode != "moe":
            P.op("dve", lambda e, xt=xt, ss=ss, hb=hb: e.scalar_tensor_tensor(
                out=hb[:], in0=xt[:], scalar=ss[:], in1=gbf[:], op0=ALU.mult, op1=ALU.mult),
                reads=[r_xt, r_ss, r_gbf], writes=[r_hb])
            st1[tb] = (hb, r_hb, None, None)
        else:
            hf, r_hf = hfr()
            hl, r_hl = hlr()
            P.op("dve", lambda e, xt=xt, ss=ss, hf=hf: e.scalar_tensor_tensor(
                out=hf[:], in0=xt[:], scalar=ss[:], in1=gbf[:], op0=ALU.mult, op1=ALU.mult),
                reads=[r_xt, r_ss, r_gbf], writes=[r_hf])
            P.op("dve", lambda e, hf=hf, hb=hb: e.tensor_copy(hb[:], hf[:]), reads=[r_hf], writes=[r_hb])
            P.op("dve", lambda e, hf=hf, hb=hb, hl=hl: e.tensor_tensor(out=hl[:], in0=hf[:], in1=hb[:], op=ALU.subtract),
                 reads=[r_hf, r_hb], writes=[r_hl])
            st1[tb] = (hb, r_hb, hl, r_hl)

    def stage1_b(tb):
        hb, r_hb, hl, r_hl = st1.pop(tb)
        if sparse:
            hi_t, r_hi = hir()
            emit_transposes_to(C, hb, r_hb, hi_t[:], r_hi, 8)
        else:
            emit_transposes_to(C, hb, r_hb, hT[:, :, tb * 128:(tb + 1) * 128], r_hT[tb], 8)
            hi_t, r_hi = hT[:, :, tb * 128:(tb + 1) * 128], r_hT[tb]
        if mode == "moe":
            lo, r_lo = lor()
            emit_transposes_to(C, hl, r_hl, lo[:], r_lo, 8, eng="dve")
            n = 0
            for kc in range(8):
                for (a, ra, b_, rb) in ((hi_t[:, kc, :], r_hi, rt_hi, r_rthi),
                                        (lo[:, kc, :], r_lo, rt_hi, r_rthi),
                                        (hi_t[:, kc, :], r_hi, rt_lo, r_rtlo)):
                    P.op("pe", lambda e, a=a, b_=b_, kc=kc, n=n: e.matmul(
                        C.pb[4][:, 0:NE], lhsT=a, rhs=b_[:, kc, :], start=(n == 0), stop=(n == 23)),
                        reads=[ra, rb], writes=[C.rpb[4]], signal=(n == 23))
                    n += 1
            lg, r_lg = smr()
            m8, r_m8 = m8r()
            cb = comb[:, tb, :]
            P.op("act", lambda e, lg=lg: e.copy(lg[:], C.pb[4][:, 0:NE]), reads=[C.rpb[4]], writes=[r_lg])
            P.op("dve", lambda e, lg=lg, m8=m8: e.max(out=m8[:], in_=lg[:]), reads=[r_lg], writes=[r_m8])
            P.op("dve", lambda e, lg=lg, m8=m8, cb=cb: e.tensor_scalar(
                out=cb, in0=lg[:], scalar1=m8[:, 1:2], scalar2=None, op0=ALU.is_ge),
                reads=[r_lg, r_m8], writes=[r_comb[tb]])
            P.op("dve", lambda e, lg=lg, m8=m8: e.tensor_scalar(
                out=lg[:], in0=lg[:], scalar1=m8[:, 0:1], scalar2=None, op0=ALU.subtract),
                reads=[r_lg, r_m8], writes=[r_lg])
            P.op("act", lambda e, lg=lg: e.activation(out=lg[:], in_=lg[:], func=AF.Exp), reads=[r_lg], writes=[r_lg])
            P.op("dve", lambda e, lg=lg, cb=cb: e.tensor_tensor(out=cb, in0=cb, in1=lg[:], op=ALU.mult),
                 reads=[r_lg, r_comb[tb]], writes=[r_comb[tb]])
            P.op("dve", lambda e, m8=m8, cb=cb: e.tensor_reduce(out=m8[:, 2:3], in_=cb, axis=AX.X, op=ALU.add),
                 reads=[r_comb[tb], r_m8], writes=[r_m8])
            P.op("dve", lambda e, m8=m8: e.reciprocal(m8[:, 2:3], m8[:, 2:3]), reads=[r_m8], writes=[r_m8])
            P.op("dve", lambda e, m8=m8, cb=cb: e.tensor_scalar(
                out=cb, in0=cb, scalar1=m8[:, 2:3], scalar2=None, op0=ALU.mult),
                reads=[r_comb[tb], r_m8], writes=[r_comb[tb]])


    for i in range(NTB + 1):
        if i < NTB:
            stage1_a(i)
        if i >= 1:
            stage1_b(i - 1)

    if sparse:
        cflat = comb[:].rearrange("p t e -> p (t e)")
        rkf = rkm[:].rearrange("p t e -> p (t e)")
        maskf, r_maskf = C.tile([128, 128], F32)
        maskb, r_maskb = C.tile([128, 128], BF16)
        Uf, r_Uf = C.tile([128, 128], F32)
        Ub, r_Ub = C.tile([128, 128], BF16)
        oneb, r_oneb = C.tile([128, 128], BF16)
        tot, r_tot = C.tile([128, NTB, NE], F32)
        off, r_off = C.tile([128, NTB, NE], F32)
        tmpm, r_tmpm = C.tile([128, 128], F32)
        ne_t, r_net = C.tile([128, NE], F32)
        flagf, r_flagf = C.tile([1, 32], F32)
        io_i, r_ioi = C.tile([128, 128], I32)
        P.op("dve", lambda e: e.tensor_scalar(out=maskf[:], in0=cflat, scalar1=0.0, scalar2=None, op0=ALU.is_gt),
             reads=r_comb, writes=[r_maskf])
        P.op("dve", lambda e: e.tensor_copy(maskb[:], maskf[:]), reads=[r_maskf], writes=[r_maskb])
        P.op("pool", lambda e: e.memset(Uf[:], 1.0), writes=[r_Uf])
        P.op("pool", lambda e: e.affine_select(out=Uf[:], in_=Uf[:], pattern=[[1, 128]], compare_op=ALU.is_ge, fill=0.0,
                                               base=-1, channel_multiplier=-1), reads=[r_Uf], writes=[r_Uf])
        P.op("dve", lambda e: e.tensor_copy(Ub[:], Uf[:]), reads=[r_Uf], writes=[r_Ub])
        P.op("dve", lambda e: e.memset(oneb[:], 1.0), writes=[r_oneb])
        P.op("pe", lambda e: e.matmul(C.pb[0][:, 0:128], lhsT=Ub[:], rhs=maskb[:], start=True, stop=True),
             reads=[r_Ub, r_maskb], writes=[C.rpb[0]])
        P.op("pe", lambda e: e.matmul(C.pb[1][:, 0:128], lhsT=oneb[:], rhs=maskb[:], start=True, stop=True),
             reads=[r_oneb, r_maskb], writes=[C.rpb[1]])
        P.op("act", lambda e: e.copy(rkf, C.pb[0][:, 0:128]), reads=[C.rpb[0]], writes=[r_rkm])
        P.op("act", lambda e: e.copy(tot[:].rearrange("p t e -> p (t e)"), C.pb[1][:, 0:128]), reads=[C.rpb[1]],
             writes=[r_tot])
        P.op("dve", lambda e: e.memset(off[:, 0, :], 0.0), writes=[r_off])
        for tb in range(1, NTB):
            P.op("dve", lambda e, tb=tb: e.tensor_tensor(out=off[:, tb, :], in0=off[:, tb - 1, :], in1=tot[:, tb - 1, :],
                                                         op=ALU.add), reads=[r_off, r_tot], writes=[r_off])
        P.op("dve", lambda e: e.tensor_tensor(out=rkf, in0=rkf, in1=off[:].rearrange("p t e -> p (t e)"), op=ALU.add),
             reads=[r_rkm, r_off], writes=[r_rkm])
        P.op("dve", lambda e: e.tensor_scalar(out=tmpm[:], in0=maskf[:], scalar1=-1.0, scalar2=1e9, op0=ALU.add,
                                              op1=ALU.mult), reads=[r_maskf], writes=[r_tmpm])
        P.op("dve", lambda e: e.tensor_tensor(out=rkf, in0=rkf, in1=maskf[:], op=ALU.mult), reads=[r_rkm, r_maskf],
             writes=[r_rkm])
        P.op("dve", lambda e: e.tensor_tensor(out=rkf, in0=rkf, in1=tmpm[:], op=ALU.add), reads=[r_rkm, r_tmpm],
             writes=[r_rkm])
        P.op("dve", lambda e: e.tensor_tensor(out=ne_t[:], in0=off[:, NTB - 1, :], in1=tot[:, NTB - 1, :], op=ALU.add),
             reads=[r_off, r_tot], writes=[r_net])
        PC = 640
        for p_ in range(4):
            P.op("dve", lambda e, p_=p_: e.tensor_scalar(out=flagf[0:1, p_ * 8:(p_ + 1) * 8], in0=ne_t[0:1, :],
                                                         scalar1=float(PC * p_), scalar2=None, op0=ALU.is_gt),
                 reads=[r_net], writes=[r_flagf])
        P.op("dve", lambda e: e.tensor_copy(flags_i[:], flagf[:]), reads=[r_flagf], writes=[r_flags])
        P.op("dve", lambda e: e.tensor_copy(chh[:].rearrange("p t e -> p (t e)"), cflat), reads=r_comb, writes=[r_chh])
        P.op("dve", lambda e: e.tensor_tensor(out=tmpm[:], in0=cflat, in1=chh[:].rearrange("p t e -> p (t e)"),
                                              op=ALU.subtract), reads=r_comb + [r_chh, r_tmpm], writes=[r_tmpm])
        P.op("dve", lambda e: e.tensor_copy(cll[:].rearrange("p t e -> p (t e)"), tmpm[:]), reads=[r_tmpm], writes=[r_cll])
        P.op("pool", lambda e: e.iota(io_i[:], pattern=[[1, 128]], base=0, channel_multiplier=0), writes=[r_ioi])
        P.op("dve", lambda e: e.tensor_copy(io_f[:], io_i[:]), reads=[r_ioi], writes=[r_iof])
        P.barrier()
        C.off = C.mark
        NSB = PC // 128
        xacc, _ = C.tile([128, NTB, D], F32)
        r_xacc = [Res(f"xacc{i}") for i in range(NTB)]
        mark2 = C.off
        for tb in range(NTB):
            P.dma("sp", lambda e, tb=tb: e.dma_start(out=xacc[:, tb, :], in_=x1s[tb * 128:(tb + 1) * 128, :]),
                  reads=[r_x1s[tb]], writes=[r_xacc[tb]], sem=P.dsem())
        hTe, r_hTe = C.tile([128, 8, PC], BF16)
        actT, _ = C.tile([128, NFC, PC], BF16)
        r_act = [Res(f"act{f}") for f in range(NFC)]
        PmT, _ = C.tile([128, NSB, TSH], BF16)
        r_PmT = [Res(f"PmT{i}") for i in range(NSB)]
        yg, _ = C.tile([128, NSB, D], BF16)
        r_yg = [Res(f"yg{i}") for i in range(NSB)]
        gs, r_gs = C.tile([128, NSB], F32)
        gtr = C.ring("gt", 1, [128, 2], F32)
        pmr = C.ring("Pm", 1, [128, NTB, 128], BF16)
        wgr = C.ring("wg", 2, [128, 8, 128], BF16, with_sem=True, sw=True)
        wur = C.ring("wu", 2, [128, 8, 128], BF16, with_sem=True, sw=True)
        wdr = C.ring("wd", 2, [128, 512], BF16, with_sem=True, sw=True)
        for ex in range(NE):
            wg_v = wg[ex].rearrange("(kc p) f -> p kc f", p=128)
            wu_v = wu[ex].rearrange("(kc p) f -> p kc f", p=128)
            for p_ in range(4):
                P.cond_begin(flags_i[0:1, p_ * 8 + ex:p_ * 8 + ex + 1], r_flags)
                for sb in range(NSB):
                    s0 = PC * p_ + 128 * sb
                    Pm, r_Pm = pmr()
                    for tb in range(NTB):
                        P.op("dve", lambda e, tb=tb, Pm=Pm, s0=s0, ex=ex: e.tensor_scalar(
                            out=Pm[:, tb, :], in0=io_f[:], scalar1=float(s0), scalar2=rkm[:, tb, ex:ex + 1],
                            op0=ALU.add, op1=ALU.is_equal), reads=[r_iof, r_rkm], writes=[r_Pm])
                    gb = (0, 1) if sb % 2 == 0 else (2, 3)
                    for kc in range(8):
                        bank, c0 = gb[kc // 4], (kc % 4) * 128
                        for tb in range(NTB):
                            P.op("pe", lambda e, tb=tb, kc=kc, bank=bank, c0=c0, Pm=Pm: e.matmul(
                                C.pb[bank][:, c0:c0 + 128], lhsT=h_tm[:, tb, kc * 128:(kc + 1) * 128], rhs=Pm[:, tb, :],
                                start=(tb == 0), stop=(tb == NTB - 1)), reads=[r_htm[tb], r_Pm], writes=[C.rpb[bank]])
                    for hh in range(2):
                        P.op("act", lambda e, hh=hh, sb=sb, gb=gb: e.copy(
                            hTe[:, hh * 4:(hh + 1) * 4, sb * 128:(sb + 1) * 128],
                            C.pb[gb[hh]][:, 0:512].rearrange("p (k s) -> p k s", k=4)),
                            reads=[C.rpb[gb[hh]]], writes=[r_hTe])
                    for col, cc in ((0, chh), (1, cll)):
                        for tb in range(NTB):
                            P.op("pe", lambda e, tb=tb, col=col, cc=cc, Pm=Pm, ex=ex: e.matmul(
                                C.pb[4][:, col:col + 1], lhsT=Pm[:, tb, :], rhs=cc[:, tb, ex:ex + 1],
                                start=(tb == 0), stop=(tb == NTB - 1)), reads=[r_Pm, r_chh, r_cll], writes=[C.rpb[4]])
                    gt, r_gt = gtr()
                    P.op("act", lambda e, gt=gt: e.copy(gt[:], C.pb[4][:, 0:2]), reads=[C.rpb[4]], writes=[r_gt])
                    P.op("dve", lambda e, gt=gt, sb=sb: e.tensor_tensor(out=gs[:, sb:sb + 1], in0=gt[:, 0:1], in1=gt[:, 1:2],
                                                                        op=ALU.add), reads=[r_gt, r_gs], writes=[r_gs])
                    for g8 in range(2):
                        bank = (5, 6)[g8]
                        for t8 in range(8):
                            tb = g8 * 8 + t8
                            P.op("pe", lambda e, tb=tb, t8=t8, bank=bank, Pm=Pm: e.transpose(
                                C.psbv[bank][:, t8 * 128:(t8 + 1) * 128], Pm[:, tb, :], C.ident[:]),
                                reads=[r_Pm, C.r_ident], writes=[C.rpb[bank]])
                        if g8 == 0:
                            P.op("act", lambda e, sb=sb, bank=bank: e.copy(PmT[:, sb, 0:1024], C.psbv[bank][:, 0:1024]),
                                 reads=[C.rpb[bank]], writes=[r_PmT[sb]])
                        else:
                            P.op("dve", lambda e, sb=sb, bank=bank: e.tensor_copy(PmT[:, sb, 1024:2048],
                                                                                  C.psbv[bank][:, 0:1024]),
                                 reads=[C.rpb[bank], r_PmT[sb]], writes=[r_PmT[sb]])
                for fc in range(NFC):
                    g_t, r_g, s_g = wgr()
                    u_t, r_u, s_u = wur()
                    P.dma("pool", lambda e, fc=fc, g_t=g_t, wg_v=wg_v: e.dma_start(
                        out=g_t[:], in_=wg_v[:, :, fc * 128:(fc + 1) * 128]), writes=[r_g], sem=s_g)
                    P.dma("pool", lambda e, fc=fc, u_t=u_t, wu_v=wu_v: e.dma_start(
                        out=u_t[:], in_=wu_v[:, :, fc * 128:(fc + 1) * 128]), writes=[r_u], sem=s_u)
                    pg0, pg1, pu0, pu1 = (0, 1, 2, 3) if fc % 2 == 0 else (4, 5, 6, 7)
                    for (bk, w_t, c0, c1, r_w_) in ((pg0, g_t, 0, 512, r_g), (pg1, g_t, 512, PC, r_g),
                                                    (pu0, u_t, 0, 512, r_u), (pu1, u_t, 512, PC, r_u)):
                        for kc in range(8):
                            P.op("pe", lambda e, kc=kc, w_t=w_t, bk=bk, c0=c0, c1=c1: e.matmul(
                                C.pb[bk][:, 0:c1 - c0], lhsT=w_t[:, kc, :], rhs=hTe[:, kc, c0:c1], start=(kc == 0),
                                stop=(kc == 7)), reads=[r_w_, r_hTe], writes=[C.rpb[bk]])
                    P.op("act", lambda e, fc=fc, pg0=pg0: e.activation(out=actT[:, fc, 0:512], in_=C.pb[pg0][:, :],
                                                                       func=AF.Silu), reads=[C.rpb[pg0]], writes=[r_act[fc]])
                    P.op("act", lambda e, fc=fc, pg1=pg1: e.activation(out=actT[:, fc, 512:PC], in_=C.pb[pg1][:, 0:PC - 512],
                                                                       func=AF.Silu), reads=[C.rpb[pg1], r_act[fc]],
                         writes=[r_act[fc]])
                    P.op("dve", lambda e, pu0=pu0, fc=fc: e.tensor_tensor(
                        out=actT[:, fc, 0:512], in0=actT[:, fc, 0:512], in1=C.pb[pu0][:, :], op=ALU.mult),
                        reads=[C.rpb[pu0], r_act[fc]], writes=[r_act[fc]])
                    P.op("dve", lambda e, pu1=pu1, fc=fc: e.tensor_tensor(
                        out=actT[:, fc, 512:PC], in0=actT[:, fc, 512:PC], in1=C.pb[pu1][:, 0:PC - 512], op=ALU.mult),
                        reads=[C.rpb[pu1], r_act[fc]], writes=[r_act[fc]])
                for hf_ in range(2):
                    banks = (0, 1, 2, 3, 4) if hf_ == 0 else (5, 6, 7, 0, 1)
                    for fc in range(NFC):
                        d_t, r_d, s_d = wdr()
                        P.dma("pool", lambda e, fc=fc, d_t=d_t, ex=ex, hf_=hf_: e.dma_start(
                            out=d_t[:], in_=wd[ex, fc * 128:(fc + 1) * 128, hf_ * 512:(hf_ + 1) * 512]),
                            writes=[r_d], sem=s_d)
                        for sb in range(NSB):
                            bk = banks[sb]
                            P.op("pe", lambda e, fc=fc, sb=sb, bk=bk, d_t=d_t: e.matmul(
                                C.pb[bk][:, :], lhsT=actT[:, fc, sb * 128:(sb + 1) * 128], rhs=d_t[:, :],
                                start=(fc == 0), stop=(fc == NFC - 1)), reads=[r_d, r_act[fc]], writes=[C.rpb[bk]])
                    for sb in range(NSB):
                        bk = banks[sb]
                        P.op("dve", lambda e, sb=sb, hf_=hf_, bk=bk: e.tensor_scalar(
                            out=yg[:, sb, hf_ * 512:(hf_ + 1) * 512], in0=C.pb[bk][:, :], scalar1=gs[:, sb:sb + 1],
                            scalar2=None, op0=ALU.mult), reads=[C.rpb[bk], r_gs, r_yg[sb]], writes=[r_yg[sb]])
                for tb in range(NTB):
                    for hf_ in range(2):
                        bk = (tb * 2 + hf_) % 4
                        for sb in range(NSB):
                            P.op("pe", lambda e, tb=tb, hf_=hf_, bk=bk, sb=sb: e.matmul(
                                C.pb[bk][:, :], lhsT=PmT[:, sb, tb * 128:(tb + 1) * 128],
                                rhs=yg[:, sb, hf_ * 512:(hf_ + 1) * 512], start=(sb == 0), stop=(sb == NSB - 1)),
                                reads=[r_PmT[sb], r_yg[sb]], writes=[C.rpb[bk]])
                        P.op("dve", lambda e, tb=tb, hf_=hf_, bk=bk: e.tensor_tensor(
                            out=xacc[:, tb, hf_ * 512:(hf_ + 1) * 512], in0=xacc[:, tb, hf_ * 512:(hf_ + 1) * 512],
                            in1=C.pb[bk][:, :], op=ALU.add), reads=[C.rpb[bk], r_xacc[tb]], writes=[r_xacc[tb]])
                P.cond_end()
        if defer_next:
            P.barrier()
            C.off = mark2
            alloc_next()
        for tb in range(NTB):
            if not last and x_out is not None:
                P.dma("sp", lambda e, tb=tb: e.dma_start(out=x_out[tb * 128:(tb + 1) * 128, :], in_=xacc[:, tb, :]),
                      reads=[r_xacc[tb]], writes=[d_out], sem=P.dsem())
            emit_next(xacc[:, tb, :], r_xacc[tb], tb)
        return

    HT = 1024
    NHB = HT // 128
    actT, _ = C.tile([128, NFC, HT], BF16)
    r_act = [[Res(f"act{f}_{s}") for s in range(HT // 512)] for f in range(NFC)]
    xacc, _ = C.tile([128, NHB, D], F32)
    r_xacc = [Res(f"xacc{i}") for i in range(NHB)]
    s_xa = [P.dsem() for _ in range(NHB)]
    wgr = C.ring("wg", 3, [128, 8, 128], BF16, with_sem=True, sw=True)
    wur = C.ring("wu", 3, [128, 8, 128], BF16, with_sem=True, sw=True)
    wdr = C.ring("wd", 3, [128, D], BF16, with_sem=True, sw=True)
    sgr = C.ring("sg", 2, [128, 512], F32)
    for half in range(TSH // HT):
        for j in range(NHB):
            tb = half * NHB + j
            P.dma("sp", lambda e, tb=tb, j=j: e.dma_start(out=xacc[:, j, :], in_=x1s[tb * 128:(tb + 1) * 128, :]),
                  reads=[r_x1s[tb]], writes=[r_xacc[j]], sem=s_xa[j])
        for ex in range(0 if os.environ.get('K_SKIP2') else ne):
            wg_v = wg[ex].rearrange("(kc p) f -> p kc f", p=128)
            wu_v = wu[ex].rearrange("(kc p) f -> p kc f", p=128)
            for fc in range(NFC):
                g_t, r_g, s_g = wgr()
                u_t, r_u, s_u = wur()
                P.dma("pool", lambda e, fc=fc, g_t=g_t, wg_v=wg_v: e.dma_start(
                    out=g_t[:], in_=wg_v[:, :, fc * 128:(fc + 1) * 128]), writes=[r_g], sem=s_g)
                P.dma("pool", lambda e, fc=fc, u_t=u_t, wu_v=wu_v: e.dma_start(
                    out=u_t[:], in_=wu_v[:, :, fc * 128:(fc + 1) * 128]), writes=[r_u], sem=s_u)
                if DBG2 == 1:
                    continue
                for st_ in range(HT // 512):
                    t0 = half * HT + st_ * 512
                    rh = [r_hT[(t0 // 128) + i] for i in range(4)]
                    pg, pu = (0, 1) if (fc * 2 + st_) % 2 == 0 else (2, 3)
                    for kc in range(8):
                        P.op("pe", lambda e, kc=kc, g_t=g_t, t0=t0, pg=pg: e.matmul(
                            C.pb[pg][:, :], lhsT=g_t[:, kc, :], rhs=hT[:, kc, t0:t0 + 512], start=(kc == 0),
                            stop=(kc == 7)), reads=[r_g] + rh, writes=[C.rpb[pg]], signal=(kc == 7))
                    for kc in range(8):
                        P.op("pe", lambda e, kc=kc, u_t=u_t, t0=t0, pu=pu: e.matmul(
                            C.pb[pu][:, :], lhsT=u_t[:, kc, :], rhs=hT[:, kc, t0:t0 + 512], start=(kc == 0),
                            stop=(kc == 7)), reads=[r_u] + rh, writes=[C.rpb[pu]], signal=(kc == 7))
                    sg, r_sg = sgr()
                    P.op("act", lambda e, sg=sg, pg=pg: e.activation(out=sg[:], in_=C.pb[pg][:, :], func=AF.Silu),
                         reads=[C.rpb[pg]], writes=[r_sg])
                    P.op("dve", lambda e, sg=sg, pu=pu, fc=fc, st_=st_: e.tensor_tensor(
                        out=actT[:, fc, st_ * 512:(st_ + 1) * 512], in0=sg[:], in1=C.pb[pu][:, :], op=ALU.mult),
                        reads=[r_sg, C.rpb[pu]], writes=[r_act[fc][st_]])
            for jg in range(0 if DBG2 in (1, 2) else HT // 512):
                for fc in range(NFC):
                    d_t, r_d, s_d = wdr()
                    P.dma("pool", lambda e, fc=fc, d_t=d_t, ex=ex: e.dma_start(
                        out=d_t[:], in_=wd[ex, fc * 128:(fc + 1) * 128, :]), writes=[r_d], sem=s_d)
                    for jj in range(4):
                        j = jg * 4 + jj
                        for hf_ in range(2):
                            bk = jj * 2 + hf_
                            P.op("pe", lambda e, fc=fc, j=j, hf_=hf_, bk=bk, d_t=d_t: e.matmul(
                                C.pb[bk][:, :], lhsT=actT[:, fc, j * 128:(j + 1) * 128],
                                rhs=d_t[:, hf_ * 512:(hf_ + 1) * 512], start=(fc == 0), stop=(fc == NFC - 1)),
                                reads=[r_d, r_act[fc][jg]], writes=[C.rpb[bk]], signal=(fc == NFC - 1 or (jj == 3 and hf_ == 1)))
                for jj in range(4):
                    j = jg * 4 + jj
                    tb = half * NHB + j
                    for hf_ in range(2):
                        bk = jj * 2 + hf_
                        if mode == "moe":
                            P.op("dve", lambda e, j=j, hf_=hf_, bk=bk, tb=tb, ex=ex: e.scalar_tensor_tensor(
                                out=xacc[:, j, hf_ * 512:(hf_ + 1) * 512], in0=C.pb[bk][:, :],
                                scalar=comb[:, tb, ex:ex + 1], in1=xacc[:, j, hf_ * 512:(hf_ + 1) * 512],
                                op0=ALU.mult, op1=ALU.add), reads=[C.rpb[bk], r_comb[tb], r_xacc[j]],
                                writes=[r_xacc[j]])
                        else:
                            P.op("dve", lambda e, j=j, hf_=hf_, bk=bk: e.tensor_tensor(
                                out=xacc[:, j, hf_ * 512:(hf_ + 1) * 512], in0=xacc[:, j, hf_ * 512:(hf_ + 1) * 512],
                                in1=C.pb[bk][:, :], op=ALU.add), reads=[C.rpb[bk], r_xacc[j]], writes=[r_xacc[j]])
        for j in range(NHB):
            tb = half * NHB + j
            if not last and x_out is not None:
                P.dma("sp", lambda e, tb=tb, j=j: e.dma_start(out=x_out[tb * 128:(tb + 1) * 128, :], in_=xacc[:, j, :]),
                      reads=[r_xacc[j]], writes=[d_out], sem=s_xa[j])
            emit_next(xacc[:, j, :], r_xacc[j], tb)
    return


def build_C(mode, last):
    nc = bass.Bass("TRN2", target_bir_lowering=False)
    C = Ctx(nc)
    ne = NE if mode in ("moe", "moes") else 1
    T = {"x": dram_in(nc, "x", [TSH, D], F32), "g_next": dram_in(nc, "g_next", [D], F32)}
    if mode != "N":
        T.update(mixT=dram_in(nc, "mixT", [D, TSH], BF16), w_out=dram_in(nc, "w_out", [D, D], F32),
                 g_ffn=dram_in(nc, "g_ffn", [D], F32), wg=dram_in(nc, "wg", [ne, D, FF], F32),
                 wu=dram_in(nc, "wu", [ne, D, FF], F32), wd=dram_in(nc, "wd", [ne, FF, D], F32))
        if mode in ("moe", "moes"):
            T["router"] = dram_in(nc, "router", [D, NE], F32)
    if last:
        T["y"] = dram_out(nc, "y", [TSH, D], F32)
    else:
        T["x_out"] = dram_out(nc, "x_out", [TSH, D], F32)
        T["hT"] = dram_out(nc, "hT", [D, TSH], BF16)
    emit_C(C, T, mode, last)
    C.P.wait_all("sp", [])
    C.P.emit()
    return nc


def emit_AB(C, T):
    nc = C.nc
    P = C.P
    hT_in = T["hT"]
    w_in, wq_up, wkv_up, qn, kvn = T["w_in"], T["wq_up"], T["wkv_up"], T["qn"], T["kvn"]
    scal, pos, cst, yT = T["scal"], T["pos"], T["cst"], T["yT"]
    C.uid += 1
    vscr = nc.dram_tensor(f"vscr{C.uid}", [S, 64], BF16).ap()
    d_out = Res("d_out")
    r_vscr = Res("vscr")
    NT = S // 512
    s_c = P.dsem()

    w_sb, r_w = C.tile([128, 8, NCOL], BF16)
    s_w = P.dsem(sw=True)
    w_v = w_in.rearrange("(kc p) n -> p kc n", p=128)
    for kc in range(8):
        P.dma("pool", lambda e, kc=kc: e.dma_start(out=w_sb[:, kc, :], in_=w_v[:, kc, :]), writes=[r_w], sem=s_w)
    wq_f, r_wqf = C.tile([128, 2, 96], F32)
    wq_s, r_wq = C.tile([128, 2, 96], BF16)
    qn_t, r_qn = C.tile([128, 2], F32)
    wkv_f, r_wkvf = C.tile([128, 128], F32)
    wkv_s, r_wkv = C.tile([128, 128], BF16)
    kvn_t, r_kvn = C.tile([128, 1], F32)
    P.dma("sp", lambda e: e.dma_start(out=wq_f[:], in_=wq_up.rearrange("(c p) n -> p c n", p=128)), writes=[r_wqf], sem=P.dsem())
    for c in range(2):
        P.dma("sp", lambda e, c=c: e.dma_start(out=qn_t[:, c:c + 1], in_=qn[c * 128:(c + 1) * 128].rearrange("(p o) -> p o", o=1)),
              writes=[r_qn], sem=P.dsem())
    P.dma("sp", lambda e: e.dma_start(out=wkv_f[:], in_=wkv_up), writes=[r_wkvf], sem=P.dsem())
    P.dma("sp", lambda e: e.dma_start(out=kvn_t[:], in_=kvn.rearrange("(p o) -> p o", o=1)), writes=[r_kvn], sem=P.dsem())
    for c in range(2):
        P.op("dve", lambda e, c=c: e.tensor_scalar(out=wq_s[:, c, :], in0=wq_f[:, c, :], scalar1=qn_t[:, c:c + 1], scalar2=None,
                                                   op0=ALU.mult), reads=[r_wqf, r_qn], writes=[r_wq])
    P.op("dve", lambda e: e.tensor_scalar(out=wkv_s[:], in0=wkv_f[:], scalar1=kvn_t[:, 0:1], scalar2=None, op0=ALU.mult),
         reads=[r_wkvf, r_kvn], writes=[r_wkv])
    sc0, r_sc0 = C.tile([1, 8], F32)
    sc64, r_sc64 = C.tile([128, 8], F32)
    P.dma("sp", lambda e: e.dma_start(out=sc0[:], in_=scal), writes=[r_sc0], sem=P.dsem())
    P.dma("sp", lambda e: e.dma_start(out=sc64[64:65, :], in_=scal), writes=[r_sc64], sem=P.dsem())
    P.op("dve", lambda e: e.tensor_scalar(out=sc0[:, 0:1], in0=sc0[:, 0:1], scalar1=-1.0, scalar2=None, op0=ALU.mult),
         reads=[r_sc0], writes=[r_sc0])
    P.op("act", lambda e: e.activation(out=sc64[64:65, 2:3], in_=sc64[64:65, 1:2], func=AF.Exp), reads=[r_sc64],
         writes=[r_sc64])
    ones_f, r_ones = C.tile([128, 64], F32)
    P.op("dve", lambda e: e.memset(ones_f[:], 1.0), writes=[r_ones])
    cst_t, r_cst = C.tile([128, 32], F32)
    P.dma("sp", lambda e: e.dma_start(out=cst_t[:], in_=cst), writes=[r_cst], sem=P.dsem())
    pos_i, r_posi = C.tile([128, 64], I32)
    pos_f, r_posf = C.tile([128, 64], F32)
    P.dma("sp", lambda e: e.dma_start(out=pos_i[:], in_=pos), writes=[r_posi], sem=P.dsem())
    P.op("dve", lambda e: e.tensor_copy(pos_f[:], pos_i[:]), reads=[r_posi], writes=[r_posf])
    ang, r_ang = C.tile([128, 64 * 16], F32)
    sin_t, r_sin = C.tile([128, 64 * 16], F32)
    cos_t, r_cos = C.tile([128, 64 * 16], F32)
    tkf, r_tkf = C.tile([128, 64 * 16], F32)
    tki, r_tki = C.tile([128, 64 * 16], I32)
    tfx, r_tfx = tkf, r_tkf
    for blk in range(64):
        P.op("dve", lambda e, blk=blk: e.tensor_scalar(out=ang[:, blk * 16:(blk + 1) * 16], in0=cst_t[:, 0:16],
                                                       scalar1=pos_f[:, blk:blk + 1], scalar2=None, op0=ALU.mult),
             reads=[r_cst, r_posf], writes=[r_ang])

    def emit_sin(dst, r_dst, off):
        md = dst
        P.op("dve", lambda e: e.tensor_scalar(out=tkf[:], in0=ang[:], scalar1=off, scalar2=1.0 / (2 * PI), op0=ALU.add,
                                              op1=ALU.mult), reads=[r_ang], writes=[r_tkf])
        P.op("dve", lambda e: e.tensor_copy(tki[:], tkf[:]), reads=[r_tkf], writes=[r_tki])
        P.op("dve", lambda e: e.tensor_copy(tkf[:], tki[:]), reads=[r_tki], writes=[r_tkf])
        P.op("dve", lambda e: e.scalar_tensor_tensor(out=md[:], in0=tkf[:], scalar=-2 * PI, in1=ang[:], op0=ALU.mult,
                                                     op1=ALU.add), reads=[r_tkf, r_ang], writes=[r_dst])
        if off != 0.0:
            P.op("dve", lambda e: e.tensor_scalar(out=md[:], in0=md[:], scalar1=off, scalar2=None, op0=ALU.add),
                 reads=[r_dst], writes=[r_dst])
        P.op("dve", lambda e: e.tensor_scalar(out=tfx[:], in0=md[:], scalar1=PI, scalar2=-2 * PI, op0=ALU.is_gt,
                                              op1=ALU.mult), reads=[r_dst], writes=[r_tfx])
        P.op("dve", lambda e: e.tensor_tensor(out=md[:], in0=md[:], in1=tfx[:], op=ALU.add), reads=[r_dst, r_tfx],
             writes=[r_dst])
        P.op("dve", lambda e: e.tensor_scalar(out=tfx[:], in0=md[:], scalar1=-PI, scalar2=2 * PI, op0=ALU.is_lt,
                                              op1=ALU.mult), reads=[r_dst], writes=[r_tfx])
        P.op("dve", lambda e: e.tensor_tensor(out=md[:], in0=md[:], in1=tfx[:], op=ALU.add), reads=[r_dst, r_tfx],
             writes=[r_dst])
        P.op("act", lambda e: e.activation(out=md[:], in_=md[:], func=AF.Sin), reads=[r_dst], writes=[r_dst])

    emit_sin(sin_t, r_sin, 0.0)
    emit_sin(cos_t, r_cos, PI / 2)
    trif, r_trif = C.tile([128, 128], F32)
    tri, r_tri = C.tile([128, 128], BF16)
    P.op("pool", lambda e: e.memset(trif[:], 1.0), writes=[r_trif])
    P.op("pool", lambda e: e.affine_select(out=trif[:], in_=trif[:], pattern=[[1, 128]], compare_op=ALU.is_ge, fill=0.0,
                                           base=0, channel_multiplier=-1), reads=[r_trif], writes=[r_trif])
    P.op("dve", lambda e: e.tensor_copy(tri[:], trif[:]), reads=[r_trif], writes=[r_tri])
    jmp_i, r_jmpi = C.tile([128, 256], I32)
    jmp_f, r_jmpf = C.tile([128, 256], F32)
    P.op("pool", lambda e: e.iota(jmp_i[:], pattern=[[1, 256]], base=0, channel_multiplier=-1), writes=[r_jmpi])
    P.op("dve", lambda e: e.tensor_copy(jmp_f[:], jmp_i[:]), reads=[r_jmpi], writes=[r_jmpf])
    Mb, r_Mb = [], []
    for m in range(4):
        t, r = C.tile([128, 256], F32)
        span = 127 if m == 0 else 128
        P.op("act", lambda e, t=t, m=m: e.activation(out=t[:], in_=jmp_f[:], func=AF.Exp, scale=cst_t[:, 16 + m:17 + m]),
             reads=[r_jmpf, r_cst], writes=[r])
        P.op("pool", lambda e, t=t: e.affine_select(out=t[:], in_=t[:], pattern=[[1, 256]], compare_op=ALU.is_ge, fill=0.0,
                                                    base=0, channel_multiplier=-1), reads=[r], writes=[r])
        P.op("pool", lambda e, t=t, span=span: e.affine_select(out=t[:], in_=t[:], pattern=[[-1, 256]], compare_op=ALU.is_ge,
                                                               fill=0.0, base=span, channel_multiplier=1), reads=[r], writes=[r])
        Mb.append(t)
        r_Mb.append(r)

    QT, _ = C.tile([128, S], BF16)
    KT, _ = C.tile([128, S], BF16)
    r_QT = [Res(f"QT{i}") for i in range(NT)]
    r_KT = [Res(f"KT{i}") for i in range(NT)]
    V = []
    r_V = []
    for i in range(3):
        t, _ = C.tile([128, 64, 65], BF16)
        V.append(t)
        r_V.append([Res(f"V{i}_{j}") for j in range(NT)])
        P.op("pool", lambda e, t=t: e.memset(t[:, :, 64:65], 1.0), writes=r_V[i])
    ysb, _ = C.tile([64, S], BF16)
    r_ysb = [Res(f"ysb{i}") for i in range(NT)]
    s_y = [P.dsem() for _ in range(NT)]
    acc, _ = C.tile([65, S], F32)
    r_acc = [Res(f"acc{i}") for i in range(NT)]
    hring = C.ring("hT", 2, [128, 8, 512], BF16, with_sem=True)
    pring = C.ring("pt", 4, [128, 512], BF16)
    ering = C.ring("et", 2, [128, 256], F32)
    oring = C.ring("osb", 3, [128, 512], F32)

    def load_hT(tt):
        sh = (tt * 512) // TSH
        tl0 = (tt * 512) % TSH
        h, r_h, s_h = hring()
        if "hT_fn" in T:
            src = T["hT_fn"](sh, tl0)
        else:
            src = hT_in[sh].rearrange("(kc p) t -> p kc t", p=128)[:, :, tl0:tl0 + 512]
        P.dma("sp", lambda e: e.dma_start(out=h[:], in_=src), writes=[r_h], sem=s_h)
        return h, r_h

    prot = {"i": 0}
    PROJ_BANKS = [0, 1, 2, 3, 5, 6]

    def next_bank():
        b = PROJ_BANKS[prot["i"] % len(PROJ_BANKS)]
        prot["i"] += 1
        return b

    def proj_fm(h, r_h, col0, m, dst_ap, r_dst, scale, bank):
        bank = next_bank()
        for kc in range(8):
            P.op("pe", lambda e, kc=kc: e.matmul(C.pb[bank][0:m, :], lhsT=w_sb[:, kc, col0:col0 + m], rhs=h[:, kc, :],
                                                 start=(kc == 0), stop=(kc == 7)), reads=[r_w, r_h], writes=[C.rpb[bank]])
        P.op("act", lambda e: e.mul(dst_ap, C.pb[bank][0:m, :], scale), reads=[C.rpb[bank]], writes=[r_dst])

    def proj_v(h, r_h, col0, tt, vt, r_vt, bank):
        bank = next_bank()
        for sb in range(4):
            for kc in range(8):
                P.op("pe", lambda e, kc=kc, sb=sb: e.matmul(
                    C.pb[bank][:, sb * 64:(sb + 1) * 64], lhsT=h[:, kc, sb * 128:(sb + 1) * 128],
                    rhs=w_sb[:, kc, col0:col0 + 64], start=(kc == 0), stop=(kc == 7)),
                    reads=[r_w, r_h], writes=[C.rpb[bank]])
        P.op("dve", lambda e: e.tensor_copy(vt[:, tt * 4:(tt + 1) * 4, 0:64],
                                            C.pb[bank][:, 0:256].rearrange("p (b d) -> p b d", b=4)),
             reads=[C.rpb[bank]], writes=[r_vt[tt]])

    def finalize(ob, src_ap, r_src, tq, sink):
        osb, r_osb = oring()
        P.op("act", lambda e: e.copy(osb[0:65, :], src_ap), reads=r_src, writes=[r_osb])
        if sink:
            P.op("dve", lambda e: e.tensor_scalar(out=osb[64:65, :], in0=osb[64:65, :], scalar1=sc64[64:65, 2:3],
                                                  scalar2=None, op0=ALU.add), reads=[r_osb, r_sc64], writes=[r_osb])
        P.op("dve", lambda e: e.reciprocal(osb[64:65, :], osb[64:65, :]), reads=[r_osb], writes=[r_osb])
        P.op("pe", lambda e: e.matmul(C.pb[4][0:64, :], lhsT=ones_f[64:65, 0:64], rhs=osb[64:65, :], start=True,
                                      stop=True), reads=[r_ones, r_osb], writes=[C.rpb[4]])
        P.op("dve", lambda e: e.tensor_tensor(out=ysb[0:64, tq * 512:(tq + 1) * 512], in0=osb[0:64, :],
                                              in1=C.pb[4][0:64, :], op=ALU.mult), reads=[r_osb, C.rpb[4]],
             writes=[r_ysb[tq]])

    def store_y(mixer):
        r_st = Res(f"yst{mixer}")
        P.dma("sp", lambda e: e.dma_start(out=yT[mixer * 64:(mixer + 1) * 64, :], in_=ysb[0:64, :]),
              reads=r_ysb, writes=[d_out, r_st], sem=s_y[0])
        if "after_store" in T:
            T["after_store"](mixer, r_st)

    def attn_causal(kdim):
        for qt in range(NT):
            t0 = qt * 512
            nkb = (t0 + 512) // 128
            ob = 2 + qt % 2

            SB = (0, 1, 5, 6)
            LA = 3

            def s_mm(kb):
                o = max(0, kb * 128 - t0)
                sb_ = SB[kb % 4]
                P.op("pe", lambda e, t0=t0, o=o, kb=kb, sb_=sb_: e.matmul(
                    C.pb[sb_][:, o:512], lhsT=KT[0:kdim, kb * 128:(kb + 1) * 128],
                    rhs=QT[0:kdim, t0 + o:t0 + 512], start=True, stop=True),
                     reads=[r_KT[kb // 4], r_QT[qt]], writes=[C.rpb[sb_]])
            for kb in range(min(LA, nkb)):
                s_mm(kb)
            for kb in range(nkb):
                if kb + LA < nkb:
                    s_mm(kb + LA)
                o = max(0, kb * 128 - t0)
                sb_ = SB[kb % 4]
                pt, r_pt = pring()
                P.op("act", lambda e, o=o, pt=pt, sb_=sb_: e.activation(out=pt[:, o:512], in_=C.pb[sb_][:, o:512],
                                                                        func=AF.Exp), reads=[C.rpb[sb_]], writes=[r_pt])
                if kb * 128 >= t0:
                    P.op("pool", lambda e, o=o, pt=pt: e.tensor_tensor(out=pt[:, o:o + 128], in0=pt[:, o:o + 128],
                                                                       in1=tri[:], op=ALU.mult), reads=[r_pt, r_tri],
                         writes=[r_pt])
                P.op("pe", lambda e, o=o, pt=pt, kb=kb, ob=ob, nkb=nkb: e.matmul(
                    C.pb[ob][0:65, o:512], lhsT=V[0][:, kb, 0:65], rhs=pt[:, o:512], start=(kb == 0),
                    stop=(kb == nkb - 1), skip_group_check=True), reads=[r_V[0][kb // 4], r_pt], writes=[C.rpb[ob]])
            finalize(ob, C.pb[ob][0:65, :], [C.rpb[ob]], qt, False)

    cnt = [0]

    def attn_banded(dil, m, vt, r_vt, evac):
        L = S // dil
        for r in range(dil):
            for qt in range(L // 512):
                n0 = qt * 512
                ob = 2 + cnt[0] % 2
                cnt[0] += 1
                kbs = [kb for kb in range(n0 // 128 - 1, n0 // 128 + 4) if kb >= 0]
                tl_lo = (dil * n0) // 512
                tl_hi = min(NT - 1, (dil * (n0 + 511) + r) // 512)
                rq = [r_QT[i] for i in range(tl_lo, tl_hi + 1)]
                SBK = (0, 1, 5, 6, 7)
                geo = []
                for i, kb in enumerate(kbs):
                    qa = max(kb * 128, n0)
                    qb = min(kb * 128 + 256, n0 + 512)
                    jo, w, co = qa - kb * 128, qb - qa, qa - n0
                    kc0 = r + dil * kb * 128
                    kcols = slice(kc0, kc0 + dil * 127 + 1, dil)
                    qcols = slice(r + dil * qa, r + dil * (qb - 1) + 1, dil)
                    ktl = sorted(set([(kc0) // 512, min(NT - 1, (kc0 + dil * 127) // 512)]))
                    rk = [r_KT[j] for j in range(ktl[0], ktl[-1] + 1)]
                    sbk = SBK[i]
                    geo.append((jo, w, co, sbk))
                    P.op("pe", lambda e, w=w, kcols=kcols, qcols=qcols, sbk=sbk: e.matmul(
                        C.pb[sbk][:, 0:w], lhsT=KT[0:64, kcols], rhs=QT[0:64, qcols], start=True, stop=True),
                        reads=rk + rq, writes=[C.rpb[sbk]])
                for i, kb in enumerate(kbs):
                    jo, w, co, sbk = geo[i]
                    et, r_et = ering()
                    pt, r_pt = pring()
                    P.op("act", lambda e, w=w, et=et, sbk=sbk: e.activation(out=et[:, 0:w], in_=C.pb[sbk][:, 0:w],
                                                                            func=AF.Exp), reads=[C.rpb[sbk]], writes=[r_et])
                    P.op("dve", lambda e, w=w, et=et, pt=pt, jo=jo: e.tensor_tensor(
                        out=pt[:, 0:w], in0=et[:, 0:w], in1=Mb[m][:, jo:jo + w], op=ALU.mult),
                        reads=[r_et, r_Mb[m]], writes=[r_pt])
                    ch = r * (L // 128) + kb
                    P.op("pe", lambda e, w=w, co=co, pt=pt, ch=ch, i=i, ob=ob, nk=len(kbs): e.matmul(
                        C.pb[ob][0:65, co:co + w], lhsT=vt[:, ch, 0:65], rhs=pt[:, 0:w], start=(i == 0),
                        stop=(i == nk - 1), skip_group_check=True), reads=[r_vt[ch // 4], r_pt], writes=[C.rpb[ob]])
                evac(ob, r, n0, tl_lo, tl_hi)

    P.op("dve", lambda e: e.memset(QT[64:70, :], 1.0), writes=r_QT)
    P.op("dve", lambda e: e.memset(KT[64:70, :], 1.0), writes=r_KT)
    s_augq = [P.dsem(sw=True) for _ in range(NT)]
    s_augk = [P.dsem(sw=True) for _ in range(NT)]
    cumr = C.ring("cum", 2, [1, 512], F32)
    one1, r_one1 = C.tile([1, 512], F32)
    P.op("dve", lambda e: e.memset(one1[:], 1.0), writes=[r_one1])
    zero1, r_zero1 = C.tile([1, 1], F32)
    P.op("dve", lambda e: e.memset(zero1[:], 0.0), writes=[r_zero1])
    spr = C.ring("sp", 1, [1, 512], F32)
    augr = C.ring("aug", 1, [1, 6, 512], BF16)
    rr = C.ring("rr", 1, [1, 2, 512], F32)
    prev = (zero1[:, 0:1], r_zero1)
    for tt in range(NT):
        h, r_h = load_hT(tt)
        cols = slice(tt * 512, (tt + 1) * 512)
        proj_fm(h, r_h, C_FQ, 64, QT[0:64, cols], r_QT[tt], 0.125, 5)
        proj_fm(h, r_h, C_FK, 64, KT[0:64, cols], r_KT[tt], 1.0, 6)
        proj_v(h, r_h, C_FV, tt, V[0], r_V[0], 5)
        for kc in range(8):
            P.op("pe", lambda e, kc=kc, h=h: e.matmul(C.pb[4][0:1, :], lhsT=w_sb[:, kc, C_FF:C_FF + 1], rhs=h[:, kc, :],
                                                      start=(kc == 0), stop=(kc == 7)), reads=[r_w, r_h], writes=[C.rpb[4]])
        sp_, r_sp = spr()
        P.op("act", lambda e, sp_=sp_: e.activation(out=sp_[:], in_=C.pb[4][0:1, :], func=AF.Exp, scale=-1.0,
                                                    bias=sc0[0:1, 0:1]), reads=[C.rpb[4], r_sc0], writes=[r_sp])
        P.op("act", lambda e, sp_=sp_: e.activation(out=sp_[:], in_=sp_[:], func=AF.Ln, bias=1.0), reads=[r_sp],
             writes=[r_sp])
        cm, r_cm = cumr()
        pv_ap, r_pv = prev
        P.op("dve", lambda e, cm=cm, sp_=sp_, pv_ap=pv_ap: e.tensor_tensor_scan(
            out=cm[:], data0=one1[:], data1=sp_[:], initial=pv_ap, op0=ALU.mult, op1=ALU.add),
            reads=[r_one1, r_sp, r_pv], writes=[r_cm])
        prev = (cm[:, 511:512], r_cm)
        ag, r_ag = augr()
        rs_, r_rs = rr()
        P.op("dve", lambda e, ag=ag, cm=cm: e.tensor_copy(ag[:, 3, :], cm[:]), reads=[r_cm], writes=[r_ag])
        P.op("dve", lambda e, ag=ag, cm=cm, rs_=rs_: e.tensor_tensor(out=rs_[:, 0, :], in0=cm[:], in1=ag[:, 3, :],
                                                                     op=ALU.subtract), reads=[r_cm, r_ag], writes=[r_rs])
        P.op("dve", lambda e, ag=ag, rs_=rs_: e.tensor_copy(ag[:, 4, :], rs_[:, 0, :]), reads=[r_rs], writes=[r_ag])
        P.op("dve", lambda e, ag=ag, rs_=rs_: e.tensor_tensor(out=rs_[:, 1, :], in0=rs_[:, 0, :], in1=ag[:, 4, :],
                                                              op=ALU.subtract), reads=[r_rs, r_ag], writes=[r_rs])
        P.op("dve", lambda e, ag=ag, rs_=rs_: e.tensor_copy(ag[:, 5, :], rs_[:, 1, :]), reads=[r_rs], writes=[r_ag])
        P.op("dve", lambda e, ag=ag: e.tensor_scalar(out=ag[:, 0:3, :], in0=ag[:, 3:6, :], scalar1=-1.0, scalar2=None,
                                                     op0=ALU.mult), reads=[r_ag], writes=[r_ag])
        for i in range(3):
            P.dma("pool", lambda e, i=i, ag=ag, cols=cols: e.dma_start(out=QT[64 + i:65 + i, cols], in_=ag[0:1, i, :]),
                  reads=[r_ag], writes=[r_QT[tt]], sem=s_augq[tt])
            P.dma("pool", lambda e, i=i, ag=ag, cols=cols: e.dma_start(out=KT[67 + i:68 + i, cols], in_=ag[0:1, 3 + i, :]),
                  reads=[r_ag], writes=[r_KT[tt]], sem=s_augk[tt])
    attn_causal(70)
    store_y(0)

    for tt in range(NT):
        h, r_h = load_hT(tt)
        cols = slice(tt * 512, (tt + 1) * 512)
        proj_fm(h, r_h, C_SQ, 64, QT[0:64, cols], r_QT[tt], 0.125, 5)
        proj_fm(h, r_h, C_SK, 64, KT[0:64, cols], r_KT[tt], 1.0, 6)
        proj_v(h, r_h, C_SV, tt, V[0], r_V[0], 5)
    attn_banded(1, 0, V[0], r_V[0],
                lambda ob, r, n0, lo, hi: finalize(ob, C.pb[ob][0:65, :], [C.rpb[ob]], n0 // 512, True))
    store_y(1)

    ctr = C.ring("ctm", 3, [128, 384], BF16)
    cTr = C.ring("cT", 3, [128, 3, 128], BF16)
    ssr = C.ring("ss2", 3, [128, 2], F32)
    qkf = C.ring("qkf", 3, [128, 2, 32], F32)
    qkb = C.ring("qkb", 3, [128, 2, 96], BF16)
    rtmp = C.ring("rtmp", 4, [128, 4, 16], F32)
    mla_state = {}

    def mla_A(blk):
        tt, sb = blk // 4, blk % 4
        if sb == 0:
            mla_state["h"] = load_hT(tt)
        h, r_h = mla_state["h"]
        bT, bQ, bC, bF = (0, 1)[blk % 2], (2, 3)[blk % 2], (5, 6)[blk % 2], (7, 4)[blk % 2]
        for kc in range(8):
            P.op("pe", lambda e, kc=kc, sb=sb, h=h, bT=bT: e.matmul(
                C.pb[bT][:, 0:416], lhsT=h[:, kc, sb * 128:(sb + 1) * 128], rhs=w_sb[:, kc, C_CQ:C_CQ + 416],
                start=(kc == 0), stop=(kc == 7)), reads=[r_w, r_h], writes=[C.rpb[bT]])
        ss, r_ss = ssr()
        P.op("dve", lambda e, ss=ss: e.memset(ss[:], 0.0), writes=[r_ss])
        P.op("act", lambda e, ss=ss, bT=bT: e.activation(out=C.junk[:, 0:256], in_=C.pb[bT][:, 0:256], func=AF.Square,
                                                  scale=1.0 / 16, accum_out=ss[:, 0:1]),
             reads=[C.rpb[bT], r_ss], writes=[C.r_junk, r_ss])
        P.op("act", lambda e, ss=ss, bT=bT: e.activation(out=C.junk[:, 0:128], in_=C.pb[bT][:, 256:384], func=AF.Square,
                                                  scale=float(128 ** -0.5), accum_out=ss[:, 1:2]),
             reads=[C.rpb[bT], r_ss], writes=[C.r_junk, r_ss])
        P.op("dve", lambda e, ss=ss: e.tensor_scalar(out=ss[:], in0=ss[:], scalar1=EPS, scalar2=None, op0=ALU.add),
             reads=[r_ss], writes=[r_ss])
        P.op("act", lambda e, ss=ss: e.activation(out=ss[:], in_=ss[:], func=AF.Sqrt), reads=[r_ss], writes=[r_ss])
        P.op("dve", lambda e, ss=ss: e.reciprocal(ss[:], ss[:]), reads=[r_ss], writes=[r_ss])
        ct, r_ct = ctr()
        P.op("dve", lambda e, ct=ct, bT=bT: e.tensor_copy(ct[:], C.pb[bT][:, 0:384]), reads=[C.rpb[bT]], writes=[r_ct])
        qf, r_qf = qkf()
        P.op("act", lambda e, qf=qf, bT=bT: e.copy(qf[:, 1, :], C.pb[bT][:, 384:416]), reads=[C.rpb[bT]], writes=[r_qf])
        mla_state[blk] = dict(ss=ss, r_ss=r_ss, ct=ct, r_ct=r_ct, qf=qf, r_qf=r_qf, bQ=bQ, bC=bC, bF=bF)

    def mla_B(blk):
        tt = blk // 4
        st = mla_state[blk]
        ss, r_ss, ct, r_ct, qf, r_qf, bQ, bC, bF = (st[k] for k in ("ss", "r_ss", "ct", "r_ct", "qf", "r_qf", "bQ", "bC", "bF"))
        cT, r_cT = cTr()
        emit_transposes_to(C, ct, r_ct, cT[:], r_cT, 3, bank=bC)
        for c in range(2):
            P.op("pe", lambda e, c=c, cT=cT, bQ=bQ: e.matmul(C.pb[bQ][:, 0:96], lhsT=cT[:, c, :], rhs=wq_s[:, c, :],
                                                      start=(c == 0), stop=(c == 1)), reads=[r_cT, r_wq],
                 writes=[C.rpb[bQ]])
        P.op("pe", lambda e, cT=cT, bQ=bQ: e.matmul(C.pb[bQ][:, 128:256], lhsT=cT[:, 2, :], rhs=wkv_s[:], start=True,
                                             stop=True), reads=[r_cT, r_wkv], writes=[C.rpb[bQ]])
        qb_, r_qb = qkb()
        sq = float(96 ** -0.5)
        P.op("dve", lambda e, qb_=qb_, ss=ss, bQ=bQ: e.tensor_scalar(out=qb_[:, 0, 0:64], in0=C.pb[bQ][:, 0:64],
                                                              scalar1=ss[:, 0:1], scalar2=sq, op0=ALU.mult, op1=ALU.mult),
             reads=[C.rpb[bQ], r_ss], writes=[r_qb])
        P.op("dve", lambda e, qf=qf, ss=ss, bQ=bQ: e.tensor_scalar(out=qf[:, 0, :], in0=C.pb[bQ][:, 64:96],
                                                            scalar1=ss[:, 0:1], scalar2=sq, op0=ALU.mult, op1=ALU.mult),
             reads=[C.rpb[bQ], r_ss, r_qf], writes=[r_qf])
        P.op("dve", lambda e, qb_=qb_, ss=ss, bQ=bQ: e.tensor_scalar(out=qb_[:, 1, 0:64], in0=C.pb[bQ][:, 128:192],
                                                              scalar1=ss[:, 1:2], scalar2=None, op0=ALU.mult),
             reads=[C.rpb[bQ], r_ss, r_qb], writes=[r_qb])
        P.op("dve", lambda e, ss=ss, blk=blk, bQ=bQ: e.tensor_scalar(out=V[0][:, blk, 0:64], in0=C.pb[bQ][:, 192:256],
                                                              scalar1=ss[:, 1:2], scalar2=None, op0=ALU.mult),
             reads=[C.rpb[bQ], r_ss], writes=[r_V[0][tt]])
        cs = cos_t[:, blk * 16:(blk + 1) * 16]
        sn = sin_t[:, blk * 16:(blk + 1) * 16]
        for w_ in range(2):
            tm_, r_tm = rtmp()
            t1 = qf[:, w_, 0:16]
            t2 = qf[:, w_, 16:32]
            eng = "pool" if w_ == 0 else "dve"
            P.op(eng, lambda e, t1=t1, tm_=tm_, cs=cs: e.tensor_tensor(out=tm_[:, 0, :], in0=t1, in1=cs, op=ALU.mult),
                 reads=[r_qf, r_cos], writes=[r_tm])
            P.op(eng, lambda e, t2=t2, tm_=tm_, sn=sn: e.tensor_tensor(out=tm_[:, 1, :], in0=t2, in1=sn, op=ALU.mult),
                 reads=[r_qf, r_sin, r_tm], writes=[r_tm])
            P.op(eng, lambda e, t2=t2, tm_=tm_, cs=cs: e.tensor_tensor(out=tm_[:, 2, :], in0=t2, in1=cs, op=ALU.mult),
                 reads=[r_qf, r_cos, r_tm], writes=[r_tm])
            P.op(eng, lambda e, t1=t1, tm_=tm_, sn=sn: e.tensor_tensor(out=tm_[:, 3, :], in0=t1, in1=sn, op=ALU.mult),
                 reads=[r_qf, r_sin, r_tm], writes=[r_tm])
            P.op(eng, lambda e, w_=w_, tm_=tm_, qb_=qb_: e.tensor_tensor(out=qb_[:, w_, 64:80], in0=tm_[:, 0, :],
                                                                         in1=tm_[:, 1, :], op=ALU.subtract),
                 reads=[r_tm, r_qb], writes=[r_qb])
            P.op(eng, lambda e, w_=w_, tm_=tm_, qb_=qb_: e.tensor_tensor(out=qb_[:, w_, 80:96], in0=tm_[:, 2, :],
                                                                         in1=tm_[:, 3, :], op=ALU.add),
                 reads=[r_tm, r_qb], writes=[r_qb])
        st.update(qb_=qb_, r_qb=r_qb)

    def mla_C(blk):
        tt = blk // 4
        st = mla_state.pop(blk)
        qb_, r_qb, bF = st["qb_"], st["r_qb"], st["bF"]
        for w_, (dst, r_d) in enumerate(((QT, r_QT[tt]), (KT, r_KT[tt]))):
            P.op("pe", lambda e, w_=w_, qb_=qb_, bF=bF: e.transpose(C.psbv[bF][0:96, w_ * 128:(w_ + 1) * 128], qb_[:, w_, :],
                                                             C.ident[:]), reads=[r_qb, C.r_ident], writes=[C.rpb[bF]])
            P.op("act", lambda e, w_=w_, dst=dst, blk=blk, bF=bF: e.copy(dst[0:96, blk * 128:(blk + 1) * 128],
                                                                  C.psbv[bF][0:96, w_ * 128:(w_ + 1) * 128]),
                 reads=[C.rpb[bF]], writes=[r_d])

    for i in range(64 + 2):
        if i < 64:
            mla_A(i)
        if 0 <= i - 1 < 64:
            mla_B(i - 1)
        if 0 <= i - 2 < 64:
            mla_C(i - 2)
    attn_causal(96)
    store_y(2)

    for tt in range(NT):
        h, r_h = load_hT(tt)
        cols = slice(tt * 512, (tt + 1) * 512)
        proj_fm(h, r_h, C_DQ, 64, QT[0:64, cols], r_QT[tt], 0.125, 5)
        proj_fm(h, r_h, C_DK, 64, KT[0:64, cols], r_KT[tt], 1.0, 6)
        proj_v(h, r_h, C_DV, tt, V[0], r_V[0], 5)
    s_v = P.dsem()
    s_vp = {1: P.dsem(), 2: P.dsem()}
    P.dma("sp", lambda e: e.dma_start(out=vscr.rearrange("(b p) d -> p b d", p=128), in_=V[0][:, :, 0:64]),
          reads=r_V[0], writes=[r_vscr], sem=s_v)
    for pi, (win, dil) in enumerate(DIL[1:], start=1):
        L = S // dil
        src = vscr.rearrange("(cc i r) d -> i r cc d", i=128, r=dil)
        dstv = V[pi][:, :, 0:64].rearrange("p (r cc) d -> p r cc d", r=dil)
        for r in range(dil):
            P.dma("sp", lambda e, r=r, src=src, dstv=dstv: e.dma_start(out=dstv[:, r, :, :], in_=src[:, r, :, :]),
                  reads=[r_vscr], writes=r_V[pi], sem=s_vp[pi])

    def evac_dil(first):
        def f(ob, r, n0, lo, hi, first=first):
            raise NotImplementedError
        return f

    for pi, (win, dil) in enumerate(DIL):
        def evac(ob, r, n0, lo, hi, pi=pi, dil=dil):
            dst = acc[0:65, slice(r + dil * n0, r + dil * (n0 + 511) + 1, dil)]
            ra = [r_acc[i] for i in range(lo, hi + 1)]
            if pi == 0:
                P.op("act", lambda e: e.copy(dst, C.pb[ob][0:65, :]), reads=[C.rpb[ob]], writes=ra)
            else:
                P.op("dve", lambda e: e.tensor_tensor(out=dst, in0=dst, in1=C.pb[ob][0:65, :], op=ALU.add),
                     reads=[C.rpb[ob]] + ra, writes=ra)
        attn_banded(dil, 1 + pi, V[pi], r_V[pi], evac)
    for tq in range(NT):
        finalize(None, acc[0:65, tq * 512:(tq + 1) * 512], [r_acc[tq]], tq, False)
    store_y(3)
    return


def build_AB():
    nc = bass.Bass("TRN2", target_bir_lowering=False)
    C = Ctx(nc)
    T = {"hT": dram_in(nc, "hT", [4, D, TSH], BF16), "w_in": dram_in(nc, "w_in", [D, NCOL], F32),
         "wq_up": dram_in(nc, "wq_up", [256, 96], F32), "wkv_up": dram_in(nc, "wkv_up", [128, 128], F32),
         "qn": dram_in(nc, "qn", [256], F32), "kvn": dram_in(nc, "kvn", [128], F32),
         "scal": dram_in(nc, "scal", [1, 8], F32), "pos": dram_in(nc, "pos", [128, 64], I32),
         "cst": dram_in(nc, "cst", [128, 32], F32), "yT": dram_out(nc, "yT", [256, S], BF16)}
    emit_AB(C, T)
    C.P.wait_all("sp", [])
    C.P.emit()
    return nc


def ab_inputs_common(head):
    slopes = 2.0 ** (-8.0 * np.arange(1, 9, dtype=np.float64) / 8.0)
    c = np.zeros((128, 32), np.float32)
    c[:, 0:16] = (10000.0 ** (-np.arange(16, dtype=np.float32) / np.float32(16))).astype(np.float32)[None, :]
    c[:, 16] = -slopes[head]
    for pi, (win, dil) in enumerate(DIL):
        c[:, 17 + pi] = -slopes[4 + head] * dil
    return c


GROUPS = [[0, 1, 2, 3], [4, 5, 6, 7]]


def build_fused():
    nc = bass.Bass("TRN2", target_bir_lowering=False, num_devices=NCORE)
    C = Ctx(nc)
    P = C.P
    x = dram_in(nc, "x", [TSH, D], F32)
    idx = dram_in(nc, "idx", [1, 1], I32)
    pos = dram_in(nc, "pos", [128, 64], I32)
    cst = dram_in(nc, "cst", [128, 32], F32)
    g0 = dram_in(nc, "g0", [D], F32)
    L = []
    for l in range(2):
        L.append({"w_in": dram_in(nc, f"w_in{l}", [D, NCOL], F32), "wq_up": dram_in(nc, f"wq_up{l}", [256, 96], F32),
                  "wkv_up": dram_in(nc, f"wkv_up{l}", [128, 128], F32), "qn": dram_in(nc, f"qn{l}", [256], F32),
                  "kvn": dram_in(nc, f"kvn{l}", [128], F32), "scal": dram_in(nc, f"scal{l}", [1, 8], F32),
                  "w_out": dram_in(nc, f"w_out{l}", [D, D], F32), "g_ffn": dram_in(nc, f"g_ffn{l}", [D], F32),
                  "g_next": dram_in(nc, f"g_next{l}", [D], F32)})
    ffn = [{"wg": dram_in(nc, "wg0", [1, D, FF], F32), "wu": dram_in(nc, "wu0", [1, D, FF], F32),
            "wd": dram_in(nc, "wd0", [1, FF, D], F32)},
           {"wg": dram_in(nc, "wg1", [NE, D, FF], F32), "wu": dram_in(nc, "wu1", [NE, D, FF], F32),
            "wd": dram_in(nc, "wd1", [NE, FF, D], F32), "router": dram_in(nc, "router", [D, NE], F32)}]
    y = dram_out(nc, "y", [TSH, D], F32)
    hT_loc = nc.dram_tensor("hT_loc", [D, TSH], BF16)
    hT_all = nc.dram_tensor("hT_all", [4 * D, TSH], BF16)
    y_loc = nc.dram_tensor("y_loc", [256, S], BF16)
    y_all = nc.dram_tensor("y_all", [1024, S], BF16)
    x_mid = nc.dram_tensor("x_mid", [TSH, D], F32)
    mix_own = nc.dram_tensor("mix_own", [D, TSH], BF16)
    reg_tok = P.es.enter_context(nc.gpsimd.register("rtok"))
    reg_tmp = P.es.enter_context(nc.gpsimd.register("rtmp"))
    idx_sb, r_idx = C.tile([1, 1], I32)
    C.base = C.off
    P.dma("sp", lambda e: e.dma_start(out=idx_sb[:], in_=idx), writes=[r_idx], sem=P.dsem())

    def ld_idx(e):
        e.reg_load(reg_tok, idx_sb[0:1, 0:1])
        return e.reg_mul(reg_tok, reg_tok, TSH)
    P.op("pool", ld_idx, reads=[r_idx])
    dmy, r_dmy = C.tile([128, 16], F32)
    C.base = C.off
    P.op("pool", lambda e: e.memset(dmy[:], 0.0), writes=[r_dmy])
    FSTOP = int(os.environ.get("K_FSTOP", "99"))

    def finish():
        P.wait_all("sp", [])
        P.emit()
        return nc
    ncc = [0]

    ccs = [P.newsem("DCC1"), P.newsem("DCC2")]

    def allgather(src_t, dst_t, nrows, rc):
        P.barrier()
        r_cc = Res("cc")
        for i in range(nrows // rc):
            P.dma("pool", lambda e, i=i: e.collective_compute(
                "AllGather", ALU.bypass, replica_groups=GROUPS, ins=[src_t.ap()[i * rc:(i + 1) * rc, :]],
                outs=[dst_t.ap()[i * 4 * rc:(i + 1) * 4 * rc, :]]), writes=[r_cc], sem=ccs[i % 2], inc=1)
        P.barrier()
        C.reset()

    hT_view = hT_all.ap().rearrange("(kc s p) t -> s p kc t", kc=8, s=4)

    def hT_fn(sh, tl0):
        return hT_view[sh][:, :, tl0:tl0 + 512]

    emit_C(C, {"x": x, "g_next": g0, "hT": hT_loc.ap(), "x_out": None}, "N", False)
    if FSTOP == 0:
        return finish()
    allgather(hT_loc, hT_all, D, 128)
    if FSTOP == 1:
        return finish()
    for l in range(2):
        T = dict(L[l])
        r_ycc = Res("ycc")

        def after_store(mixer, r_st):
            for i in (2 * mixer, 2 * mixer + 1):
                P.dma("pool", lambda e, i=i: e.collective_compute(
                    "AllGather", ALU.bypass, replica_groups=GROUPS, ins=[y_loc.ap()[i * 32:(i + 1) * 32, :]],
                    outs=[y_all.ap()[i * 128:(i + 1) * 128, :]]), reads=[r_st], writes=[r_ycc], sem=ccs[i % 2], inc=1)
        T.update(hT=None, hT_fn=hT_fn, pos=pos, cst=cst, yT=y_loc.ap(), after_store=after_store)
        emit_AB(C, T)
        if FSTOP == 2:
            return finish()
        P.barrier()
        C.reset()
        if FSTOP == 3:
            return finish()
        T = dict(L[l])
        T.update(ffn[l])
        for half in range(2):
            def ld_own(e, half=half):
                e.reg_add(reg_tmp, reg_tok, half * 512 * S)
                src = bass.AP(y_all, reg_tmp, [[S, 512], [1, TSH]])
                return e.dma_start(out=mix_own.ap()[half * 512:(half + 1) * 512, :], in_=src)
            P.dma("pool", ld_own, sem=P.dsem(sw=True))
        P.barrier()
        if FSTOP == 4:
            return finish()
        T.update(x=(x if l == 0 else x_mid.ap()), mixT=mix_own.ap())
        if l == 0:
            T.update(x_out=x_mid.ap(), hT=hT_loc.ap())
            emit_C(C, T, "dense", False)
            if FSTOP == 5:
                return finish()
            allgather(hT_loc, hT_all, D, 128)
        else:
            T["y"] = y
            emit_C(C, T, "moes", True)
    P.wait_all("sp", [])
    P.emit()
    return nc


_PROGS = {}


def _prog(key, fn):
    if key not in _PROGS:
        _PROGS[key] = fn()
    return _PROGS[key]


def _run(nc, maps):
    res = run_bass_kernel_spmd(nc, maps, core_ids=list(range(NCORE)))
    return res.results


def kernel_unfused(x, positions, attn_norm, w_in, b_forget, mla_q_norm, w_q_up, mla_kv_norm, w_kv_up, sinks, w_out, ffn_norm,
           dense_w_gate, dense_w_up, dense_w_down, router, moe_w_gate, moe_w_up, moe_w_down, final_norm):
    f32 = np.float32
    x = np.asarray(x, f32)
    cores = list(range(NCORE))
    bt = [(c // 4, c % 4) for c in cores]
    maps = [{"x": np.ascontiguousarray(x[b, j * TSH:(j + 1) * TSH]), "g_next": np.asarray(attn_norm[0], f32)}
            for (b, j) in bt]
    r = _run(_prog("N", lambda: build_C("N", False)), maps)
    x_cur = [rr["x_out"] for rr in r]
    hT = [rr["hT"] for rr in r]
    o_fq, o_fk, o_fv, o_ff, o_sq, o_sk, o_sv, o_cq, o_dq, o_dk, o_dv = 0, 256, 512, 768, 772, 1028, 1156, 1284, 1700, 1956, 2212
    pos = np.asarray(positions, np.int32)
    out = None
    for layer in range(2):
        wl = np.asarray(w_in[layer], f32)
        maps = []
        for (b, j) in bt:
            kv = j // 2
            cols = np.concatenate([
                np.arange(o_fq + j * 64, o_fq + (j + 1) * 64), np.arange(o_fk + j * 64, o_fk + (j + 1) * 64),
                np.arange(o_fv + j * 64, o_fv + (j + 1) * 64), np.arange(o_ff + j, o_ff + j + 1),
                np.arange(o_sq + j * 64, o_sq + (j + 1) * 64), np.arange(o_sk + kv * 64, o_sk + (kv + 1) * 64),
                np.arange(o_sv + kv * 64, o_sv + (kv + 1) * 64), np.arange(o_cq, o_cq + 416),
                np.arange(o_dq + j * 64, o_dq + (j + 1) * 64), np.arange(o_dk + j * 64, o_dk + (j + 1) * 64),
                np.arange(o_dv + j * 64, o_dv + (j + 1) * 64)])
            scal = np.zeros((1, 8), f32)
            scal[0, 0] = b_forget[layer][j]
            scal[0, 1] = sinks[layer][j]
            maps.append({
                "hT": np.ascontiguousarray(np.stack([hT[b * 4 + s] for s in range(4)], 0)),
                "w_in": np.ascontiguousarray(wl[:, cols]),
                "wq_up": np.ascontiguousarray(np.asarray(w_q_up[layer], f32)[:, j * 96:(j + 1) * 96]),
                "wkv_up": np.ascontiguousarray(np.asarray(w_kv_up[layer], f32)[:, j * 128:(j + 1) * 128]),
                "qn": np.asarray(mla_q_norm[layer], f32), "kvn": np.asarray(mla_kv_norm[layer], f32),
                "scal": scal, "pos": np.ascontiguousarray(pos[b].reshape(64, 128).T),
                "cst": ab_inputs_common(j)})
        r = _run(_prog("AB", build_AB), maps)
        yT = [rr["yT"] for rr in r]
        perm = np.array([m * 256 + j * 64 + d for j in range(4) for m in range(4) for d in range(64)])
        wo = np.ascontiguousarray(np.asarray(w_out[layer], f32)[perm])
        last = layer == 1
        maps = []
        for (b, j) in bt:
            mixT = np.ascontiguousarray(np.concatenate([yT[b * 4 + jj][:, j * TSH:(j + 1) * TSH] for jj in range(4)], 0))
            m = {"x": x_cur[b * 4 + j], "mixT": mixT, "w_out": wo, "g_ffn": np.asarray(ffn_norm[layer], f32),
                 "g_next": np.asarray(final_norm if last else attn_norm[layer + 1], f32)}
            if layer == 0:
                m.update(wg=np.asarray(dense_w_gate, f32), wu=np.asarray(dense_w_up, f32), wd=np.asarray(dense_w_down, f32))
            else:
                m.update(wg=np.asarray(moe_w_gate[0], f32), wu=np.asarray(moe_w_up[0], f32),
                         wd=np.asarray(moe_w_down[0], f32), router=np.asarray(router[0], f32))
            maps.append(m)
        if layer == 0:
            r = _run(_prog("Cd", lambda: build_C("dense", False)), maps)
            x_cur = [rr["x_out"] for rr in r]
            hT = [rr["hT"] for rr in r]
        else:
            r = _run(_prog("Cm", lambda: build_C("moe", True)), maps)
            out = np.zeros((NB, S, D), f32)
            for (b, j), rr in zip(bt, r):
                out[b, j * TSH:(j + 1) * TSH] = rr["y"]
    return out


def _core_cols(j):
    o_fq, o_fk, o_fv, o_ff, o_sq, o_sk, o_sv, o_cq, o_dq, o_dk, o_dv = 0, 256, 512, 768, 772, 1028, 1156, 1284, 1700, 1956, 2212
    kv = j // 2
    return np.concatenate([
        np.arange(o_fq + j * 64, o_fq + (j + 1) * 64), np.arange(o_fk + j * 64, o_fk + (j + 1) * 64),
        np.arange(o_fv + j * 64, o_fv + (j + 1) * 64), np.arange(o_ff + j, o_ff + j + 1),
        np.arange(o_sq + j * 64, o_sq + (j + 1) * 64), np.arange(o_sk + kv * 64, o_sk + (kv + 1) * 64),
        np.arange(o_sv + kv * 64, o_sv + (kv + 1) * 64), np.arange(o_cq, o_cq + 416),
        np.arange(o_dq + j * 64, o_dq + (j + 1) * 64), np.arange(o_dk + j * 64, o_dk + (j + 1) * 64),
        np.arange(o_dv + j * 64, o_dv + (j + 1) * 64)])


def kernel(x, positions, attn_norm, w_in, b_forget, mla_q_norm, w_q_up, mla_kv_norm, w_kv_up, sinks, w_out, ffn_norm,
           dense_w_gate, dense_w_up, dense_w_down, router, moe_w_gate, moe_w_up, moe_w_down, final_norm):
    f32 = np.float32
    x = np.asarray(x, f32)
    pos = np.asarray(positions, np.int32)
    perm = np.array([((c8 * 32 + r) // 64) * 256 + j * 64 + (c8 * 32 + r) % 64
                     for c8 in range(8) for j in range(4) for r in range(32)])
    wo = [np.ascontiguousarray(np.asarray(w_out[l], f32)[perm]) for l in range(2)]
    shared = {"g0": np.asarray(attn_norm[0], f32),
              "wg0": np.asarray(dense_w_gate, f32), "wu0": np.asarray(dense_w_up, f32), "wd0": np.asarray(dense_w_down, f32),
              "wg1": np.asarray(moe_w_gate[0], f32), "wu1": np.asarray(moe_w_up[0], f32), "wd1": np.asarray(moe_w_down[0], f32),
              "router": np.asarray(router[0], f32)}
    for l in range(2):
        shared[f"qn{l}"] = np.asarray(mla_q_norm[l], f32)
        shared[f"kvn{l}"] = np.asarray(mla_kv_norm[l], f32)
        shared[f"w_out{l}"] = wo[l]
        shared[f"g_ffn{l}"] = np.asarray(ffn_norm[l], f32)
        shared[f"g_next{l}"] = np.asarray(attn_norm[1] if l == 0 else final_norm, f32)
    maps = []
    for c in range(NCORE):
        b, j = c // 4, c % 4
        m = dict(shared)
        m["x"] = np.ascontiguousarray(x[b, j * TSH:(j + 1) * TSH])
        m["idx"] = np.array([[j]], np.int32)
        m["pos"] = np.ascontiguousarray(pos[b].reshape(64, 128).T)
        m["cst"] = ab_inputs_common(j)
        cols = _core_cols(j)
        for l in range(2):
            m[f"w_in{l}"] = np.ascontiguousarray(np.asarray(w_in[l], f32)[:, cols])
            m[f"wq_up{l}"] = np.ascontiguousarray(np.asarray(w_q_up[l], f32)[:, j * 96:(j + 1) * 96])
            m[f"wkv_up{l}"] = np.ascontiguousarray(np.asarray(w_kv_up[l], f32)[:, j * 128:(j + 1) * 128])
            sc = np.zeros((1, 8), f32)
            sc[0, 0] = b_forget[l][j]
            sc[0, 1] = sinks[l][j]
            m[f"scal{l}"] = sc
        maps.append(m)
    r = _run(_prog("fused", build_fused), maps)
    out = np.zeros((NB, S, D), f32)
    for c in range(NCORE):
        out[c // 4, (c % 4) * TSH:(c % 4 + 1) * TSH] = r[c]["y"]
    return out
```
